# Optimizing a Trainium2 kernel written in Bass

```python
import jax, jax.numpy as jnp
from jax import lax
import numpy as np

D_MODEL = 2048
BATCH = 4
SEQ = 4096
DEPTH = 2

MLA_HEADS = 16
MLA_NOPE = 128
MLA_ROPE = 64
MLA_QK = MLA_NOPE + MLA_ROPE
MLA_V = 128
Q_LORA = 512
KV_LORA = 512
ROPE_THETA = 10000.0
SB_HEADS = 16
SB_DIM = 64
FOX_HEADS = 16
FOX_DIM = 64
BLOCK_Q = 128
N_BRANCH = 3
EPS = 1e-6
N_GROUPS = 8
EXPERTS_PER_GROUP = 8
N_EXPERTS = N_GROUPS * EXPERTS_PER_GROUP
TOP_K = 2
D_EXPERT = 512
MOE_BLOCK = 128
D_IN = (Q_LORA + KV_LORA + MLA_ROPE + 3 * SB_HEADS * SB_DIM + 3 * FOX_HEADS * FOX_DIM
        + FOX_HEADS + N_BRANCH * D_MODEL)

kernel_name = "hybrid_mla_stickbreak_fox_hmoe"


def _segment_offsets():
    segs = (("mla_cq", Q_LORA), ("mla_ckv", KV_LORA), ("mla_krope", MLA_ROPE),
            ("sb_q", SB_HEADS * SB_DIM), ("sb_k", SB_HEADS * SB_DIM), ("sb_v", SB_HEADS * SB_DIM),
            ("fox_q", FOX_HEADS * FOX_DIM), ("fox_k", FOX_HEADS * FOX_DIM), ("fox_v", FOX_HEADS * FOX_DIM),
            ("fox_f", FOX_HEADS), ("gates", N_BRANCH * D_MODEL))
    off, start = {}, 0
    for name, width in segs:
        off[name] = (start, start + width)
        start += width
    return off


def _proj(xn, w_in_l, off, name):
    a, b = off[name]
    return xn @ w_in_l[:, a:b]


def _rms(x, g):
    xf = x.astype(jnp.float32)
    y = xf * lax.rsqrt(jnp.mean(xf * xf, axis=-1, keepdims=True) + EPS)
    return (y * g.astype(jnp.float32)).astype(x.dtype)


def _rope_tables(positions):
    inv_freq = ROPE_THETA ** (-jnp.arange(0, MLA_ROPE, 2, dtype=jnp.float32) / MLA_ROPE)
    ang = positions.astype(jnp.float32)[..., None] * inv_freq
    return jnp.cos(ang), jnp.sin(ang)


def _rope(x, cos, sin):
    half = x.shape[-1] // 2
    xf = x.astype(jnp.float32)
    x1, x2 = xf[..., :half], xf[..., half:]
    return jnp.concatenate([x1 * cos - x2 * sin, x2 * cos + x1 * sin], axis=-1).astype(x.dtype)


def _softmax_attention(q, k, v, scale, log_decay=None):
    S = q.shape[1]
    outs = []
    for i in range(S // BLOCK_Q):
        lo, hi = i * BLOCK_Q, (i + 1) * BLOCK_Q
        s = jnp.einsum("bqhd,bkhd->bhqk", q[:, lo:hi], k[:, :hi]).astype(jnp.float32) * scale
        if log_decay is not None:
            s = s + (log_decay[:, :, lo:hi, None] - log_decay[:, :, None, :hi])
        mask = (lo + jnp.arange(BLOCK_Q))[:, None] >= jnp.arange(hi)[None, :]
        p = jax.nn.softmax(jnp.where(mask, s, -jnp.inf), axis=-1)
        outs.append(jnp.einsum("bhqk,bkhd->bqhd", p.astype(v.dtype), v[:, :hi]))
    return jnp.concatenate(outs, axis=1)


def _stick_breaking_attention(q, k, v, scale):
    S = q.shape[1]
    outs = []
    for i in range(S // BLOCK_Q):
        lo, hi = i * BLOCK_Q, (i + 1) * BLOCK_Q
        z = jnp.einsum("bqhd,bkhd->bhqk", q[:, lo:hi], k[:, :hi]).astype(jnp.float32) * scale
        strict = (lo + jnp.arange(BLOCK_Q))[:, None] > jnp.arange(hi)[None, :]
        log_keep = jnp.where(strict, jax.nn.log_sigmoid(-z), 0.0)
        suffix = lax.cumsum(log_keep, axis=3, reverse=True) - log_keep
        w = jnp.where(strict, jnp.exp(jax.nn.log_sigmoid(z) + suffix), 0.0)
        outs.append(jnp.einsum("bhqk,bkhd->bqhd", w.astype(v.dtype), v[:, :hi]))
    return jnp.concatenate(outs, axis=1)


def _mla_branch(xn, w_in_l, off, g_cq, w_uq, g_ckv, w_ukv, g_q, g_k, cos, sin):
    B, S, _ = xn.shape
    c_q = _rms(_proj(xn, w_in_l, off, "mla_cq"), g_cq)
    q = (c_q @ w_uq).reshape(B, S, MLA_HEADS, MLA_QK)
    c_kv = _rms(_proj(xn, w_in_l, off, "mla_ckv"), g_ckv)
    kv = (c_kv @ w_ukv).reshape(B, S, MLA_HEADS, MLA_NOPE + MLA_V)
    k_nope, v = kv[..., :MLA_NOPE], kv[..., MLA_NOPE:]
    k_rope = _proj(xn, w_in_l, off, "mla_krope")
    k_rope = jnp.broadcast_to(k_rope[:, :, None, :], (B, S, MLA_HEADS, MLA_ROPE))
    k = jnp.concatenate([k_nope, k_rope], axis=-1)
    q, k = _rms(q, g_q), _rms(k, g_k)
    c, s = cos[:, :, None, :], sin[:, :, None, :]
    q = jnp.concatenate([q[..., :MLA_NOPE], _rope(q[..., MLA_NOPE:], c, s)], axis=-1)
    k = jnp.concatenate([k[..., :MLA_NOPE], _rope(k[..., MLA_NOPE:], c, s)], axis=-1)
    o = _softmax_attention(q, k, v, MLA_QK ** -0.5)
    return o.reshape(B, S, MLA_HEADS * MLA_V)


def _sb_branch(xn, w_in_l, off, g_q, g_k):
    B, S, _ = xn.shape
    shp = (B, S, SB_HEADS, SB_DIM)
    q = _rms(_proj(xn, w_in_l, off, "sb_q").reshape(shp), g_q)
    k = _rms(_proj(xn, w_in_l, off, "sb_k").reshape(shp), g_k)
    v = _proj(xn, w_in_l, off, "sb_v").reshape(shp)
    o = _stick_breaking_attention(q, k, v, SB_DIM ** -0.5)
    return o.reshape(B, S, SB_HEADS * SB_DIM)


def _fox_branch(xn, w_in_l, off, g_q, g_k, f_bias):
    B, S, _ = xn.shape
    shp = (B, S, FOX_HEADS, FOX_DIM)
    q = _rms(_proj(xn, w_in_l, off, "fox_q").reshape(shp), g_q)
    k = _rms(_proj(xn, w_in_l, off, "fox_k").reshape(shp), g_k)
    v = _proj(xn, w_in_l, off, "fox_v").reshape(shp)
    f_logit = _proj(xn, w_in_l, off, "fox_f").astype(jnp.float32) + f_bias.astype(jnp.float32)
    cum_log_f = jnp.cumsum(jax.nn.log_sigmoid(f_logit), axis=1)
    o = _softmax_attention(q, k, v, FOX_DIM ** -0.5, jnp.transpose(cum_log_f, (0, 2, 1)))
    return o.reshape(B, S, FOX_HEADS * FOX_DIM)


def _hier_moe(h, w_group, w_router, w_gate, w_up, w_down):
    N, D = h.shape
    rows = jnp.arange(N)
    g_logits = (h @ w_group).astype(jnp.float32)
    g_prob = jax.nn.softmax(g_logits, axis=-1)
    g_idx = jnp.argmax(g_logits, axis=-1)
    p_group = g_prob[rows, g_idx]
    e_logits = (h @ w_router).astype(jnp.float32).reshape(N, N_GROUPS, EXPERTS_PER_GROUP)
    e_in = e_logits[rows, g_idx]
    top_v, top_i = lax.top_k(e_in, TOP_K)
    gate = p_group[:, None] * jax.nn.softmax(top_v, axis=-1)
    expert = g_idx[:, None] * EXPERTS_PER_GROUP + top_i
    A = N * TOP_K
    e_flat = expert.reshape(-1)
    tok_flat = jnp.repeat(rows, TOP_K)
    order = jnp.argsort(e_flat)
    e_s, tok_s, w_s = e_flat[order], tok_flat[order], gate.reshape(-1)[order]
    counts = jnp.bincount(e_flat, length=N_EXPERTS)
    starts = jnp.cumsum(counts) - counts
    padded = ((counts + MOE_BLOCK - 1) // MOE_BLOCK) * MOE_BLOCK
    pad_ends = jnp.cumsum(padded)
    pad_starts = pad_ends - padded
    dest = pad_starts[e_s] + (jnp.arange(A) - starts[e_s])
    n_blocks = (A + N_EXPERTS * (MOE_BLOCK - 1)) // MOE_BLOCK + 1
    P = n_blocks * MOE_BLOCK
    buf_tok = jnp.zeros((P,), jnp.int32).at[dest].set(tok_s.astype(jnp.int32))
    buf_w = jnp.zeros((P,), h.dtype).at[dest].set(w_s.astype(h.dtype))
    block_e = jnp.minimum(
        jnp.searchsorted(pad_ends, jnp.arange(n_blocks) * MOE_BLOCK, side="right"), N_EXPERTS - 1)

    def expert_block(args):
        tok, e = args
        xb = h[tok]
        return (jax.nn.silu(xb @ w_gate[e]) * (xb @ w_up[e])) @ w_down[e]

    y = lax.map(expert_block, (buf_tok.reshape(n_blocks, MOE_BLOCK), block_e))
    y = y.reshape(P, D) * buf_w[:, None]
    return jnp.zeros((N, D), h.dtype).at[buf_tok].add(y)


def setup_inputs(seed: int = 0) -> dict:
    key = jax.random.key(seed)
    k = jax.random.split(key, 32)
    f32 = jnp.float32
    L = DEPTH

    def nrm(i, shape, fan_in):
        return jax.random.normal(k[i], shape, f32) * (fan_in ** -0.5)

    def gain(i, shape):
        return 1.0 + 0.02 * jax.random.normal(k[i], shape, f32)

    x = jax.random.normal(k[0], (BATCH, SEQ, D_MODEL), f32)
    offs = jax.random.randint(k[1], (BATCH, 1), 0, 1024, dtype=jnp.int32)
    positions = (offs + jnp.arange(SEQ, dtype=jnp.int32)[None, :]).astype(jnp.int32)
    return {
        "x": x,
        "positions": positions,
        "norm_mix": gain(2, (L, D_MODEL)),
        "w_in": nrm(3, (L, D_MODEL, D_IN), D_MODEL),
        "mla_cq_norm": gain(4, (L, Q_LORA)),
        "mla_w_uq": nrm(5, (L, Q_LORA, MLA_HEADS * MLA_QK), Q_LORA),
        "mla_ckv_norm": gain(6, (L, KV_LORA)),
        "mla_w_ukv": nrm(7, (L, KV_LORA, MLA_HEADS * (MLA_NOPE + MLA_V)), KV_LORA),
        "mla_q_norm": gain(8, (L, MLA_QK)),
        "mla_k_norm": gain(9, (L, MLA_QK)),
        "sb_q_norm": gain(10, (L, SB_DIM)),
        "sb_k_norm": gain(11, (L, SB_DIM)),
        "fox_q_norm": gain(12, (L, FOX_DIM)),
        "fox_k_norm": gain(13, (L, FOX_DIM)),
        "fox_f_bias": 3.0 + 0.5 * jax.random.normal(k[14], (L, FOX_HEADS), f32),
        "w_branch_mla": nrm(15, (L, MLA_HEADS * MLA_V, D_MODEL), MLA_HEADS * MLA_V),
        "w_branch_sb": nrm(16, (L, SB_HEADS * SB_DIM, D_MODEL), SB_HEADS * SB_DIM),
        "w_branch_fox": nrm(17, (L, FOX_HEADS * FOX_DIM, D_MODEL), FOX_HEADS * FOX_DIM),
        "w_out": nrm(18, (L, D_MODEL, D_MODEL), D_MODEL),
        "norm_ffn": gain(19, (L, D_MODEL)),
        "w_group": nrm(20, (L, D_MODEL, N_GROUPS), D_MODEL),
        "w_router": nrm(21, (L, D_MODEL, N_EXPERTS), D_MODEL),
        "w_e_gate": nrm(22, (L, N_EXPERTS, D_MODEL, D_EXPERT), D_MODEL),
        "w_e_up": nrm(23, (L, N_EXPERTS, D_MODEL, D_EXPERT), D_MODEL),
        "w_e_down": nrm(24, (L, N_EXPERTS, D_EXPERT, D_MODEL), D_EXPERT),
    }


def reference(x, positions, norm_mix, w_in, mla_cq_norm, mla_w_uq, mla_ckv_norm, mla_w_ukv,
              mla_q_norm, mla_k_norm, sb_q_norm, sb_k_norm, fox_q_norm, fox_k_norm, fox_f_bias,
              w_branch_mla, w_branch_sb, w_branch_fox, w_out, norm_ffn, w_group, w_router,
              w_e_gate, w_e_up, w_e_down):
    B, S, D = x.shape
    cos, sin = _rope_tables(positions)
    off = _segment_offsets()
    for l in range(DEPTH):
        xn = _rms(x, norm_mix[l])
        w_in_l = w_in[l]
        o_a = _mla_branch(xn, w_in_l, off, mla_cq_norm[l], mla_w_uq[l], mla_ckv_norm[l],
                          mla_w_ukv[l], mla_q_norm[l], mla_k_norm[l], cos, sin)
        o_b = _sb_branch(xn, w_in_l, off, sb_q_norm[l], sb_k_norm[l])
        o_c = _fox_branch(xn, w_in_l, off, fox_q_norm[l], fox_k_norm[l], fox_f_bias[l])
        gates = jax.nn.sigmoid(_proj(xn, w_in_l, off, "gates").astype(jnp.float32))
        gates = gates.reshape(B, S, N_BRANCH, D).astype(x.dtype)
        merged = (gates[:, :, 0] * (o_a @ w_branch_mla[l])
                  + gates[:, :, 1] * (o_b @ w_branch_sb[l])
                  + gates[:, :, 2] * (o_c @ w_branch_fox[l]))
        x = x + merged @ w_out[l]
        h = _rms(x, norm_ffn[l]).reshape(B * S, D)
        x = x + _hier_moe(h, w_group[l], w_router[l], w_e_gate[l], w_e_up[l], w_e_down[l]).reshape(B, S, D)
    return x
```

```python
from concourse.bass_utils import run_bass_kernel_spmd
import contextlib
import numpy as np
import concourse.bass as bass
import concourse.mybir as mybir

F32 = mybir.dt.float32
BF16 = mybir.dt.bfloat16
I32 = mybir.dt.int32
AF = mybir.ActivationFunctionType
ALU = mybir.AluOpType
AX = mybir.AxisListType

ENGS = ("pe", "act", "dve", "pool", "sp")


class Buf:
    def __init__(self, name, multi=False, persist=False):
        self.name = name
        self.multi = multi
        self.persist = persist
        self.writers = {}
        self.readers = {}
        self.sem = None
        self.ndma = 0


class Op:
    __slots__ = ("eng", "fn", "deps", "kind", "buf", "token", "needed", "key")

    def __init__(self, eng, fn, kind):
        self.eng = eng
        self.fn = fn
        self.kind = kind
        self.deps = []
        self.buf = None
        self.token = None
        self.needed = False
        self.key = None


class Sched:
    def __init__(self, nc, es):
        self.nc = nc
        self.es = es
        self.ops = []
        self.esem = {e: es.enter_context(nc.semaphore("sem_" + e)) for e in ENGS}
        self.ecount = {e: 0 for e in ENGS}
        self.seen = {e: {} for e in ENGS}
        self.nsem = 0
        self.last_op = {e: None for e in ENGS}
        self.dma_pending = {}
        self.pool_free = []
        self.phase_bufs = []

    def _bufsem(self, buf):
        if buf.sem is None:
            if self.pool_free and not buf.persist:
                buf.sem, buf.ndma = self.pool_free.pop()
            else:
                buf.sem = self.es.enter_context(self.nc.semaphore("bs%d_%s" % (self.nsem, buf.name)))
                self.nsem += 1
            if not buf.persist:
                self.phase_bufs.append(buf)
        return buf.sem

    def _add(self, op, reads, writes):
        deps = {}
        for b in reads:
            for w in b.writers.values():
                deps[id(w)] = w
        for b in writes:
            for r in b.readers.values():
                deps[id(r)] = r
            if not b.multi:
                for w in b.writers.values():
                    deps[id(w)] = w
        deps.pop(id(op), None)
        op.deps = list(deps.values())
        for d in op.deps:
            d.needed = True
        for b in reads:
            b.readers[op.key] = op
        for b in writes:
            if b.readers or not b.multi:
                b.writers = {}
                b.readers = {}
            b.writers[op.key] = op
        self.ops.append(op)
        self.last_op[op.eng] = op

    def op(self, eng, fn, reads=(), writes=()):
        o = Op(eng, fn, "c")
        o.key = eng
        self._add(o, reads, writes)
        return o

    def dma(self, eng, fn, reads=(), writes=()):
        o = Op(eng, fn, "d")
        b = writes[0]
        o.buf = b
        self._bufsem(b)
        o.key = "dma_" + b.name
        self._add(o, reads, writes)
        self.dma_pending[o.key] = o
        return o

    def barrier(self):
        lasts = [o for o in self.last_op.values() if o is not None]
        pend = list(self.dma_pending.values())
        for e in ENGS:
            o = Op(e, None, "c")
            o.key = e
            o.deps = [d for d in lasts + pend]
            for d in o.deps:
                d.needed = True
            self.ops.append(o)
        self.dma_pending = {}

    def emit(self):
        nc = self.nc
        for o in self.ops:
            if o.kind == "d":
                o.buf.ndma += 1
                o.token = (o.buf.sem, 16 * o.buf.ndma)
            elif o.needed and o.fn is not None:
                self.ecount[o.eng] += 1
                o.token = (self.esem[o.eng], self.ecount[o.eng])
        per = {e: [o for o in self.ops if o.eng == e] for e in ENGS}

        def run(engname, eng):
            seen = self.seen[engname]
            for o in per[engname]:
                need = {}
                stack = list(o.deps)
                while stack:
                    d = stack.pop()
                    if d.token is None:
                        stack.extend(d.deps)
                        continue
                    if d.kind == "c" and d.eng == engname and engname == "pe":
                        continue
                    sem, val = d.token
                    k = id(sem)
                    if k not in need or need[k][1] < val:
                        need[k] = (sem, val)
                for k, (sem, val) in need.items():
                    if seen.get(k, 0) >= val:
                        continue
                    eng.wait_ge(sem, val)
                    seen[k] = val
                if o.fn is None:
                    continue
                inst = o.fn(eng)
                if o.token is not None:
                    sem, val = o.token
                    inst.then_inc(sem, 16 if o.kind == "d" else 1)

        with nc.Block() as block:
            @block.tensor
            def _(t):
                run("pe", t)

            @block.scalar
            def _(s):
                run("act", s)

            @block.vector
            def _(v):
                run("dve", v)

            @block.gpsimd
            def _(g):
                run("pool", g)

            @block.sync
            def _(sy):
                run("sp", sy)
        self.ops = []
        self.last_op = {e: None for e in ENGS}
        for b in self.phase_bufs:
            self.pool_free.append((b.sem, b.ndma))
            b.sem = None
        self.phase_bufs = []


import contextlib
import numpy as np
import concourse.bass as bass
import concourse.mybir as mybir

D = 2048
KD = D // 128
H = 16
EPS = 1e-6
SEG = {}
_o = 0
for _n, _w in (("mla_cq", 512), ("mla_ckv", 512), ("mla_krope", 64), ("sb_q", 1024), ("sb_k", 1024),
               ("sb_v", 1024), ("fox_q", 1024), ("fox_k", 1024), ("fox_v", 1024), ("fox_f", 16),
               ("gates", 6144)):
    SEG[_n] = (_o, _o + _w)
    _o += _w
D_IN = _o


class Ctx:
    pass


_UID = [0]


def uname(n):
    _UID[0] += 1
    return "%s_u%d" % (n, _UID[0])


def own_blocks(nb, par):
    return [i for i in range(nb) if (i % 4 in (0, 3)) == (par == 0)]


def phase_xnT(c, sch, x_ap, gain_ap, xnT_ap, blocks, wr_buf):
    nc = c.nc
    with contextlib.ExitStack() as es:
        T = lambda name, shape, dt: es.enter_context(nc.sbuf_tensor(uname(name), shape, dt))
        xin = [T("xin%d" % i, [128, D], F32) for i in range(2)]
        sq = T("sq", [128, D], F32)
        gbc = T("gbc", [128, D], F32)
        xn = [T("xn%d" % i, [128, D], BF16) for i in range(2)]
        ss = T("ss", [128, 4], F32)
        xt = [T("xt%d" % i, [128, KD, 128], BF16) for i in range(2)]
        pst = [es.enter_context(nc.psum_tensor(uname("pst%d" % i), [128, 4, 128], BF16)) for i in range(4)]
        b_xin = [Buf("xin%d" % i) for i in range(2)]
        b_sq, b_g, b_ss = Buf("sq"), Buf("gbc"), Buf("ss")
        b_xn = [Buf("xn%d" % i) for i in range(2)]
        b_xt = [Buf("xt%d" % i) for i in range(2)]
        b_ps = [Buf("pst%d" % i) for i in range(4)]
        sch.dma("sp", lambda e: e.dma_start(out=gbc[:], in_=gain_ap.partition_broadcast(128)), writes=[b_g])
        for n, tb in enumerate(blocks):
            i = n % 2
            sch.dma("sp", lambda e, i=i, tb=tb: e.dma_start(out=xin[i][:], in_=x_ap(tb)),
                    writes=[b_xin[i]])
            sch.op("act", lambda e, i=i: e.activation(out=sq[:], in_=xin[i][:], func=AF.Square),
                   reads=[b_xin[i]], writes=[b_sq])
            sch.op("dve", lambda e: e.tensor_reduce(out=ss[:, 0:1], in_=sq[:], axis=AX.X, op=ALU.add),
                   reads=[b_sq], writes=[b_ss])
            sch.op("dve", lambda e: e.tensor_scalar(out=ss[:, 1:2], in0=ss[:, 0:1], scalar1=1.0 / D, scalar2=EPS,
                                                    op0=ALU.mult, op1=ALU.add), reads=[b_ss], writes=[b_ss])
            sch.op("act", lambda e: e.activation(out=ss[:, 3:4], in_=ss[:, 1:2], func=AF.Ln), reads=[b_ss], writes=[b_ss])
            sch.op("act", lambda e: e.activation(out=ss[:, 2:3], in_=ss[:, 3:4], func=AF.Exp, scale=-0.5),
                   reads=[b_ss], writes=[b_ss])
            sch.op("dve", lambda e, i=i: e.scalar_tensor_tensor(out=xn[i][:], in0=xin[i][:], scalar=ss[:, 2:3],
                                                                in1=gbc[:], op0=ALU.mult, op1=ALU.mult),
                   reads=[b_xin[i], b_ss, b_g], writes=[b_xn[i]])
            for q in range(4):
                pi = (n * 4 + q) % 4
                for j in range(4):
                    k = q * 4 + j
                    sch.op("pe", lambda e, i=i, k=k, pi=pi, j=j: e.transpose(
                        out=pst[pi][:, j, :], in_=xn[i][:, k * 128:(k + 1) * 128], identity=c.ident[:]),
                        reads=[b_xn[i], c.b_const], writes=[b_ps[pi]])
                eng = "act" if q % 2 else "dve"
                if eng == "act":
                    sch.op("act", lambda e, i=i, q=q, pi=pi: e.copy(out=xt[i][:, q * 4:(q + 1) * 4, :], in_=pst[pi][:]),
                           reads=[b_ps[pi]], writes=[b_xt[i]])
                else:
                    sch.op("dve", lambda e, i=i, q=q, pi=pi: e.tensor_copy(out=xt[i][:, q * 4:(q + 1) * 4, :], in_=pst[pi][:]),
                           reads=[b_ps[pi]], writes=[b_xt[i]])
            sch.dma("sp", lambda e, i=i, tb=tb: e.dma_start(out=xnT_ap[:, :, tb * 128:(tb + 1) * 128], in_=xt[i][:]),
                    reads=[b_xt[i]], writes=[wr_buf])
        sch.barrier()
        sch.emit()


def setup_consts(c, sch, es):
    nc = c.nc
    c.ident = es.enter_context(nc.sbuf_tensor("ident", [128, 128], BF16))
    c.identf = es.enter_context(nc.sbuf_tensor("identf", [128, 128], F32))
    c.b_const = Buf("const")
    sch.op("pool", lambda e: e.memset(c.identf[:], 1.0), writes=[c.b_const])
    sch.op("pool", lambda e: e.affine_select(out=c.identf[:], in_=c.identf[:], pattern=[[-1, 128]],
                                             compare_op=ALU.is_equal, fill=0.0, base=0, channel_multiplier=1),
           reads=[c.b_const], writes=[c.b_const])
    sch.op("pool", lambda e: e.tensor_copy(out=c.ident[:], in_=c.identf[:]), reads=[c.b_const], writes=[c.b_const])


def rstd_ops(sch, ss_ap, tmp_ap, out_ap, d, b_small):
    sch.op("dve", lambda e: e.tensor_scalar(out=tmp_ap, in0=ss_ap, scalar1=1.0 / d, scalar2=EPS,
                                            op0=ALU.mult, op1=ALU.add), reads=[b_small], writes=[b_small])
    sch.op("act", lambda e: e.activation(out=tmp_ap, in_=tmp_ap, func=AF.Ln), reads=[b_small], writes=[b_small])
    sch.op("act", lambda e: e.activation(out=out_ap, in_=tmp_ap, func=AF.Exp, scale=-0.5),
           reads=[b_small], writes=[b_small])


def phase_linear(c, sch, inT_ap, KC, W_ap, chunks, blocks, post, extra_setup=None, TT=8):
    nc = c.nc
    with contextlib.ExitStack() as es:
        T = lambda name, shape, dt: es.enter_context(nc.sbuf_tensor(uname(name), shape, dt))
        inT = T("inT", [128, KC, TT * 128], BF16)
        b_in = Buf("inT")
        wch = [T("wch%d" % i, [128, KC, 512], BF16) for i in range(2)]
        b_w = [Buf("wch%d" % i) for i in range(2)]
        ps = [es.enter_context(nc.psum_tensor(uname("lps%d" % i), [128, 512], F32)) for i in range(3)]
        b_ps = [Buf("lps%d" % i) for i in range(3)]
        env = Ctx()
        env.T, env.es, env.nc = T, es, nc
        if extra_setup is not None:
            extra_setup(env)
        Wv = W_ap.rearrange("(k p) n -> p k n", p=128)
        cnt = 0
        wcnt = 0
        for t0 in range(0, len(blocks), TT):
            tbs = blocks[t0:t0 + TT]
            for j, tb in enumerate(tbs):
                sch.dma("sp", lambda e, j=j, tb=tb: e.dma_start(out=inT[:, :, j * 128:(j + 1) * 128],
                                                                 in_=inT_ap[:, :, tb * 128:(tb + 1) * 128]),
                        writes=[b_in])
            for ci, (lo, n, tag) in enumerate(chunks):
                wi = wcnt % 2
                wcnt += 1
                sch.dma("pool", lambda e, wi=wi, lo=lo, n=n: e.dma_start(out=wch[wi][:, :, 0:n], in_=Wv[:, :, lo:lo + n]),
                        writes=[b_w[wi]])
                for j, tb in enumerate(tbs):
                    pi = cnt % 3
                    cnt += 1
                    for k in range(KC):
                        sch.op("pe", lambda e, pi=pi, k=k, j=j, wi=wi, n=n: e.matmul(
                            ps[pi][:, 0:n], inT[:, k, j * 128:(j + 1) * 128], wch[wi][:, k, 0:n],
                            start=(k == 0), stop=(k == KC - 1)),
                            reads=[b_in, b_w[wi]], writes=[b_ps[pi]])
                    post(env, tag, ci, tb, t0 + j, ps[pi], b_ps[pi])
        sch.barrier()
        sch.emit()


def transpose_out(c, sch, env, src_ap, ncols, dst_fn, b_src, key="tr"):
    nc = env.nc
    if not hasattr(env, "trs"):
        env.trs = [env.es.enter_context(nc.psum_tensor(uname("trp%d" % i), [128, 128], BF16)) for i in range(2)]
        env.b_trs = [Buf("trp%d" % i) for i in range(2)]
        env.tro = [env.T("tro%d" % i, [128, 128], BF16) for i in range(4)]
        env.b_tro = [Buf("tro%d" % i) for i in range(4)]
        env.trn = 0
    n = env.trn
    env.trn += 1
    p, o = n % 2, n % 4
    sch.op("pe", lambda e: e.transpose(out=env.trs[p][0:ncols, :], in_=src_ap, identity=c.ident[:]),
           reads=[b_src, c.b_const], writes=[env.b_trs[p]])
    if n % 2:
        sch.op("act", lambda e: e.copy(out=env.tro[o][0:ncols, :], in_=env.trs[p][0:ncols, :]),
               reads=[env.b_trs[p]], writes=[env.b_tro[o]])
    else:
        sch.op("dve", lambda e: e.tensor_copy(out=env.tro[o][0:ncols, :], in_=env.trs[p][0:ncols, :]),
               reads=[env.b_trs[p]], writes=[env.b_tro[o]])
    dst_ap, b_dst = dst_fn()
    sch.dma("sp", lambda e: e.dma_start(out=dst_ap, in_=env.tro[o][0:ncols, :]), reads=[env.b_tro[o]], writes=[b_dst])


def headnorm(c, sch, env, ps, b_ps, nh, d, gain_t, b_gain, out_ap, b_out):
    if not hasattr(env, "hn_sq"):
        env.hn_sq = env.T("hn_sq", [128, 512], F32)
        env.hn_t = env.T("hn_t", [128, 512], F32)
        env.hn_s = env.T("hn_s", [128, 64], F32)
        env.b_hn = Buf("hn")
        env.b_hns = Buf("hns")
    n = nh * d
    sq, t1, s = env.hn_sq, env.hn_t, env.hn_s
    sch.op("act", lambda e: e.activation(out=sq[:, 0:n], in_=ps[:, 0:n], func=AF.Square), reads=[b_ps], writes=[env.b_hn])
    sch.op("dve", lambda e: e.tensor_reduce(out=s[:, 0:nh], in_=sq[:, 0:n].rearrange("p (h d) -> p h d", d=d),
                                            axis=AX.X, op=ALU.add), reads=[env.b_hn], writes=[env.b_hns])
    rstd_ops(sch, s[:, 0:nh], s[:, 16:16 + nh], s[:, 32:32 + nh], d, env.b_hns)
    sch.op("dve", lambda e: e.tensor_tensor(out=t1[:, 0:n].rearrange("p (h d) -> p h d", d=d),
                                            in0=ps[:, 0:n].rearrange("p (h d) -> p h d", d=d),
                                            in1=s[:, 32:32 + nh].unsqueeze(2).to_broadcast([128, nh, d]), op=ALU.mult),
           reads=[b_ps, env.b_hns, env.b_hn], writes=[env.b_hn])
    sch.op("dve", lambda e: e.tensor_tensor(out=out_ap.rearrange("p (h d) -> p h d", d=d),
                                            in0=t1[:, 0:n].rearrange("p (h d) -> p h d", d=d),
                                            in1=gain_t[:, 0:d].unsqueeze(1).to_broadcast([128, nh, d]), op=ALU.mult),
           reads=[env.b_hn, b_gain], writes=[b_out])


def setup_rope(c, sch, es, pos_ap, NB, suffix=""):
    nc = c.nc
    import math
    cos_t = es.enter_context(nc.sbuf_tensor(uname("cos"), [128, NB, 32], F32))
    sin_t = es.enter_context(nc.sbuf_tensor(uname("sin"), [128, NB, 32], F32))
    setattr(c, "cos" + suffix, cos_t)
    setattr(c, "sin" + suffix, sin_t)
    if not hasattr(c, "b_rope"):
        c.b_rope = Buf("rope")
    with contextlib.ExitStack() as es2:
        T = lambda name, shape, dt: es2.enter_context(nc.sbuf_tensor(uname(name), shape, dt))
        posi = T("posi", [128, NB], I32)
        posf = T("posf", [128, NB], F32)
        invf = T("invf", [128, 32], F32)
        ang = T("ang", [128, NB, 32], F32)
        r = T("rr", [128, NB, 32], F32)
        b = Buf("ropetmp")
        sch.dma("sp", lambda e: e.dma_start(out=posi[:], in_=pos_ap.rearrange("(b p) -> p b", p=128), allow_slow_non_contiguous=True), writes=[b])
        sch.op("dve", lambda e: e.tensor_copy(out=posf[:], in_=posi[:]), reads=[b], writes=[b])
        for i in range(32):
            v = float(np.float32(10000.0) ** np.float32(-(2.0 * i) / 64.0))
            sch.op("pool", lambda e, i=i, v=v: e.memset(invf[:, i:i + 1], v), writes=[b])
        sch.op("dve", lambda e: e.tensor_tensor(out=ang[:], in0=posf[:].unsqueeze(2).to_broadcast([128, NB, 32]),
                                                in1=invf[:].unsqueeze(1).to_broadcast([128, NB, 32]), op=ALU.mult),
               reads=[b], writes=[b])
        qi = T("qi", [128, NB, 32], I32)
        m = T("rm", [128, NB, 32], F32)
        TWO_PI = 2 * math.pi
        for dst, sh in ((sin_t, 0.0), (cos_t, math.pi / 2)):
            sch.op("dve", lambda e, sh=sh: e.tensor_scalar(out=m[:], in0=ang[:], scalar1=sh, scalar2=None, op0=ALU.add),
                   reads=[b], writes=[b])
            sch.op("dve", lambda e: e.tensor_scalar(out=r[:], in0=m[:], scalar1=1.0 / TWO_PI, scalar2=None, op0=ALU.mult),
                   reads=[b], writes=[b])
            sch.op("dve", lambda e: e.tensor_copy(out=qi[:], in_=r[:]), reads=[b], writes=[b])
            sch.op("dve", lambda e: e.tensor_copy(out=r[:], in_=qi[:]), reads=[b], writes=[b])
            sch.op("dve", lambda e: e.scalar_tensor_tensor(out=r[:], in0=r[:], scalar=-TWO_PI, in1=m[:],
                                                           op0=ALU.mult, op1=ALU.add), reads=[b], writes=[b])
            sch.op("dve", lambda e: e.tensor_scalar(out=m[:], in0=r[:], scalar1=math.pi, scalar2=None, op0=ALU.is_gt),
                   reads=[b], writes=[b])
            sch.op("dve", lambda e: e.scalar_tensor_tensor(out=r[:], in0=m[:], scalar=-TWO_PI, in1=r[:],
                                                           op0=ALU.mult, op1=ALU.add), reads=[b], writes=[b])
            sch.op("dve", lambda e: e.tensor_scalar(out=m[:], in0=r[:], scalar1=-math.pi, scalar2=None, op0=ALU.is_lt),
                   reads=[b], writes=[b])
            sch.op("dve", lambda e: e.scalar_tensor_tensor(out=r[:], in0=m[:], scalar=TWO_PI, in1=r[:],
                                                           op0=ALU.mult, op1=ALU.add), reads=[b], writes=[b])
            sch.op("act", lambda e, dst=dst: e.activation(out=dst[:], in_=r[:], func=AF.Sin), reads=[b], writes=[c.b_rope])
        sch.barrier()
        sch.emit()


def rope_ops(c, sch, env, x_ap, out_ap, nh, tb, b_x, b_out, own=False):
    if not hasattr(env, "rp_a"):
        env.rp_a = env.T("rp_a", [128, 16, 32], F32)
        env.rp_b = env.T("rp_b", [128, 16, 32], F32)
        env.b_rp = Buf("rp")
    a, b2 = env.rp_a[:, 0:nh, :], env.rp_b[:, 0:nh, :]
    cos_t, sin_t = (c.cos_o, c.sin_o) if (own and not c.full) else (c.cos, c.sin)
    cs = cos_t[:, tb, :].unsqueeze(1).to_broadcast([128, nh, 32])
    sn = sin_t[:, tb, :].unsqueeze(1).to_broadcast([128, nh, 32])
    x1, x2 = x_ap[:, :, 0:32], x_ap[:, :, 32:64]
    rd = [b_x, c.b_rope, env.b_rp]
    sch.op("dve", lambda e: e.tensor_tensor(out=a, in0=x1, in1=cs, op=ALU.mult), reads=rd, writes=[env.b_rp])
    sch.op("dve", lambda e: e.tensor_tensor(out=b2, in0=x2, in1=sn, op=ALU.mult), reads=rd, writes=[env.b_rp])
    sch.op("dve", lambda e: e.tensor_tensor(out=out_ap[:, :, 0:32], in0=a, in1=b2, op=ALU.subtract),
           reads=[env.b_rp], writes=[b_out])
    sch.op("dve", lambda e: e.tensor_tensor(out=a, in0=x2, in1=cs, op=ALU.mult), reads=rd + [b_out], writes=[env.b_rp])
    sch.op("dve", lambda e: e.tensor_tensor(out=b2, in0=x1, in1=sn, op=ALU.mult), reads=rd, writes=[env.b_rp])
    sch.op("dve", lambda e: e.tensor_tensor(out=out_ap[:, :, 32:64], in0=a, in1=b2, op=ALU.add),
           reads=[env.b_rp], writes=[b_out])


def load_bc(sch, env, name, ap, n):
    t = env.T(name, [128, n], F32)
    b = Buf(name)
    sch.dma("sp", lambda e: e.dma_start(out=t[:], in_=ap.partition_broadcast(128)), writes=[b])
    return t, b


def stage(env, kind):
    if not hasattr(env, "stg"):
        env.stg = {"b": [env.T("stb%d" % i, [128, 512], BF16) for i in range(4)],
                   "f": [env.T("stf%d" % i, [128, 512], F32) for i in range(3)]}
        env.bstg = {"b": [Buf("stb%d" % i) for i in range(4)], "f": [Buf("stf%d" % i) for i in range(3)]}
        env.nstg = {"b": 0, "f": 0}
    i = env.nstg[kind] % len(env.stg[kind])
    env.nstg[kind] += 1
    return env.stg[kind][i], env.bstg[kind][i]


def phase_inproj(c, sch, L):
    W = c.w
    S, So = c.S, c.So
    sc = c.sc
    w_in = W["w_in"][L]

    def chunks_of(names):
        out = []
        for nm in names:
            lo, hi = SEG[nm]
            for q, s0 in enumerate(range(lo, hi, 512)):
                out.append((s0, min(512, hi - s0), (nm, q)))
        return out

    def setup_q(env):
        env.g_cq = load_bc(sch, env, "g_cq", W["mla_cq_norm"][L], 512)
        env.g_sbq = load_bc(sch, env, "g_sbq", W["sb_q_norm"][L], 64)
        env.g_fxq = load_bc(sch, env, "g_fxq", W["fox_q_norm"][L], 64)

    def post_q(env, tag, ci, tb, nl, ps, b_ps):
        nm, q = tag
        tok = slice(nl * 128, (nl + 1) * 128)
        if nm == "mla_cq":
            ob, b_ob = stage(env, "b")
            headnorm(c, sch, env, ps, b_ps, 1, 512, env.g_cq[0], env.g_cq[1], ob[:, 0:512], b_ob)
            for k in range(4):
                transpose_out(c, sch, env, ob[:, k * 128:(k + 1) * 128], 128,
                              lambda k=k: (sc["cqT"][:, k, tok], c.b_sc["cqT"]), b_ob)
        elif nm in ("sb_q", "fox_q"):
            g = env.g_sbq if nm == "sb_q" else env.g_fxq
            dst = "sbQT" if nm == "sb_q" else "foxQT"
            ob, b_ob = stage(env, "b")
            headnorm(c, sch, env, ps, b_ps, 8, 64, g[0], g[1], ob[:, 0:512], b_ob)
            for k in range(4):
                transpose_out(c, sch, env, ob[:, k * 128:(k + 1) * 128], 128,
                              lambda k=k: (sc[dst][q * 4 + k, :, tok], c.b_sc[dst]), b_ob)
        elif nm == "gates":
            ob, b_ob = stage(env, "b")
            sch.op("act", lambda e: e.activation(out=ob[:], in_=ps[:], func=AF.Sigmoid), reads=[b_ps], writes=[b_ob])
            sch.dma("sp", lambda e: e.dma_start(out=sc["gates"][tok, q * 512:(q + 1) * 512], in_=ob[:]),
                    reads=[b_ob], writes=[c.b_sc["gates"]])

    phase_linear(c, sch, sc["xnT"] if c.full else sc["xnTo"], KD, w_in, chunks_of(["mla_cq", "sb_q", "fox_q", "gates"]),
                 list(range(c.NBo)), post_q, setup_q)

    def setup_k(env):
        env.g_ckv = load_bc(sch, env, "g_ckv", W["mla_ckv_norm"][L], 512)
        env.g_sbk = load_bc(sch, env, "g_sbk", W["sb_k_norm"][L], 64)
        env.g_fxk = load_bc(sch, env, "g_fxk", W["fox_k_norm"][L], 64)
        env.fb = load_bc(sch, env, "fbias", W["fox_f_bias"][L], 16)

    def post_k(env, tag, ci, tb, nl, ps, b_ps):
        nm, q = tag
        tok = slice(tb * 128, (tb + 1) * 128)
        if nm == "mla_ckv":
            ob, b_ob = stage(env, "b")
            headnorm(c, sch, env, ps, b_ps, 1, 512, env.g_ckv[0], env.g_ckv[1], ob[:, 0:512], b_ob)
            for k in range(4):
                transpose_out(c, sch, env, ob[:, k * 128:(k + 1) * 128], 128,
                              lambda k=k: (sc["ckvT"][:, k, tok], c.b_sc["ckvT"]), b_ob)
        elif nm in ("sb_k", "fox_k"):
            g = env.g_sbk if nm == "sb_k" else env.g_fxk
            dst = "sbKT" if nm == "sb_k" else "foxKT"
            ob, b_ob = stage(env, "b")
            headnorm(c, sch, env, ps, b_ps, 8, 64, g[0], g[1], ob[:, 0:512], b_ob)
            for k in range(4):
                transpose_out(c, sch, env, ob[:, k * 128:(k + 1) * 128], 128,
                              lambda k=k: (sc[dst][q * 4 + k, :, tok], c.b_sc[dst]), b_ob)
        elif nm in ("sb_v", "fox_v"):
            dst = "sbV" if nm == "sb_v" else "foxV"
            ob, b_ob = stage(env, "b")
            sch.op("act", lambda e: e.copy(out=ob[:], in_=ps[:]), reads=[b_ps], writes=[b_ob])
            sch.dma("sp", lambda e: e.dma_start(out=sc[dst][tok, q * 512:(q + 1) * 512], in_=ob[:]),
                    reads=[b_ob], writes=[c.b_sc[dst]])
        elif nm == "mla_krope":
            of, b_of = stage(env, "f")
            sch.op("act", lambda e: e.copy(out=of[:, 0:64], in_=ps[:, 0:64]), reads=[b_ps], writes=[b_of])
            sch.dma("sp", lambda e: e.dma_start(out=sc["kr"][tok, :], in_=of[:, 0:64]), reads=[b_of], writes=[c.b_sc["kr"]])
        elif nm == "fox_f":
            of, b_of = stage(env, "f")
            sch.op("dve", lambda e: e.tensor_tensor(out=of[:, 0:16], in0=ps[:, 0:16], in1=env.fb[0][:, 0:16], op=ALU.add),
                   reads=[b_ps, env.fb[1]], writes=[b_of])
            sch.op("act", lambda e: e.activation(out=of[:, 16:32], in_=of[:, 0:16], func=AF.Exp, scale=-1.0),
                   reads=[b_of], writes=[b_of])
            sch.op("act", lambda e: e.activation(out=of[:, 32:48], in_=of[:, 16:32], func=AF.Ln, bias=1.0),
                   reads=[b_of], writes=[b_of])
            sch.op("dve", lambda e: e.tensor_scalar(out=of[:, 48:64], in0=of[:, 32:48], scalar1=-1.0, scalar2=None,
                                                    op0=ALU.mult), reads=[b_of], writes=[b_of])
            sch.dma("sp", lambda e: e.dma_start(out=sc["lf"][tok, :], in_=of[:, 48:64]), reads=[b_of], writes=[c.b_sc["lf"]])

    phase_linear(c, sch, sc["xnT"], KD, w_in,
                 chunks_of(["mla_ckv", "mla_krope", "sb_k", "sb_v", "fox_k", "fox_v", "fox_f"]),
                 list(range(c.NB)), post_k, setup_k)


WSHAPES = {
    "norm_mix": (D,), "w_in": (D, D_IN), "mla_cq_norm": (512,), "mla_w_uq": (512, 3072), "mla_ckv_norm": (512,),
    "mla_w_ukv": (512, 4096), "mla_q_norm": (192,), "mla_k_norm": (192,), "sb_q_norm": (64,), "sb_k_norm": (64,),
    "fox_q_norm": (64,), "fox_k_norm": (64,), "fox_f_bias": (16,), "w_branch_mla": (2048, 2048),
    "w_branch_sb": (1024, 2048), "w_branch_fox": (1024, 2048), "w_out": (2048, 2048), "norm_ffn": (D,),
    "w_group": (D, 8), "w_router": (D, 64), "w_e_gate": (64, D, 512), "w_e_up": (64, D, 512), "w_e_down": (64, 512, D),
}


def scratch_shapes(S):
    So = S
    return {
        "xnT": ([128, KD, S], BF16), "xnTo": ([128, KD, So], BF16), "sbQT": ([8, 128, So], BF16), "sbKT": ([8, 128, S], BF16), "sbV": ([S, 1024], BF16),
        "foxQT": ([8, 128, So], BF16), "foxKT": ([8, 128, S], BF16), "foxV": ([S, 1024], BF16),
        "foxQa": ([64, So], BF16), "foxKa": ([64, S], BF16), "lf": ([S, 16], F32),
        "cqT": ([128, 4, So], BF16), "ckvT": ([128, 4, S], BF16), "kr": ([S, 64], F32),
        "mlaQTn": ([16, 128, So], BF16), "mlaQTr": ([16, 64, So], BF16), "mlaKTn": ([16, 128, S], BF16),
        "mlaKTr": ([16, 64, S], BF16), "mlaV": ([S, 2048], BF16), "gates": ([So, 6144], BF16),
        "OT_mla": ([2048, So], BF16), "OT_sb": ([1024, So], BF16), "OT_fox": ([1024, So], BF16),
        "xmid": ([So, D], F32),
    }


def make_scratch(c, dump=()):
    c.sc, c.b_sc = {}, {}
    for nm, (shape, dt) in scratch_shapes(c.S).items():
        kind = "ExternalOutput" if nm in dump else "Internal"
        c.sc[nm] = c.nc.dram_tensor("sc_" + nm, shape, dt, kind=kind).ap()
        c.b_sc[nm] = Buf("sc_" + nm, multi=True)


def setup_masks(c, sch, es):
    nc = c.nc
    mk = lambda n, dt: es.enter_context(nc.sbuf_tensor(uname(n), [128, 128], dt))
    c.mask_incl, c.mask_strict = mk("mincl", BF16), mk("mstrict", BF16)
    c.ntril8, c.nones8, c.ones_bf = mk("ntril8", BF16), mk("nones8", BF16), mk("onesbf", BF16)
    c.triu_f, c.ones_f = mk("triuf", F32), mk("onesf", F32)
    tmp = mk("mtmp", F32)
    b = c.b_const

    def sel(dst, val, pattern, cm, op):
        sch.op("pool", lambda e: e.memset(tmp[:], val), reads=[b], writes=[b])
        sch.op("pool", lambda e: e.affine_select(out=tmp[:], in_=tmp[:], pattern=pattern, compare_op=op, fill=0.0,
                                                 base=0, channel_multiplier=cm), reads=[b], writes=[b])
        sch.op("pool", lambda e: e.tensor_copy(out=dst[:], in_=tmp[:]), reads=[b], writes=[b])
    sel(c.mask_incl, 1.0, [[1, 128]], -1, ALU.is_ge)
    sel(c.mask_strict, 1.0, [[1, 128]], -1, ALU.is_gt)
    sel(c.ntril8, -8.0, [[-1, 128]], 1, ALU.is_ge)
    sel(c.triu_f, 1.0, [[1, 128]], -1, ALU.is_ge)
    sch.op("pool", lambda e: e.memset(c.nones8[:], -8.0), reads=[b], writes=[b])
    sch.op("pool", lambda e: e.memset(c.ones_bf[:], 1.0), reads=[b], writes=[b])
    sch.op("pool", lambda e: e.memset(c.ones_f[:], 1.0), reads=[b], writes=[b])


def phase_attn(c, sch, kind):
    nc = c.nc
    sc, S, So, NB = c.sc, c.S, c.So, c.NB
    dv = 128 if kind == "mla" else 64
    scale = (192 if kind == "mla" else 64) ** -0.5
    with contextlib.ExitStack() as es:
        T = lambda name, shape, dt: es.enter_context(nc.sbuf_tensor(uname(name), shape, dt))
        P = lambda name, shape, dt: es.enter_context(nc.psum_tensor(uname(name), shape, dt))
        nparts = 2 if kind == "mla" else 1
        KT = [[T("kt", [128, S], BF16) for _ in range(nparts)] for _ in range(2)]
        QT = [[T("qt", [128, So], BF16) for _ in range(nparts)] for _ in range(2)]
        Vt = [T("vt", [128, NB, dv], BF16) for _ in range(2)]
        b_kt = [[Buf("kt%d%d" % (i, j)) for j in range(nparts)] for i in range(2)]
        b_qt = [[Buf("qt%d%d" % (i, j)) for j in range(nparts)] for i in range(2)]
        b_kta = [Buf("kta%d" % i) for i in range(2)]
        b_qta = [Buf("qta%d" % i) for i in range(2)]
        b_v = [Buf("vt%d" % i) for i in range(2)]
        ps_s = [P("pss", [128, 4, 128], F32) for _ in range(2)]
        b_ps_s = [Buf("pss%d" % i) for i in range(2)]
        ps_o = [P("pso", [128, 128], F32) for _ in range(2)]
        b_ps_o = [Buf("pso%d" % i) for i in range(2)]
        pt = [T("pt", [128, 4, 128], BF16) for _ in range(3)]
        b_pt = [Buf("pt%d" % i) for i in range(3)]
        osb = [T("osb", [128, 128], BF16) for _ in range(2)]
        b_osb = [Buf("osb%d" % i) for i in range(2)]
        if kind == "sb":
            ps_b = [P("psb", [128, 4, 128], F32) for _ in range(2)]
            b_ps_b = [Buf("psb%d" % i) for i in range(2)]
            et = [T("et", [128, 4, 128], F32) for _ in range(2)]
            b_et = [Buf("et%d" % i) for i in range(2)]
            lb = [T("lb", [128, 4, 128], BF16) for _ in range(2)]
            b_lb = [Buf("lb%d" % i) for i in range(2)]
            rs = [T("rs", [128, 128], BF16) for _ in range(2)]
            b_rs = [Buf("rs%d" % i) for i in range(2)]
        else:
            ps_d = [P("psd", [128, 128], F32) for _ in range(2)]
            b_ps_d = [Buf("psd%d" % i) for i in range(2)]
            rd = [T("rd", [128, 128], F32) for _ in range(2)]
            b_rd = [Buf("rd%d" % i) for i in range(2)]
        if kind == "mla":
            rows = [128, 64]
        elif kind == "fox":
            rows = [68]
        else:
            rows = [64]
        cnt = {"g": 0, "o": 0, "pt": 0, "rs": 0}
        def do_head(h):
            hb = h % 2
            if kind == "mla":
                ksrc = [sc["mlaKTn"][h], sc["mlaKTr"][h]]
                qsrc = [sc["mlaQTn"][h][:, 0:So], sc["mlaQTr"][h][:, 0:So]]
                knames, qnames = ["mlaKTn", "mlaKTr"], ["mlaQTn", "mlaQTr"]
                vsrc, vname = sc["mlaV"][:, h * 128:(h + 1) * 128], "mlaV"
            else:
                pre = "fox" if kind == "fox" else "sb"
                ksrc = [sc[pre + "KT"][h // 2, (h % 2) * 64:(h % 2) * 64 + 64, :]]
                qsrc = [sc[pre + "QT"][h // 2, (h % 2) * 64:(h % 2) * 64 + 64, 0:So]]
                knames, qnames = [pre + "KT"], [pre + "QT"]
                vsrc, vname = sc[pre + "V"][:, h * 64:(h + 1) * 64], pre + "V"
            for p in range(nparts):
                r = min(rows[p], 128 if kind == "mla" else 64)
                sch.dma("sp", lambda e, p=p, r=r: e.dma_start(out=KT[hb][p][0:r, :], in_=ksrc[p]),
                        reads=[c.b_sc[knames[p]]], writes=[b_kt[hb][p]])
                sch.dma("sp", lambda e, p=p, r=r: e.dma_start(out=QT[hb][p][0:r, :], in_=qsrc[p]),
                        reads=[c.b_sc[qnames[p]]], writes=[b_qt[hb][p]])
            kr_, qr_ = [b_kt[hb][p] for p in range(nparts)], [b_qt[hb][p] for p in range(nparts)]
            if kind == "fox":
                sch.dma("sp", lambda e: e.dma_start(out=KT[hb][0][64:68, :], in_=sc["foxKa"][h * 4:(h + 1) * 4, :]),
                        reads=[c.b_sc["foxKa"]], writes=[b_kta[hb]])
                sch.dma("sp", lambda e: e.dma_start(out=QT[hb][0][64:68, :], in_=sc["foxQa"][h * 4:(h + 1) * 4, 0:So]),
                        reads=[c.b_sc["foxQa"]], writes=[b_qta[hb]])
                kr_, qr_ = kr_ + [b_kta[hb]], qr_ + [b_qta[hb]]
            sch.dma("pool", lambda e: e.dma_start(out=Vt[hb][:], in_=vsrc.rearrange("(b p) d -> p b d", p=128)),
                    reads=[c.b_sc[vname]], writes=[b_v[hb]])

            def qk(psum_ap, kb, j, first=True, last=True):
                for p in range(nparts):
                    r = rows[p]
                    sch.op("pe", lambda e, p=p, r=r: e.matmul(
                        psum_ap, KT[hb][p][0:r, kb * 128:(kb + 1) * 128], QT[hb][p][0:r, j * 128:(j + 1) * 128],
                        start=(first and p == 0), stop=(last and p == nparts - 1)), reads=kr_ + qr_, writes=[])

            def do_q(j):
                jp = j % 2
                if c.full:
                    i = j
                    mlist = [(i, c.mask_incl[:], c.mask_strict[:])]
                else:
                    i = 2 * j + 1
                    mlist = [(i - 1 + wch, c.maskt[:, 0 * 4 + jp * 2 + wch, :], c.maskt[:, 1 * 4 + jp * 2 + wch, :])
                             for wch in range(2)]
                oi = cnt["o"] % 2
                cnt["o"] += 1
                groups = [list(range(g0, min(g0 + 4, i + 1))) for g0 in range(0, i + 1, 4)]
                if kind == "sb":
                    groups = [list(reversed(g)) for g in reversed(groups)]
                st = {"nproc": 0}

                def do_grp(grp):
                    gi = cnt["g"] % 2
                    cnt["g"] += 1
                    pi = cnt["pt"] % 3
                    cnt["pt"] += 1
                    n = len(grp)
                    for sl, kb in enumerate(grp):
                        for p in range(nparts):
                            r = rows[p]
                            sch.op("pe", lambda e, p=p, r=r, sl=sl, kb=kb: e.matmul(
                                ps_s[gi][0:128, sl, :], KT[hb][p][0:r, kb * 128:(kb + 1) * 128],
                                QT[hb][p][0:r, j * 128:(j + 1) * 128], start=(p == 0), stop=(p == nparts - 1)),
                                reads=kr_ + qr_, writes=[b_ps_s[gi]])
                    if kind != "sb":
                        sch.op("act", lambda e, n=n, gi=gi, pi=pi: e.activation(out=pt[pi][:, 0:n, :], in_=ps_s[gi][:, 0:n, :],
                                                                                func=AF.Exp, scale=scale),
                               reads=[b_ps_s[gi]], writes=[b_pt[pi]])
                        if i in grp:
                            for (mkb, m_incl, m_strict) in mlist:
                                sl = grp.index(mkb)
                                sch.op("dve", lambda e, sl=sl, pi=pi, m_incl=m_incl: e.tensor_tensor(
                                    out=pt[pi][:, sl, :], in0=pt[pi][:, sl, :], in1=m_incl, op=ALU.mult),
                                    reads=[b_pt[pi], c.b_const], writes=[b_pt[pi]])
                        for sl, kb in enumerate(grp):
                            sch.op("pe", lambda e, sl=sl, kb=kb, pi=pi, oi=oi: e.matmul(
                                ps_o[oi][0:dv, :], Vt[hb][:, kb, :], pt[pi][:, sl, :], start=(kb == 0), stop=(kb == i)),
                                reads=[b_v[hb], b_pt[pi]], writes=[b_ps_o[oi]])
                            sch.op("pe", lambda e, sl=sl, kb=kb, pi=pi, oi=oi: e.matmul(
                                ps_d[oi][0:dv, :], c.ones_bf[:, 0:dv], pt[pi][:, sl, :], start=(kb == 0), stop=(kb == i)),
                                reads=[c.b_const, b_pt[pi]], writes=[b_ps_d[oi]])
                    else:
                        ei = gi
                        sch.op("act", lambda e, n=n, gi=gi: e.activation(out=et[gi][:, 0:n, :], in_=ps_s[gi][:, 0:n, :],
                                                                         func=AF.Exp, scale=scale),
                               reads=[b_ps_s[gi]], writes=[b_et[gi]])
                        sch.op("act", lambda e, n=n, gi=gi: e.activation(out=lb[gi][:, 0:n, :], in_=et[gi][:, 0:n, :],
                                                                         func=AF.Ln, bias=1.0),
                               reads=[b_et[gi]], writes=[b_lb[gi]])
                        if i in grp:
                            for (mkb, m_incl, m_strict) in mlist:
                                sl = grp.index(mkb)
                                sch.op("dve", lambda e, sl=sl, gi=gi, m_strict=m_strict: e.tensor_tensor(
                                    out=lb[gi][:, sl, :], in0=lb[gi][:, sl, :], in1=m_strict, op=ALU.mult),
                                    reads=[b_lb[gi], c.b_const], writes=[b_lb[gi]])
                        for sl, kb in enumerate(grp):
                            first = (st["nproc"] == 0)
                            ri = cnt["rs"] % 2
                            for p in range(nparts):
                                r = rows[p]
                                sch.op("pe", lambda e, p=p, r=r, sl=sl, kb=kb: e.matmul(
                                    ps_b[gi][0:128, sl, :], KT[hb][p][0:r, kb * 128:(kb + 1) * 128],
                                    QT[hb][p][0:r, j * 128:(j + 1) * 128], start=True, stop=False),
                                    reads=kr_ + qr_, writes=[b_ps_b[gi]])
                            sch.op("pe", lambda e, sl=sl, first=first: e.matmul(ps_b[gi][0:128, sl, :], c.ntril8[:], lb[gi][:, sl, :],
                                                                   start=False, stop=first),
                                   reads=[c.b_const, b_lb[gi]], writes=[b_ps_b[gi]])
                            if not first:
                                sch.op("pe", lambda e, sl=sl, ri=ri: e.matmul(ps_b[gi][0:128, sl, :], c.nones8[:], rs[ri][:],
                                                                              start=False, stop=True),
                                       reads=[c.b_const, b_rs[ri]], writes=[b_ps_b[gi]])
                            if first:
                                sch.op("dve", lambda e, sl=sl, gi=gi, ri=ri: e.tensor_copy(out=rs[1 - ri][:], in_=lb[gi][:, sl, :]),
                                       reads=[b_lb[gi]], writes=[b_rs[1 - ri]])
                            else:
                                sch.op("dve", lambda e, sl=sl, gi=gi, ri=ri: e.tensor_tensor(
                                    out=rs[1 - ri][:], in0=rs[ri][:], in1=lb[gi][:, sl, :], op=ALU.add),
                                    reads=[b_lb[gi], b_rs[ri]], writes=[b_rs[1 - ri]])
                            cnt["rs"] += 1
                            st["nproc"] += 1
                        sch.op("act", lambda e, n=n, gi=gi, pi=pi: e.activation(out=pt[pi][:, 0:n, :], in_=ps_b[gi][:, 0:n, :],
                                                                                func=AF.Exp, scale=scale),
                               reads=[b_ps_b[gi]], writes=[b_pt[pi]])
                        if i in grp:
                            for (mkb, m_incl, m_strict) in mlist:
                                sl = grp.index(mkb)
                                sch.op("dve", lambda e, sl=sl, pi=pi, m_strict=m_strict: e.tensor_tensor(
                                    out=pt[pi][:, sl, :], in0=pt[pi][:, sl, :], in1=m_strict, op=ALU.mult),
                                    reads=[b_pt[pi], c.b_const], writes=[b_pt[pi]])
                        for sl, kb in enumerate(grp):
                            sch.op("pe", lambda e, sl=sl, kb=kb, pi=pi, oi=oi: e.matmul(
                                ps_o[oi][0:dv, :], Vt[hb][:, kb, :], pt[pi][:, sl, :], start=(kb == i), stop=(kb == 0)),
                                reads=[b_v[hb], b_pt[pi]], writes=[b_ps_o[oi]])
                for grp in groups:
                    do_grp(grp)
                if kind != "sb":
                    sch.op("dve", lambda e, oi=oi: e.reciprocal(out=rd[oi][0:dv, :], in_=ps_d[oi][0:dv, :]),
                           reads=[b_ps_d[oi]], writes=[b_rd[oi]])
                    sch.op("dve", lambda e, oi=oi: e.tensor_tensor(out=osb[oi][0:dv, :], in0=ps_o[oi][0:dv, :],
                                                                   in1=rd[oi][0:dv, :], op=ALU.mult),
                           reads=[b_ps_o[oi], b_rd[oi]], writes=[b_osb[oi]])
                else:
                    sch.op("act", lambda e, oi=oi: e.copy(out=osb[oi][0:dv, :], in_=ps_o[oi][0:dv, :]),
                           reads=[b_ps_o[oi]], writes=[b_osb[oi]])
                dst = "OT_" + kind
                sch.dma("sp", lambda e, oi=oi, j=j: e.dma_start(out=sc[dst][h * dv:(h + 1) * dv, j * 128:(j + 1) * 128],
                                                                in_=osb[oi][0:dv, :]),
                        reads=[b_osb[oi]], writes=[c.b_sc[dst]])
            for j in range(c.NBo):
                do_q(j)

        for h in range(H):
            do_head(h)
        sch.barrier()
        sch.emit()


def phase_mla_up(c, sch, L):
    W, sc = c.w, c.sc

    def setup_q(env):
        env.g_q = load_bc(sch, env, "g_mq", W["mla_q_norm"][L], 192)
        env.qf = [env.T("mqf%d" % i, [128, 384], F32) for i in range(2)]
        env.b_qf = [Buf("mqf%d" % i) for i in range(2)]
        env.qn = 0

    def post_q(env, tag, ci, tb, nl, ps, b_ps):
        tok = slice(nl * 128, (nl + 1) * 128)
        qi = env.qn % 2
        env.qn += 1
        qf, b_qf = env.qf[qi], env.b_qf[qi]
        headnorm(c, sch, env, ps, b_ps, 2, 192, env.g_q[0], env.g_q[1], qf[:, 0:384], b_qf)
        ob, b_ob = stage(env, "b")
        v3 = qf[:, 0:384].rearrange("p (h d) -> p h d", d=192)
        o3 = ob[:, 0:384].rearrange("p (h d) -> p h d", d=192)
        sch.op("act", lambda e: e.copy(out=o3[:, :, 0:128], in_=v3[:, :, 0:128]), reads=[b_qf], writes=[b_ob])
        rope_ops(c, sch, env, v3[:, :, 128:192], o3[:, :, 128:192], 2, nl, b_qf, b_ob, own=True)
        for hh in range(2):
            h = ci * 2 + hh
            transpose_out(c, sch, env, ob[:, hh * 192:hh * 192 + 128], 128,
                          lambda h=h: (sc["mlaQTn"][h, :, tok], c.b_sc["mlaQTn"]), b_ob)
            transpose_out(c, sch, env, ob[:, hh * 192 + 128:hh * 192 + 192], 64,
                          lambda h=h: (sc["mlaQTr"][h, :, tok], c.b_sc["mlaQTr"]), b_ob)

    chunks = [(i * 384, 384, "mq") for i in range(8)]
    phase_linear(c, sch, sc["cqT"], 4, W["mla_w_uq"][L], chunks, list(range(c.NBo)), post_q, setup_q)

    def setup_k(env):
        env.g_k = load_bc(sch, env, "g_mk", W["mla_k_norm"][L], 192)
        env.krt = [env.T("krt%d" % i, [128, 64], F32) for i in range(2)]
        env.b_krt = [Buf("krt%d" % i) for i in range(2)]
        env.krr = [env.T("krr%d" % i, [128, 64], F32) for i in range(2)]
        env.sm = env.T("mks", [128, 16], F32)
        env.b_sm = Buf("mks")
        env.kf = env.T("mkf", [128, 256], F32)
        env.b_kf = Buf("mkf")
        env.kn = 0
        env.last_tb = None

    def post_k(env, tag, ci, tb, nl, ps, b_ps):
        tok = slice(tb * 128, (tb + 1) * 128)
        ki = env.kn % 2
        env.kn += 1
        krt, krr, b_krt = env.krt[ki], env.krr[ki], env.b_krt[ki]
        sm, b_sm = env.sm, env.b_sm
        sch.dma("sp", lambda e: e.dma_start(out=krt[:], in_=sc["kr"][tok, :]), reads=[c.b_sc["kr"]], writes=[b_krt])
        sch.op("dve", lambda e: e.tensor_tensor(out=krr[:], in0=krt[:], in1=krt[:], op=ALU.mult), reads=[b_krt], writes=[b_krt])
        sch.op("dve", lambda e: e.tensor_reduce(out=sm[:, 0:1], in_=krr[:], axis=AX.X, op=ALU.add), reads=[b_krt], writes=[b_sm])
        sch.op("dve", lambda e: e.tensor_tensor(out=krt[:], in0=krt[:], in1=env.g_k[0][:, 128:192], op=ALU.mult),
               reads=[b_krt, env.g_k[1]], writes=[b_krt])
        rope_ops(c, sch, env, krt[:].rearrange("p (h d) -> p h d", h=1), krr[:].rearrange("p (h d) -> p h d", h=1), 1, tb,
                 b_krt, b_krt)
        kf, b_kf = env.kf, env.b_kf
        p3 = ps[:, 0:512].rearrange("p (h d) -> p h d", d=256)
        k3 = kf[:, 0:256].rearrange("p (h d) -> p h d", d=128)
        sch.op("act", lambda e: e.activation(out=k3, in_=p3[:, :, 0:128], func=AF.Square), reads=[b_ps], writes=[b_kf])
        sch.op("dve", lambda e: e.tensor_reduce(out=sm[:, 1:3], in_=k3, axis=AX.X, op=ALU.add), reads=[b_kf], writes=[b_sm])
        sch.op("dve", lambda e: e.tensor_tensor(out=sm[:, 1:3], in0=sm[:, 1:3], in1=sm[:, 0:1].to_broadcast([128, 2]), op=ALU.add),
               reads=[b_sm], writes=[b_sm])
        rstd_ops(sch, sm[:, 1:3], sm[:, 4:6], sm[:, 8:10], 192, b_sm)
        sch.op("dve", lambda e: e.tensor_tensor(out=k3, in0=p3[:, :, 0:128], in1=sm[:, 8:10].unsqueeze(2).to_broadcast([128, 2, 128]),
                                                op=ALU.mult), reads=[b_ps, b_sm, b_kf], writes=[b_kf])
        ob, b_ob = stage(env, "b")
        o3 = ob[:, 0:512].rearrange("p (h d) -> p h d", d=256)
        sch.op("dve", lambda e: e.tensor_tensor(out=o3[:, :, 0:128], in0=k3,
                                                in1=env.g_k[0][:, 0:128].unsqueeze(1).to_broadcast([128, 2, 128]), op=ALU.mult),
               reads=[b_kf, env.g_k[1]], writes=[b_ob])
        sch.op("act", lambda e: e.copy(out=o3[:, :, 128:256], in_=p3[:, :, 128:256]), reads=[b_ps], writes=[b_ob])
        ob2, b_ob2 = stage(env, "b")
        sch.op("dve", lambda e: e.tensor_tensor(out=ob2[:, 0:128].rearrange("p (h d) -> p h d", d=64),
                                                in0=krr[:].unsqueeze(1).to_broadcast([128, 2, 64]),
                                                in1=sm[:, 8:10].unsqueeze(2).to_broadcast([128, 2, 64]), op=ALU.mult),
               reads=[b_krt, b_sm], writes=[b_ob2])
        for hh in range(2):
            h = ci * 2 + hh
            transpose_out(c, sch, env, ob[:, hh * 256:hh * 256 + 128], 128,
                          lambda h=h: (sc["mlaKTn"][h, :, tok], c.b_sc["mlaKTn"]), b_ob)
            transpose_out(c, sch, env, ob2[:, hh * 64:(hh + 1) * 64], 64,
                          lambda h=h: (sc["mlaKTr"][h, :, tok], c.b_sc["mlaKTr"]), b_ob2)
            sch.dma("sp", lambda e, h=h, hh=hh: e.dma_start(out=sc["mlaV"][tok, h * 128:(h + 1) * 128],
                                                            in_=ob[:, hh * 256 + 128:hh * 256 + 256]),
                    reads=[b_ob], writes=[c.b_sc["mlaV"]])

    chunks = [(i * 512, 512, "mkv") for i in range(8)]
    phase_linear(c, sch, sc["ckvT"], 4, W["mla_w_ukv"][L], chunks, list(range(c.NB)), post_k, setup_k)


def phase_foxcum(c, sch):
    nc, sc, NB = c.nc, c.sc, c.NB
    with contextlib.ExitStack() as es:
        T = lambda name, shape, dt: es.enter_context(nc.sbuf_tensor(uname(name), shape, dt))
        lf = T("lf", [128, NB, 16], F32)
        cum = T("cum", [128, NB, 16], F32)
        pre = T("pre", [128, NB, 16], F32)
        hi = T("hi", [128, NB, 16], BF16)
        hif = T("hif", [128, NB, 16], F32)
        lo = T("lo", [128, NB, 16], BF16)
        aug = [T("aug%d" % i, [128, 16, 4], BF16) for i in range(2)]
        b_aug = [Buf("aug%d" % i) for i in range(2)]
        ps1 = es.enter_context(nc.psum_tensor(uname("cps1"), [128, NB * 16], F32))
        ps2 = es.enter_context(nc.psum_tensor(uname("cps2"), [128, NB * 16], F32))
        b = Buf("cumall")
        seltmp = T("seltmp", [128, 16], BF16)
        b_st = Buf("seltmp")
        b_p1, b_p2 = Buf("cps1"), Buf("cps2")
        env = Ctx()
        env.T, env.es, env.nc = T, es, nc
        sch.dma("sp", lambda e: e.dma_start(out=lf[:], in_=sc["lf"].rearrange("(b p) h -> p b h", p=128)),
                reads=[c.b_sc["lf"]], writes=[b])
        lf2 = lf[:].rearrange("p b h -> p (b h)")
        sch.op("pe", lambda e: e.matmul(ps1[:], c.triu_f[:], lf2, start=True, stop=True), reads=[b, c.b_const], writes=[b_p1])
        sch.op("pe", lambda e: e.matmul(ps2[:], c.ones_f[:], lf2, start=True, stop=True), reads=[b, c.b_const], writes=[b_p2])
        sch.op("dve", lambda e: e.memset(pre[:, 0, :], 0.0), writes=[b])
        p2 = ps2[:].rearrange("p (b h) -> p b h", h=16)
        for bb in range(1, NB):
            sch.op("dve", lambda e, bb=bb: e.tensor_tensor(out=pre[:, bb, :], in0=pre[:, bb - 1, :], in1=p2[:, bb - 1, :], op=ALU.add),
                   reads=[b, b_p2], writes=[b])
        sch.op("dve", lambda e: e.tensor_tensor(out=cum[:], in0=pre[:], in1=ps1[:].rearrange("p (b h) -> p b h", h=16), op=ALU.add),
               reads=[b, b_p1], writes=[b])
        sch.op("dve", lambda e: e.tensor_scalar(out=cum[:], in0=cum[:], scalar1=8.0, scalar2=None, op0=ALU.mult), reads=[b], writes=[b])
        sch.op("dve", lambda e: e.tensor_copy(out=hi[:], in_=cum[:]), reads=[b], writes=[b])
        sch.op("dve", lambda e: e.tensor_copy(out=hif[:], in_=hi[:]), reads=[b], writes=[b])
        sch.op("dve", lambda e: e.tensor_tensor(out=hif[:], in0=cum[:], in1=hif[:], op=ALU.subtract), reads=[b], writes=[b])
        sch.op("dve", lambda e: e.tensor_copy(out=lo[:], in_=hif[:]), reads=[b], writes=[b])
        n = 0
        for side in ("k", "q"):
            blks = list(range(NB)) if side == "k" else list(range(c.NBo))
            for nl, tb in enumerate(blks):
                ai = n % 2
                n += 1
                a, b_a = aug[ai], b_aug[ai]
                if side == "q" and c.full:
                    sch.op("dve", lambda e, a=a, tb=tb: e.tensor_copy(out=a[:, :, 0], in_=hi[:, tb, :]), reads=[b], writes=[b_a])
                    sch.op("dve", lambda e, a=a, tb=tb: e.tensor_copy(out=a[:, :, 1], in_=lo[:, tb, :]), reads=[b], writes=[b_a])
                    sch.op("dve", lambda e, a=a: e.memset(a[:, :, 2:4], 1.0), writes=[b_a])
                    dst, tok = "foxQa", slice(nl * 128, (nl + 1) * 128)
                elif side == "q":
                    sa, sb_ = c.selt[:, 2 * (nl % 2):2 * (nl % 2) + 1], c.selt[:, 2 * (nl % 2) + 1:2 * (nl % 2) + 2]
                    for col, src in ((0, hi), (1, lo)):
                        sch.op("dve", lambda e, src=src, nl=nl, sa=sa: e.tensor_scalar(out=seltmp[:], in0=src[:, 2 * nl, :], scalar1=sa,
                                                                                      scalar2=None, op0=ALU.mult),
                               reads=[b, c.b_const], writes=[b_st])
                        sch.op("dve", lambda e, src=src, nl=nl, sb_=sb_, a=a, col=col: e.scalar_tensor_tensor(
                            out=a[:, :, col], in0=src[:, 2 * nl + 1, :], scalar=sb_, in1=seltmp[:], op0=ALU.mult, op1=ALU.add),
                            reads=[b, b_st, c.b_const], writes=[b_a])
                    sch.op("dve", lambda e, a=a: e.memset(a[:, :, 2:4], 1.0), writes=[b_a])
                    dst, tok = "foxQa", slice(nl * 128, (nl + 1) * 128)
                else:
                    sch.op("dve", lambda e, a=a: e.memset(a[:, :, 0:2], 1.0), writes=[b_a])
                    sch.op("dve", lambda e, a=a, tb=tb: e.tensor_scalar(out=a[:, :, 2], in0=hi[:, tb, :], scalar1=-1.0, scalar2=None,
                                                                        op0=ALU.mult), reads=[b], writes=[b_a])
                    sch.op("dve", lambda e, a=a, tb=tb: e.tensor_scalar(out=a[:, :, 3], in0=lo[:, tb, :], scalar1=-1.0, scalar2=None,
                                                                        op0=ALU.mult), reads=[b], writes=[b_a])
                    dst, tok = "foxKa", slice(tb * 128, (tb + 1) * 128)
                transpose_out(c, sch, env, a[:].rearrange("p h j -> p (h j)"), 64,
                              lambda dst=dst, tok=tok: (sc[dst][:, tok], c.b_sc[dst]), b_a)
        sch.barrier()
        sch.emit()


def phase_merge_out(c, sch, L, x_ap):
    nc, sc, W = c.nc, c.sc, c.w
    NBo = c.NBo
    TT = 4
    with contextlib.ExitStack() as es:
        T = lambda name, shape, dt: es.enter_context(nc.sbuf_tensor(uname(name), shape, dt))
        P = lambda name, shape, dt: es.enter_context(nc.psum_tensor(uname(name), shape, dt))
        brs = [("mla", 16), ("sb", 8), ("fox", 8)]
        ot = [T("ot" + n, [128, kc, TT * 128], BF16) for n, kc in brs]
        b_ot = [Buf("ot" + n) for n, kc in brs]
        wb = [[T("wb" + n, [128, kc, 512], BF16) for n, kc in brs] for _ in range(2)]
        b_wb = [[Buf("wb%d%s" % (i, n)) for n, kc in brs] for i in range(2)]
        mT = T("mT", [128, 16, TT * 128], BF16)
        b_mT = Buf("mT")
        wo = [T("wo", [128, 16, 512], BF16) for _ in range(2)]
        b_wo = [Buf("wo%d" % i) for i in range(2)]
        gt = [T("gt", [128, 3, 512], BF16) for _ in range(2)]
        b_gt = [Buf("gt%d" % i) for i in range(2)]
        t0 = [T("mt0", [128, 512], F32) for _ in range(2)]
        t1 = [T("mt1", [128, 512], F32) for _ in range(2)]
        b_t0 = [Buf("mt0%d" % i) for i in range(2)]
        b_t1 = [Buf("mt1%d" % i) for i in range(2)]
        mb = [T("mb", [128, 512], BF16) for _ in range(2)]
        b_mb = [Buf("mb%d" % i) for i in range(2)]
        xo = [T("xo", [128, 512], F32) for _ in range(2)]
        b_xo = [Buf("xo%d" % i) for i in range(2)]
        psb = [P("psbr", [128, 512], F32) for _ in range(3)]
        b_psb = [Buf("psbr%d" % i) for i in range(3)]
        pst = [P("pstr", [128, 4, 128], BF16) for _ in range(2)]
        b_pst = [Buf("pstr%d" % i) for i in range(2)]
        pso = [P("psout", [128, 512], F32) for _ in range(2)]
        b_pso = [Buf("psout%d" % i) for i in range(2)]
        wsrc = [W["w_branch_mla"][L], W["w_branch_sb"][L], W["w_branch_fox"][L]]
        wv = [w.rearrange("(k p) n -> p k n", p=128) for w in wsrc]
        wov = W["w_out"][L].rearrange("(k p) n -> p k n", p=128)
        ctr = {"w": 0, "g": 0, "t": 0, "o": 0, "x": 0}

        def do_tile(tl0):
            nt = min(TT, NBo - tl0)
            tsl = slice(tl0 * 128, (tl0 + nt) * 128)
            for bi, (n, kc) in enumerate(brs):
                sch.dma("sp", lambda e, bi=bi, n=n: e.dma_start(
                    out=ot[bi][:, :, 0:nt * 128], in_=sc["OT_" + n][:, tsl].rearrange("(k p) t -> p k t", p=128)),
                    reads=[c.b_sc["OT_" + n]], writes=[b_ot[bi]])
            for cc in range(4):
                wi = ctr["w"] % 2
                ctr["w"] += 1
                for bi in range(3):
                    sch.dma("pool", lambda e, bi=bi, wi=wi, cc=cc: e.dma_start(out=wb[wi][bi][:], in_=wv[bi][:, :, cc * 512:(cc + 1) * 512]),
                            writes=[b_wb[wi][bi]])
                for j in range(nt):
                    gi = ctr["g"] % 2
                    ctr["g"] += 1
                    tok = slice((tl0 + j) * 128, (tl0 + j + 1) * 128)
                    sch.dma("sp", lambda e, gi=gi, tok=tok, cc=cc: e.dma_start(
                        out=gt[gi][:], in_=sc["gates"][tok, :].rearrange("t (b n) -> t b n", b=3)[:, :, cc * 512:(cc + 1) * 512]),
                        reads=[c.b_sc["gates"]], writes=[b_gt[gi]])
                    for bi, (n, kc) in enumerate(brs):
                        for k in range(kc):
                            sch.op("pe", lambda e, bi=bi, k=k, kc=kc, j=j, wi=wi: e.matmul(
                                psb[bi][:], ot[bi][:, k, j * 128:(j + 1) * 128], wb[wi][bi][:, k, :],
                                start=(k == 0), stop=(k == kc - 1)), reads=[b_ot[bi], b_wb[wi][bi]], writes=[b_psb[bi]])
                    sch.op("dve", lambda e, gi=gi: e.tensor_tensor(out=t0[gi][:], in0=psb[0][:], in1=gt[gi][:, 0, :], op=ALU.mult),
                           reads=[b_psb[0], b_gt[gi]], writes=[b_t0[gi]])
                    sch.op("dve", lambda e, gi=gi: e.tensor_tensor(out=t1[gi][:], in0=psb[1][:], in1=gt[gi][:, 1, :], op=ALU.mult),
                           reads=[b_psb[1], b_gt[gi]], writes=[b_t1[gi]])
                    sch.op("pool", lambda e, gi=gi: e.tensor_tensor(out=t0[gi][:], in0=t0[gi][:], in1=t1[gi][:], op=ALU.add),
                           reads=[b_t0[gi], b_t1[gi]], writes=[b_t0[gi]])
                    sch.op("dve", lambda e, gi=gi: e.tensor_tensor(out=t1[gi][:], in0=psb[2][:], in1=gt[gi][:, 2, :], op=ALU.mult),
                           reads=[b_psb[2], b_gt[gi]], writes=[b_t1[gi]])
                    sch.op("pool", lambda e, gi=gi: e.tensor_tensor(out=mb[gi][:], in0=t0[gi][:], in1=t1[gi][:], op=ALU.add),
                           reads=[b_t0[gi], b_t1[gi]], writes=[b_mb[gi]])
                    ti = ctr["t"] % 2
                    ctr["t"] += 1
                    for q in range(4):
                        sch.op("pe", lambda e, gi=gi, q=q, ti=ti: e.transpose(out=pst[ti][:, q, :], in_=mb[gi][:, q * 128:(q + 1) * 128],
                                                                              identity=c.ident[:]),
                               reads=[b_mb[gi], c.b_const], writes=[b_pst[ti]])
                    sch.op("act", lambda e, ti=ti, cc=cc, j=j: e.copy(out=mT[:, cc * 4:(cc + 1) * 4, j * 128:(j + 1) * 128], in_=pst[ti][:]),
                           reads=[b_pst[ti]], writes=[b_mT])
            for oc in range(4):
                wi = ctr["o"] % 2
                ctr["o"] += 1
                sch.dma("pool", lambda e, wi=wi, oc=oc: e.dma_start(out=wo[wi][:], in_=wov[:, :, oc * 512:(oc + 1) * 512]),
                        writes=[b_wo[wi]])
                for j in range(nt):
                    xi = ctr["x"] % 2
                    ctr["x"] += 1
                    gb = tl0 + j
                    tok = slice((tl0 + j) * 128, (tl0 + j + 1) * 128)
                    sch.dma("sp", lambda e, xi=xi, gb=gb, oc=oc: e.dma_start(out=xo[xi][:], in_=x_ap[gb * 128:(gb + 1) * 128, oc * 512:(oc + 1) * 512]),
                            writes=[b_xo[xi]])
                    for k in range(16):
                        sch.op("pe", lambda e, xi=xi, k=k, j=j, wi=wi: e.matmul(
                            pso[xi][:], mT[:, k, j * 128:(j + 1) * 128], wo[wi][:, k, :], start=(k == 0), stop=(k == 15)),
                            reads=[b_mT, b_wo[wi]], writes=[b_pso[xi]])
                    sch.op("dve", lambda e, xi=xi: e.tensor_tensor(out=xo[xi][:], in0=pso[xi][:], in1=xo[xi][:], op=ALU.add),
                           reads=[b_pso[xi], b_xo[xi]], writes=[b_xo[xi]])
                    sch.dma("sp", lambda e, xi=xi, tok=tok, oc=oc: e.dma_start(out=sc["xmid"][tok, oc * 512:(oc + 1) * 512], in_=xo[xi][:]),
                            reads=[b_xo[xi]], writes=[c.b_sc["xmid"]])
        for tl0 in range(0, NBo, TT):
            do_tile(tl0)
        sch.barrier()
        sch.emit()


def phase_moe(c, sch, L, out_ap, b_out):
    nc, sc, W = c.nc, c.sc, c.w
    NBo = c.NBo
    TT = 4
    with contextlib.ExitStack() as es:
        T = lambda name, shape, dt: es.enter_context(nc.sbuf_tensor(uname(name), shape, dt))
        P = lambda name, shape, dt: es.enter_context(nc.psum_tensor(uname(name), shape, dt))
        yacc = T("yacc", [128, TT, D], F32)
        b_y = [Buf("yacc%d" % i) for i in range(TT)]
        sq = T("msq", [128, D], F32)
        b_sq = Buf("msq")
        hf = T("mhf", [128, D], F32)
        b_hf = Buf("mhf")
        gbc = T("mgbc", [128, D], F32)
        b_g = Buf("mgbc")
        hT = T("mhT", [128, KD, TT * 128], BF16)
        b_hT = Buf("mhT")
        hhi = T("mhhi", [128, D], BF16)
        hlo = T("mhlo", [128, D], BF16)
        hloT = T("mhloT", [128, KD, 128], BF16)
        b_hhi, b_hlo, b_hloT = Buf("mhhi"), Buf("mhlo"), Buf("mhloT")
        w72 = T("mw72", [128, KD, 72], F32)
        whi = T("mwhi", [128, KD, 72], BF16)
        wlo = T("mwlo", [128, KD, 72], BF16)
        wr = T("mwr", [128, KD, 64], F32)
        wgp = T("mwgp", [128, KD, 8], F32)
        b_wr = Buf("mwr")
        b_wgp = Buf("mwgp")
        sm = T("msm", [128, 512], F32)
        b_sm = Buf("msm")
        coef = T("mcoef", [128, TT, 64], F32)
        b_coef = Buf("mcoef")
        wg = [T("mwg", [128, KD, 512], BF16) for _ in range(1)]
        wu = [T("mwu", [128, KD, 512], BF16) for _ in range(1)]
        wd = [T("mwd", [128, 4, D], BF16) for _ in range(1)]
        b_wg = [Buf("mwg%d" % i) for i in range(2)]
        b_wu = [Buf("mwu%d" % i) for i in range(2)]
        b_wd = [Buf("mwd%d" % i) for i in range(2)]
        aT = [T("maT", [128, 4, TT * 128], BF16) for _ in range(2)]
        b_aT = [Buf("maT%d" % i) for i in range(2)]
        sg = [T("msg", [128, TT * 128], F32) for _ in range(2)]
        b_sg = [Buf("msg%d" % i) for i in range(2)]
        pt = [P("mpt", [128, 4, 128], BF16) for _ in range(2)]
        b_pt = [Buf("mpt%d" % i) for i in range(2)]
        pr = P("mpr", [128, 72], F32)
        b_pr = Buf("mpr")
        pg = [P("mpg", [128, TT * 128], F32) for _ in range(2)]
        pu = [P("mpu", [128, TT * 128], F32) for _ in range(2)]
        b_pg = [Buf("mpg%d" % i) for i in range(2)]
        b_pu = [Buf("mpu%d" % i) for i in range(2)]
        pd = [P("mpd", [128, 512], F32) for _ in range(1)]
        b_pd = [Buf("mpd%d" % i) for i in range(1)]
        ctr = {"t": 0, "w": 0, "m": 0, "d": 0}
        sch.dma("sp", lambda e: e.dma_start(out=gbc[:], in_=W["norm_ffn"][L].partition_broadcast(128)), writes=[b_g])
        import os
        if int(os.environ.get("ROUTE_STAGE", 9)) >= 1:
            sch.dma("pool", lambda e: e.dma_start(out=wgp[:], in_=W["w_group"][L].rearrange("(k p) n -> p k n", p=128)), writes=[b_wgp])
        if int(os.environ.get("ROUTE_STAGE", 9)) >= 1:
            sch.dma("pool", lambda e: e.dma_start(out=wr[:], in_=W["w_router"][L].rearrange("(k p) n -> p k n", p=128)), writes=[b_wr])
        sch.op("dve", lambda e: e.tensor_copy(out=w72[:, :, 0:8], in_=wgp[:]), reads=[b_wgp], writes=[b_wr])
        sch.op("dve", lambda e: e.tensor_copy(out=w72[:, :, 8:72], in_=wr[:]), reads=[b_wr], writes=[b_wr])
        sch.op("dve", lambda e: e.tensor_copy(out=whi[:], in_=w72[:]), reads=[b_wr], writes=[b_wr])
        sch.op("dve", lambda e: e.tensor_tensor(out=wlo[:], in0=w72[:], in1=whi[:], op=ALU.subtract), reads=[b_wr], writes=[b_wr])
        S_ = lambda a, b: sm[:, a:b]

        def route(j, tl):
            tok = slice(tl * 128, (tl + 1) * 128)
            sch.dma("sp", lambda e: e.dma_start(out=yacc[:, j, :], in_=sc["xmid"][tok, :]), reads=[c.b_sc["xmid"]], writes=[b_y[j]])
            sch.op("act", lambda e: e.activation(out=sq[:], in_=yacc[:, j, :], func=AF.Square), reads=[b_y[j]], writes=[b_sq])
            sch.op("dve", lambda e: e.tensor_reduce(out=S_(0, 1), in_=sq[:], axis=AX.X, op=ALU.add), reads=[b_sq], writes=[b_sm])
            rstd_ops(sch, S_(0, 1), S_(1, 2), S_(2, 3), D, b_sm)
            sch.op("dve", lambda e: e.scalar_tensor_tensor(out=hf[:], in0=yacc[:, j, :], scalar=S_(2, 3), in1=gbc[:],
                                                           op0=ALU.mult, op1=ALU.mult), reads=[b_y[j], b_sm, b_g], writes=[b_hf])
            import os
            stage = int(os.environ.get("ROUTE_STAGE", 9))
            if stage < 2:
                return
            sch.op("dve", lambda e: e.tensor_copy(out=hhi[:], in_=hf[:]), reads=[b_hf], writes=[b_hhi])
            sch.op("dve", lambda e: e.tensor_tensor(out=hlo[:], in0=hf[:], in1=hhi[:], op=ALU.subtract),
                   reads=[b_hf, b_hhi], writes=[b_hlo])
            for src, b_src, which in ((hhi, b_hhi, 0), (hlo, b_hlo, 1)):
                for q in range(4):
                    ti = ctr["t"] % 2
                    ctr["t"] += 1
                    for k4 in range(4):
                        k = q * 4 + k4
                        sch.op("pe", lambda e, k=k, k4=k4, ti=ti, src=src: e.transpose(
                            out=pt[ti][:, k4, :], in_=src[:, k * 128:(k + 1) * 128], identity=c.ident[:]),
                            reads=[b_src, c.b_const], writes=[b_pt[ti]])
                    if which == 0:
                        sch.op("act", lambda e, q=q, ti=ti: e.copy(out=hT[:, q * 4:(q + 1) * 4, j * 128:(j + 1) * 128], in_=pt[ti][:]),
                               reads=[b_pt[ti]], writes=[b_hT])
                    else:
                        sch.op("dve", lambda e, q=q, ti=ti: e.tensor_copy(out=hloT[:, q * 4:(q + 1) * 4, :], in_=pt[ti][:]),
                               reads=[b_pt[ti]], writes=[b_hloT])
            if stage < 3:
                return
            combos = [(0, whi), (0, wlo), (1, whi)]
            for ci_, (which, wt) in enumerate(combos):
                for k in range(KD):
                    lhs = hT[:, k, j * 128:(j + 1) * 128] if which == 0 else hloT[:, k, :]
                    sch.op("pe", lambda e, k=k, lhs=lhs, wt=wt, ci_=ci_: e.matmul(
                        pr[:], lhs, wt[:, k, :], start=(ci_ == 0 and k == 0), stop=(ci_ == 2 and k == KD - 1)),
                        reads=[b_hT, b_hloT, b_wr], writes=[b_pr])
            nmax = int(os.environ.get("ROUTE_NOPS", 999))
            cnt_ = {"n": 0}

            def V(fn, extra=()):
                cnt_["n"] += 1
                if cnt_["n"] <= nmax:
                    sch.op("dve", fn, reads=[b_sm] + list(extra), writes=[b_sm])

            def A(fn):
                cnt_["n"] += 1
                if cnt_["n"] <= nmax:
                    sch.op("act", fn, reads=[b_sm], writes=[b_sm])
            lg, lgE = S_(8, 80), S_(16, 80)
            if stage < 4:
                return
            V(lambda e: e.tensor_copy(out=lg, in_=pr[:]), [b_pr])
            V(lambda e: e.tensor_reduce(out=S_(3, 4), in_=S_(8, 16), axis=AX.X, op=ALU.max))
            V(lambda e: e.tensor_scalar(out=S_(80, 88), in0=S_(8, 16), scalar1=S_(3, 4), scalar2=None, op0=ALU.is_equal))
            V(lambda e: e.tensor_scalar(out=S_(4, 5), in0=S_(3, 4), scalar1=-1.0, scalar2=None, op0=ALU.mult))
            A(lambda e: e.activation(out=S_(88, 96), in_=S_(8, 16), func=AF.Exp, bias=S_(4, 5), scale=1.0))
            V(lambda e: e.tensor_reduce(out=S_(5, 6), in_=S_(88, 96), axis=AX.X, op=ALU.add))
            V(lambda e: e.reciprocal(out=S_(5, 6), in_=S_(5, 6)))
            V(lambda e: e.tensor_tensor(out=S_(96, 160).rearrange("p (g j) -> p g j", j=8),
                                        in0=lgE.rearrange("p (g j) -> p g j", j=8),
                                        in1=S_(80, 88).unsqueeze(2).to_broadcast([128, 8, 8]), op=ALU.mult))
            V(lambda e: e.tensor_reduce(out=S_(160, 168), in_=S_(96, 160).rearrange("p (g j) -> p j g", j=8), axis=AX.X, op=ALU.add))
            V(lambda e: e.tensor_reduce(out=S_(6, 7), in_=S_(160, 168), axis=AX.X, op=ALU.max))
            V(lambda e: e.tensor_scalar(out=S_(168, 176), in0=S_(160, 168), scalar1=S_(6, 7), scalar2=None, op0=ALU.is_equal))
            V(lambda e: e.scalar_tensor_tensor(out=S_(176, 184), in0=S_(168, 176), scalar=-1e30, in1=S_(160, 168),
                                               op0=ALU.mult, op1=ALU.add))
            V(lambda e: e.tensor_reduce(out=S_(7, 8), in_=S_(176, 184), axis=AX.X, op=ALU.max))
            V(lambda e: e.tensor_scalar(out=S_(184, 192), in0=S_(176, 184), scalar1=S_(7, 8), scalar2=None, op0=ALU.is_equal))
            V(lambda e: e.tensor_tensor(out=S_(192, 193), in0=S_(7, 8), in1=S_(6, 7), op=ALU.subtract))
            A(lambda e: e.activation(out=S_(193, 194), in_=S_(192, 193), func=AF.Exp))
            V(lambda e: e.tensor_scalar(out=S_(194, 195), in0=S_(193, 194), scalar1=1.0, scalar2=None, op0=ALU.add))
            V(lambda e: e.reciprocal(out=S_(194, 195), in_=S_(194, 195)))
            V(lambda e: e.tensor_tensor(out=S_(195, 196), in0=S_(193, 194), in1=S_(194, 195), op=ALU.mult))
            V(lambda e: e.tensor_tensor(out=S_(194, 195), in0=S_(194, 195), in1=S_(5, 6), op=ALU.mult))
            V(lambda e: e.tensor_tensor(out=S_(195, 196), in0=S_(195, 196), in1=S_(5, 6), op=ALU.mult))
            V(lambda e: e.tensor_scalar(out=S_(200, 208), in0=S_(168, 176), scalar1=S_(194, 195), scalar2=None, op0=ALU.mult))
            V(lambda e: e.scalar_tensor_tensor(out=S_(200, 208), in0=S_(184, 192), scalar=S_(195, 196), in1=S_(200, 208),
                                               op0=ALU.mult, op1=ALU.add))
            V(lambda e: e.tensor_copy(out=S_(208, 272).rearrange("p (g j) -> p g j", j=8),
                                      in_=S_(200, 208).unsqueeze(1).to_broadcast([128, 8, 8])))
            sch.op("dve", lambda e: e.tensor_tensor(out=coef[:, j, :].rearrange("p (g j) -> p g j", j=8),
                                                    in0=S_(208, 272).rearrange("p (g j) -> p g j", j=8),
                                                    in1=S_(80, 88).unsqueeze(2).to_broadcast([128, 8, 8]), op=ALU.mult),
                   reads=[b_sm], writes=[b_coef])

        def expert(e_, nt):
            wi = 0
            N = nt * 128
            sch.dma("pool", lambda e: e.dma_start(out=wg[wi][:], in_=W["w_e_gate"][L][e_].rearrange("(k p) n -> p k n", p=128)),
                    writes=[b_wg[wi]])
            sch.dma("pool", lambda e: e.dma_start(out=wu[wi][:], in_=W["w_e_up"][L][e_].rearrange("(k p) n -> p k n", p=128)),
                    writes=[b_wu[wi]])
            sch.dma("pool", lambda e: e.dma_start(out=wd[wi][:], in_=W["w_e_down"][L][e_].rearrange("(k p) n -> p k n", p=128)),
                    writes=[b_wd[wi]])
            ai = ctr["w"] % 2
            ctr["w"] += 1
            for m in range(4):
                mi = ctr["m"] % 2
                ctr["m"] += 1
                for k in range(KD):
                    sch.op("pe", lambda e, k=k, m=m, mi=mi: e.matmul(pg[mi][:, 0:N], wg[wi][:, k, m * 128:(m + 1) * 128], hT[:, k, 0:N],
                                                                     start=(k == 0), stop=(k == KD - 1)),
                           reads=[b_wg[wi], b_hT], writes=[b_pg[mi]])
                for k in range(KD):
                    sch.op("pe", lambda e, k=k, m=m, mi=mi: e.matmul(pu[mi][:, 0:N], wu[wi][:, k, m * 128:(m + 1) * 128], hT[:, k, 0:N],
                                                                     start=(k == 0), stop=(k == KD - 1)),
                           reads=[b_wu[wi], b_hT], writes=[b_pu[mi]])
                sch.op("act", lambda e, mi=mi: e.activation(out=sg[mi][:, 0:N], in_=pg[mi][:, 0:N], func=AF.Silu),
                       reads=[b_pg[mi]], writes=[b_sg[mi]])
                sch.op("dve", lambda e, mi=mi, m=m: e.tensor_tensor(out=aT[ai][:, m, 0:N], in0=pu[mi][:, 0:N], in1=sg[mi][:, 0:N], op=ALU.mult),
                       reads=[b_pu[mi], b_sg[mi]], writes=[b_aT[ai]])
            for j in range(nt):
                for n in range(4):
                    di = 0
                    for cch in range(4):
                        sch.op("pe", lambda e, cch=cch, j=j, n=n: e.matmul(pd[di][:], aT[ai][:, cch, j * 128:(j + 1) * 128],
                                                                           wd[wi][:, cch, n * 512:(n + 1) * 512],
                                                                           start=(cch == 0), stop=(cch == 3)),
                               reads=[b_aT[ai], b_wd[wi]], writes=[b_pd[di]])
                    sch.op("dve", lambda e, j=j, n=n: e.scalar_tensor_tensor(
                        out=yacc[:, j, n * 512:(n + 1) * 512], in0=pd[di][:], scalar=coef[:, j, e_:e_ + 1],
                        in1=yacc[:, j, n * 512:(n + 1) * 512], op0=ALU.mult, op1=ALU.add),
                        reads=[b_pd[di], b_coef, b_y[j]], writes=[b_y[j]])

        def do_tile(tl0):
            nt = min(TT, NBo - tl0)
            for j in range(nt):
                route(j, tl0 + j)
            import os
            for e_ in range(int(os.environ.get("MOE_NE", 64))):
                expert(e_, nt)
            for j in range(nt):
                tl = tl0 + j
                sch.dma("sp", lambda e, j=j, tl=tl: e.dma_start(out=out_ap[tl * 128:(tl + 1) * 128, :], in_=yacc[:, j, :]),
                        reads=[b_y[j]], writes=[b_out])

        for tl0 in range(0, NBo, TT):
            do_tile(tl0)
        sch.barrier()
        sch.emit()


def setup_percore(c, sch, es, masks_ap, selv_ap):
    nc = c.nc
    c.maskt = es.enter_context(nc.sbuf_tensor(uname("maskt"), [128, 8, 128], BF16))
    c.selt = es.enter_context(nc.sbuf_tensor(uname("selt"), [128, 4], F32))
    b = Buf("percore", multi=True, persist=True)
    sch.dma("pool", lambda e: e.dma_start(out=c.maskt[:], in_=masks_ap.rearrange("m p q -> p m q")), writes=[b])
    sch.dma("sp", lambda e: e.dma_start(out=c.selt[:], in_=selv_ap.partition_broadcast(128)), writes=[b])
    sch.op("dve", lambda e: e.tensor_copy(out=c.selt[:], in_=c.selt[:]), reads=[b, c.b_const], writes=[c.b_const])


def percore_consts(par):
    q = np.arange(128)[None, :]
    p = np.arange(128)[:, None]
    tri = {0: (q >= p).astype(np.float32), 1: (q > p).astype(np.float32)}
    ones, zeros = np.ones((128, 128), np.float32), np.zeros((128, 128), np.float32)
    masks = np.zeros((2, 2, 2, 128, 128), np.float32)
    selv = np.zeros((4,), np.float32)
    for jp in range(2):
        first = (par == 0) == (jp == 0)
        selv[2 * jp], selv[2 * jp + 1] = (1.0, 0.0) if first else (0.0, 1.0)
        for kind in range(2):
            masks[kind, jp, 0] = tri[kind] if first else ones
            masks[kind, jp, 1] = zeros if first else tri[kind]
    return masks.reshape(8, 128, 128), selv


def layer(c, sch, L, xblk_all, x_own_ap, out_ap, b_out, full=False):
    c.full = full
    c.NBo = c.NB if full else c.NB // 2
    c.So = c.NBo * 128
    NBo = c.NBo
    phase_xnT(c, sch, xblk_all, c.w["norm_mix"][L], c.sc["xnT"], list(range(c.NB)), c.b_sc["xnT"])
    if not full:
        phase_xnT(c, sch, lambda tb: x_own_ap[tb * 128:(tb + 1) * 128, :], c.w["norm_mix"][L], c.sc["xnTo"],
                  list(range(NBo)), c.b_sc["xnTo"])
    phase_inproj(c, sch, L)
    phase_mla_up(c, sch, L)
    phase_foxcum(c, sch)
    for kind in ("mla", "sb", "fox"):
        phase_attn(c, sch, kind)
    phase_merge_out(c, sch, L, x_own_ap)
    import os
    if not os.environ.get("NOMOE"):
        phase_moe(c, sch, L, out_ap, b_out)


def phase_select_own(c, sch, xall_ap, xown_ap, b_in, b_out):
    nc = c.nc
    NBo = c.NB // 2
    with contextlib.ExitStack() as es:
        T = lambda name, shape, dt: es.enter_context(nc.sbuf_tensor(uname(name), shape, dt))
        ta = [T("sela", [128, D], F32) for _ in range(2)]
        tb_ = [T("selb", [128, D], F32) for _ in range(2)]
        b_a = [Buf("sela%d" % i) for i in range(2)]
        b_b = [Buf("selb%d" % i) for i in range(2)]
        for j in range(NBo):
            i = j % 2
            jp = j % 2
            sa, sb_ = c.selt[:, 2 * jp:2 * jp + 1], c.selt[:, 2 * jp + 1:2 * jp + 2]
            sch.dma("sp", lambda e, i=i, j=j: e.dma_start(out=ta[i][:], in_=xall_ap[(2 * j) * 128:(2 * j + 1) * 128, :]),
                    reads=[b_in], writes=[b_a[i]])
            sch.dma("sp", lambda e, i=i, j=j: e.dma_start(out=tb_[i][:], in_=xall_ap[(2 * j + 1) * 128:(2 * j + 2) * 128, :]),
                    reads=[b_in], writes=[b_b[i]])
            sch.op("dve", lambda e, i=i, sa=sa: e.tensor_scalar(out=ta[i][:], in0=ta[i][:], scalar1=sa, scalar2=None, op0=ALU.mult),
                   reads=[b_a[i], c.b_const], writes=[b_a[i]])
            sch.op("dve", lambda e, i=i, sb_=sb_: e.scalar_tensor_tensor(out=tb_[i][:], in0=tb_[i][:], scalar=sb_, in1=ta[i][:],
                                                                         op0=ALU.mult, op1=ALU.add),
                   reads=[b_a[i], b_b[i], c.b_const], writes=[b_b[i]])
            sch.dma("sp", lambda e, i=i, j=j: e.dma_start(out=xown_ap[j * 128:(j + 1) * 128, :], in_=tb_[i][:]),
                    reads=[b_b[i]], writes=[b_out])
        sch.barrier()
        sch.emit()


NCORE = 8


def build_program(S, depth):
    nc = bass.Bass("TRN2", target_bir_lowering=False)
    c = Ctx()
    c.nc, c.S, c.NB = nc, S, S // 128
    So = S // 2
    x = nc.dram_tensor("x", [S, D], F32, kind="ExternalInput").ap()
    pos = nc.dram_tensor("pos", [S], I32, kind="ExternalInput").ap()
    poso = nc.dram_tensor("poso", [So], I32, kind="ExternalInput").ap()
    masks = nc.dram_tensor("masks", [8, 128, 128], F32, kind="ExternalInput").ap()
    selv = nc.dram_tensor("selv", [4], F32, kind="ExternalInput").ap()
    out = nc.dram_tensor("out", [So, D], F32, kind="ExternalOutput").ap()
    xs = [nc.dram_tensor("xl%d" % l, [S, D], F32, kind="Internal").ap() for l in range(depth - 1)]
    xown = nc.dram_tensor("xown", [So, D], F32, kind="Internal").ap()
    c.w = {}
    for n, shp in WSHAPES.items():
        t = nc.dram_tensor(n, [depth] + list(shp), F32, kind="ExternalInput").ap()
        c.w[n] = [t[l] for l in range(depth)]
    make_scratch(c)
    with contextlib.ExitStack() as es:
        sch = Sched(nc, es)
        setup_consts(c, sch, es)
        setup_masks(c, sch, es)
        setup_percore(c, sch, es, masks, selv)
        setup_rope(c, sch, es, pos, c.NB)
        setup_rope(c, sch, es, poso, c.NB // 2, "_o")
        b_xs = [Buf("xl%d" % l, multi=True, persist=True) for l in range(depth - 1)]
        b_xown = Buf("xown", multi=True, persist=True)
        b_out = Buf("out", multi=True, persist=True)
        cur, b_cur = x, None
        for l in range(depth):
            xblk = (lambda cur: (lambda tb: cur[tb * 128:(tb + 1) * 128, :]))(cur)
            if l < depth - 1:
                layer(c, sch, l, xblk, cur, xs[l], b_xs[l], full=True)
                cur, b_cur = xs[l], b_xs[l]
            else:
                phase_select_own(c, sch, cur, xown, b_cur if b_cur is not None else Buf("xin", multi=True, persist=True), b_xown)
                layer(c, sch, l, xblk, xown, out, b_out, full=False)
    return nc


_PROGS = {}


def kernel(**inputs):
    x = np.ascontiguousarray(np.asarray(inputs["x"], dtype=np.float32))
    positions = np.asarray(inputs["positions"]).astype(np.int32)
    B, S, _ = x.shape
    NB = S // 128
    depth = int(np.asarray(inputs["norm_mix"]).shape[0])
    key = (S, depth)
    if key not in _PROGS:
        _PROGS[key] = build_program(S, depth)
    ws = {n: np.ascontiguousarray(np.asarray(inputs[n], dtype=np.float32)) for n in WSHAPES}
    in_maps, owntoks = [], []
    for core in range(NCORE):
        b, par = core // 2, core % 2
        own = own_blocks(NB, par)
        owntok = np.concatenate([np.arange(i * 128, (i + 1) * 128) for i in own])
        owntoks.append(owntok)
        mk_, sv_ = percore_consts(par)
        m = {"x": x[b], "pos": positions[b], "poso": np.ascontiguousarray(positions[b][owntok]), "masks": mk_, "selv": sv_}
        m.update(ws)
        in_maps.append(m)
    res = run_bass_kernel_spmd(_PROGS[key], in_maps, core_ids=list(range(NCORE)))
    outp = np.empty_like(x)
    for core in range(NCORE):
        outp[core // 2][owntoks[core]] = np.asarray(res.results[core]["out"])
    return outp
```

```python
from concourse.bass_utils import run_bass_kernel_spmd
import contextlib
import numpy as np
import concourse.bass as bass
import concourse.mybir as mybir

F32 = mybir.dt.float32
BF16 = mybir.dt.bfloat16
I32 = mybir.dt.int32
AF = mybir.ActivationFunctionType
ALU = mybir.AluOpType
AX = mybir.AxisListType

ENGS = ("pe", "act", "dve", "pool", "sp")


class Buf:
    def __init__(self, name, multi=False, persist=False):
        self.name = name
        self.multi = multi
        self.persist = persist
        self.writers = {}
        self.readers = {}
        self.sem = None
        self.ndma = 0


class Op:
    __slots__ = ("eng", "fn", "deps", "kind", "buf", "token", "needed", "key")

    def __init__(self, eng, fn, kind):
        self.eng = eng
        self.fn = fn
        self.kind = kind
        self.deps = []
        self.buf = None
        self.token = None
        self.needed = False
        self.key = None


class Sched:
    def __init__(self, nc, es):
        self.nc = nc
        self.es = es
        self.ops = []
        self.esem = {e: es.enter_context(nc.semaphore("sem_" + e)) for e in ENGS}
        self.ecount = {e: 0 for e in ENGS}
        self.seen = {e: {} for e in ENGS}
        self.nsem = 0
        self.last_op = {e: None for e in ENGS}
        self.dma_pending = {}
        self.pool_free = []
        self.phase_bufs = []

    def _bufsem(self, buf):
        if buf.sem is None:
            if self.pool_free and not buf.persist:
                buf.sem, buf.ndma = self.pool_free.pop()
            else:
                buf.sem = self.es.enter_context(self.nc.semaphore("bs%d_%s" % (self.nsem, buf.name)))
                self.nsem += 1
            if not buf.persist:
                self.phase_bufs.append(buf)
        return buf.sem

    def _add(self, op, reads, writes):
        deps = {}
        for b in reads:
            for w in b.writers.values():
                deps[id(w)] = w
        for b in writes:
            for r in b.readers.values():
                deps[id(r)] = r
            if not b.multi:
                for w in b.writers.values():
                    deps[id(w)] = w
        deps.pop(id(op), None)
        op.deps = list(deps.values())
        for d in op.deps:
            d.needed = True
        for b in reads:
            b.readers[op.key] = op
        for b in writes:
            if b.readers or not b.multi:
                b.writers = {}
                b.readers = {}
            b.writers[op.key] = op
        self.ops.append(op)
        self.last_op[op.eng] = op

    def op(self, eng, fn, reads=(), writes=()):
        o = Op(eng, fn, "c")
        o.key = eng
        self._add(o, reads, writes)
        return o

    def dma(self, eng, fn, reads=(), writes=()):
        o = Op(eng, fn, "d")
        b = writes[0]
        o.buf = b
        self._bufsem(b)
        o.key = "dma_" + b.name
        self._add(o, reads, writes)
        self.dma_pending[o.key] = o
        return o

    def barrier(self):
        lasts = [o for o in self.last_op.values() if o is not None]
        pend = list(self.dma_pending.values())
        for e in ENGS:
            o = Op(e, None, "c")
            o.key = e
            o.deps = [d for d in lasts + pend]
            for d in o.deps:
                d.needed = True
            self.ops.append(o)
        self.dma_pending = {}

    def emit(self):
        nc = self.nc
        for o in self.ops:
            if o.kind == "d":
                o.buf.ndma += 1
                o.token = (o.buf.sem, 16 * o.buf.ndma)
            elif o.needed and o.fn is not None:
                self.ecount[o.eng] += 1
                o.token = (self.esem[o.eng], self.ecount[o.eng])
        per = {e: [o for o in self.ops if o.eng == e] for e in ENGS}

        def run(engname, eng):
            seen = self.seen[engname]
            for o in per[engname]:
                need = {}
                stack = list(o.deps)
                while stack:
                    d = stack.pop()
                    if d.token is None:
                        stack.extend(d.deps)
                        continue
                    if d.kind == "c" and d.eng == engname and engname == "pe":
                        continue
                    sem, val = d.token
                    k = id(sem)
                    if k not in need or need[k][1] < val:
                        need[k] = (sem, val)
                for k, (sem, val) in need.items():
                    if seen.get(k, 0) >= val:
                        continue
                    eng.wait_ge(sem, val)
                    seen[k] = val
                if o.fn is None:
                    continue
                inst = o.fn(eng)
                if o.token is not None:
                    sem, val = o.token
                    inst.then_inc(sem, 16 if o.kind == "d" else 1)

        with nc.Block() as block:
            @block.tensor
            def _(t):
                run("pe", t)

            @block.scalar
            def _(s):
                run("act", s)

            @block.vector
            def _(v):
                run("dve", v)

            @block.gpsimd
            def _(g):
                run("pool", g)

            @block.sync
            def _(sy):
                run("sp", sy)
        self.ops = []
        self.last_op = {e: None for e in ENGS}
        for b in self.phase_bufs:
            self.pool_free.append((b.sem, b.ndma))
            b.sem = None
        self.phase_bufs = []


import contextlib
import numpy as np
import concourse.bass as bass
import concourse.mybir as mybir

D = 2048
KD = D // 128
H = 16
EPS = 1e-6
SEG = {}
_o = 0
for _n, _w in (("mla_cq", 512), ("mla_ckv", 512), ("mla_krope", 64), ("sb_q", 1024), ("sb_k", 1024),
               ("sb_v", 1024), ("fox_q", 1024), ("fox_k", 1024), ("fox_v", 1024), ("fox_f", 16),
               ("gates", 6144)):
    SEG[_n] = (_o, _o + _w)
    _o += _w
D_IN = _o


class Ctx:
    pass


_UID = [0]


def uname(n):
    _UID[0] += 1
    return "%s_u%d" % (n, _UID[0])


def own_blocks(nb, par):
    return [i for i in range(nb) if (i % 4 in (0, 3)) == (par == 0)]


def phase_xnT(c, sch, x_ap, gain_ap, xnT_ap, blocks, wr_buf):
    nc = c.nc
    with contextlib.ExitStack() as es:
        T = lambda name, shape, dt: es.enter_context(nc.sbuf_tensor(uname(name), shape, dt))
        xin = [T("xin%d" % i, [128, D], F32) for i in range(2)]
        sq = T("sq", [128, D], F32)
        gbc = T("gbc", [128, D], F32)
        xn = [T("xn%d" % i, [128, D], BF16) for i in range(2)]
        ss = T("ss", [128, 4], F32)
        xt = [T("xt%d" % i, [128, KD, 128], BF16) for i in range(2)]
        pst = [es.enter_context(nc.psum_tensor(uname("pst%d" % i), [128, 4, 128], BF16)) for i in range(4)]
        b_xin = [Buf("xin%d" % i) for i in range(2)]
        b_sq, b_g, b_ss = Buf("sq"), Buf("gbc"), Buf("ss")
        b_xn = [Buf("xn%d" % i) for i in range(2)]
        b_xt = [Buf("xt%d" % i) for i in range(2)]
        b_ps = [Buf("pst%d" % i) for i in range(4)]
        sch.dma("sp", lambda e: e.dma_start(out=gbc[:], in_=gain_ap.partition_broadcast(128)), writes=[b_g])
        for n, tb in enumerate(blocks):
            i = n % 2
            sch.dma("sp", lambda e, i=i, tb=tb: e.dma_start(out=xin[i][:], in_=x_ap(tb)),
                    writes=[b_xin[i]])
            sch.op("act", lambda e, i=i: e.activation(out=sq[:], in_=xin[i][:], func=AF.Square),
                   reads=[b_xin[i]], writes=[b_sq])
            sch.op("dve", lambda e: e.tensor_reduce(out=ss[:, 0:1], in_=sq[:], axis=AX.X, op=ALU.add),
                   reads=[b_sq], writes=[b_ss])
            sch.op("dve", lambda e: e.tensor_scalar(out=ss[:, 1:2], in0=ss[:, 0:1], scalar1=1.0 / D, scalar2=EPS,
                                                    op0=ALU.mult, op1=ALU.add), reads=[b_ss], writes=[b_ss])
            sch.op("act", lambda e: e.activation(out=ss[:, 3:4], in_=ss[:, 1:2], func=AF.Ln), reads=[b_ss], writes=[b_ss])
            sch.op("act", lambda e: e.activation(out=ss[:, 2:3], in_=ss[:, 3:4], func=AF.Exp, scale=-0.5),
                   reads=[b_ss], writes=[b_ss])
            sch.op("dve", lambda e, i=i: e.scalar_tensor_tensor(out=xn[i][:], in0=xin[i][:], scalar=ss[:, 2:3],
                                                                in1=gbc[:], op0=ALU.mult, op1=ALU.mult),
                   reads=[b_xin[i], b_ss, b_g], writes=[b_xn[i]])
            for q in range(4):
                pi = (n * 4 + q) % 4
                for j in range(4):
                    k = q * 4 + j
                    sch.op("pe", lambda e, i=i, k=k, pi=pi, j=j: e.transpose(
                        out=pst[pi][:, j, :], in_=xn[i][:, k * 128:(k + 1) * 128], identity=c.ident[:]),
                        reads=[b_xn[i], c.b_const], writes=[b_ps[pi]])
                eng = "act" if q % 2 else "dve"
                if eng == "act":
                    sch.op("act", lambda e, i=i, q=q, pi=pi: e.copy(out=xt[i][:, q * 4:(q + 1) * 4, :], in_=pst[pi][:]),
                           reads=[b_ps[pi]], writes=[b_xt[i]])
                else:
                    sch.op("dve", lambda e, i=i, q=q, pi=pi: e.tensor_copy(out=xt[i][:, q * 4:(q + 1) * 4, :], in_=pst[pi][:]),
                           reads=[b_ps[pi]], writes=[b_xt[i]])
            sch.dma("sp", lambda e, i=i, tb=tb: e.dma_start(out=xnT_ap[:, :, tb * 128:(tb + 1) * 128], in_=xt[i][:]),
                    reads=[b_xt[i]], writes=[wr_buf])
        sch.barrier()
        sch.emit()


def setup_consts(c, sch, es):
    nc = c.nc
    c.ident = es.enter_context(nc.sbuf_tensor("ident", [128, 128], BF16))
    c.identf = es.enter_context(nc.sbuf_tensor("identf", [128, 128], F32))
    c.b_const = Buf("const")
    sch.op("pool", lambda e: e.memset(c.identf[:], 1.0), writes=[c.b_const])
    sch.op("pool", lambda e: e.affine_select(out=c.identf[:], in_=c.identf[:], pattern=[[-1, 128]],
                                             compare_op=ALU.is_equal, fill=0.0, base=0, channel_multiplier=1),
           reads=[c.b_const], writes=[c.b_const])
    sch.op("pool", lambda e: e.tensor_copy(out=c.ident[:], in_=c.identf[:]), reads=[c.b_const], writes=[c.b_const])


def rstd_ops(sch, ss_ap, tmp_ap, out_ap, d, b_small):
    sch.op("dve", lambda e: e.tensor_scalar(out=tmp_ap, in0=ss_ap, scalar1=1.0 / d, scalar2=EPS,
                                            op0=ALU.mult, op1=ALU.add), reads=[b_small], writes=[b_small])
    sch.op("act", lambda e: e.activation(out=tmp_ap, in_=tmp_ap, func=AF.Ln), reads=[b_small], writes=[b_small])
    sch.op("act", lambda e: e.activation(out=out_ap, in_=tmp_ap, func=AF.Exp, scale=-0.5),
           reads=[b_small], writes=[b_small])


def phase_linear(c, sch, inT_ap, KC, W_ap, chunks, blocks, post, extra_setup=None, TT=8):
    nc = c.nc
    with contextlib.ExitStack() as es:
        T = lambda name, shape, dt: es.enter_context(nc.sbuf_tensor(uname(name), shape, dt))
        inT = T("inT", [128, KC, TT * 128], BF16)
        b_in = Buf("inT")
        wch = [T("wch%d" % i, [128, KC, 512], BF16) for i in range(2)]
        b_w = [Buf("wch%d" % i) for i in range(2)]
        ps = [es.enter_context(nc.psum_tensor(uname("lps%d" % i), [128, 512], F32)) for i in range(3)]
        b_ps = [Buf("lps%d" % i) for i in range(3)]
        env = Ctx()
        env.T, env.es, env.nc = T, es, nc
        if extra_setup is not None:
            extra_setup(env)
        Wv = W_ap.rearrange("(k p) n -> p k n", p=128)
        cnt = 0
        wcnt = 0
        for t0 in range(0, len(blocks), TT):
            tbs = blocks[t0:t0 + TT]
            for j, tb in enumerate(tbs):
                sch.dma("sp", lambda e, j=j, tb=tb: e.dma_start(out=inT[:, :, j * 128:(j + 1) * 128],
                                                                 in_=inT_ap[:, :, tb * 128:(tb + 1) * 128]),
                        writes=[b_in])
            for ci, (lo, n, tag) in enumerate(chunks):
                wi = wcnt % 2
                wcnt += 1
                sch.dma("pool", lambda e, wi=wi, lo=lo, n=n: e.dma_start(out=wch[wi][:, :, 0:n], in_=Wv[:, :, lo:lo + n]),
                        writes=[b_w[wi]])
                for j, tb in enumerate(tbs):
                    pi = cnt % 3
                    cnt += 1
                    for k in range(KC):
                        sch.op("pe", lambda e, pi=pi, k=k, j=j, wi=wi, n=n: e.matmul(
                            ps[pi][:, 0:n], inT[:, k, j * 128:(j + 1) * 128], wch[wi][:, k, 0:n],
                            start=(k == 0), stop=(k == KC - 1)),
                            reads=[b_in, b_w[wi]], writes=[b_ps[pi]])
                    post(env, tag, ci, tb, t0 + j, ps[pi], b_ps[pi])
        sch.barrier()
        sch.emit()


def transpose_out(c, sch, env, src_ap, ncols, dst_fn, b_src, key="tr"):
    nc = env.nc
    if not hasattr(env, "trs"):
        env.trs = [env.es.enter_context(nc.psum_tensor(uname("trp%d" % i), [128, 128], BF16)) for i in range(2)]
        env.b_trs = [Buf("trp%d" % i) for i in range(2)]
        env.tro = [env.T("tro%d" % i, [128, 128], BF16) for i in range(4)]
        env.b_tro = [Buf("tro%d" % i) for i in range(4)]
        env.trn = 0
    n = env.trn
    env.trn += 1
    p, o = n % 2, n % 4
    sch.op("pe", lambda e: e.transpose(out=env.trs[p][0:ncols, :], in_=src_ap, identity=c.ident[:]),
           reads=[b_src, c.b_const], writes=[env.b_trs[p]])
    if n % 2:
        sch.op("act", lambda e: e.copy(out=env.tro[o][0:ncols, :], in_=env.trs[p][0:ncols, :]),
               reads=[env.b_trs[p]], writes=[env.b_tro[o]])
    else:
        sch.op("dve", lambda e: e.tensor_copy(out=env.tro[o][0:ncols, :], in_=env.trs[p][0:ncols, :]),
               reads=[env.b_trs[p]], writes=[env.b_tro[o]])
    dst_ap, b_dst = dst_fn()
    sch.dma("sp", lambda e: e.dma_start(out=dst_ap, in_=env.tro[o][0:ncols, :]), reads=[env.b_tro[o]], writes=[b_dst])


def headnorm(c, sch, env, ps, b_ps, nh, d, gain_t, b_gain, out_ap, b_out):
    if not hasattr(env, "hn_sq"):
        env.hn_sq = env.T("hn_sq", [128, 512], F32)
        env.hn_t = env.T("hn_t", [128, 512], F32)
        env.hn_s = env.T("hn_s", [128, 64], F32)
        env.b_hn = Buf("hn")
        env.b_hns = Buf("hns")
    n = nh * d
    sq, t1, s = env.hn_sq, env.hn_t, env.hn_s
    sch.op("act", lambda e: e.activation(out=sq[:, 0:n], in_=ps[:, 0:n], func=AF.Square), reads=[b_ps], writes=[env.b_hn])
    sch.op("dve", lambda e: e.tensor_reduce(out=s[:, 0:nh], in_=sq[:, 0:n].rearrange("p (h d) -> p h d", d=d),
                                            axis=AX.X, op=ALU.add), reads=[env.b_hn], writes=[env.b_hns])
    rstd_ops(sch, s[:, 0:nh], s[:, 16:16 + nh], s[:, 32:32 + nh], d, env.b_hns)
    sch.op("dve", lambda e: e.tensor_tensor(out=t1[:, 0:n].rearrange("p (h d) -> p h d", d=d),
                                            in0=ps[:, 0:n].rearrange("p (h d) -> p h d", d=d),
                                            in1=s[:, 32:32 + nh].unsqueeze(2).to_broadcast([128, nh, d]), op=ALU.mult),
           reads=[b_ps, env.b_hns, env.b_hn], writes=[env.b_hn])
    sch.op("dve", lambda e: e.tensor_tensor(out=out_ap.rearrange("p (h d) -> p h d", d=d),
                                            in0=t1[:, 0:n].rearrange("p (h d) -> p h d", d=d),
                                            in1=gain_t[:, 0:d].unsqueeze(1).to_broadcast([128, nh, d]), op=ALU.mult),
           reads=[env.b_hn, b_gain], writes=[b_out])


def setup_rope(c, sch, es, pos_ap, NB, suffix=""):
    nc = c.nc
    import math
    cos_t = es.enter_context(nc.sbuf_tensor(uname("cos"), [128, NB, 32], F32))
    sin_t = es.enter_context(nc.sbuf_tensor(uname("sin"), [128, NB, 32], F32))
    setattr(c, "cos" + suffix, cos_t)
    setattr(c, "sin" + suffix, sin_t)
    if not hasattr(c, "b_rope"):
        c.b_rope = Buf("rope")
    with contextlib.ExitStack() as es2:
        T = lambda name, shape, dt: es2.enter_context(nc.sbuf_tensor(uname(name), shape, dt))
        posi = T("posi", [128, NB], I32)
        posf = T("posf", [128, NB], F32)
        invf = T("invf", [128, 32], F32)
        ang = T("ang", [128, NB, 32], F32)
        r = T("rr", [128, NB, 32], F32)
        b = Buf("ropetmp")
        sch.dma("sp", lambda e: e.dma_start(out=posi[:], in_=pos_ap.rearrange("(b p) -> p b", p=128), allow_slow_non_contiguous=True), writes=[b])
        sch.op("dve", lambda e: e.tensor_copy(out=posf[:], in_=posi[:]), reads=[b], writes=[b])
        for i in range(32):
            v = float(np.float32(10000.0) ** np.float32(-(2.0 * i) / 64.0))
            sch.op("pool", lambda e, i=i, v=v: e.memset(invf[:, i:i + 1], v), writes=[b])
        sch.op("dve", lambda e: e.tensor_tensor(out=ang[:], in0=posf[:].unsqueeze(2).to_broadcast([128, NB, 32]),
                                                in1=invf[:].unsqueeze(1).to_broadcast([128, NB, 32]), op=ALU.mult),
               reads=[b], writes=[b])
        qi = T("qi", [128, NB, 32], I32)
        m = T("rm", [128, NB, 32], F32)
        TWO_PI = 2 * math.pi
        for dst, sh in ((sin_t, 0.0), (cos_t, math.pi / 2)):
            sch.op("dve", lambda e, sh=sh: e.tensor_scalar(out=m[:], in0=ang[:], scalar1=sh, scalar2=None, op0=ALU.add),
                   reads=[b], writes=[b])
            sch.op("dve", lambda e: e.tensor_scalar(out=r[:], in0=m[:], scalar1=1.0 / TWO_PI, scalar2=None, op0=ALU.mult),
                   reads=[b], writes=[b])
            sch.op("dve", lambda e: e.tensor_copy(out=qi[:], in_=r[:]), reads=[b], writes=[b])
            sch.op("dve", lambda e: e.tensor_copy(out=r[:], in_=qi[:]), reads=[b], writes=[b])
            sch.op("dve", lambda e: e.scalar_tensor_tensor(out=r[:], in0=r[:], scalar=-TWO_PI, in1=m[:],
                                                           op0=ALU.mult, op1=ALU.add), reads=[b], writes=[b])
            sch.op("dve", lambda e: e.tensor_scalar(out=m[:], in0=r[:], scalar1=math.pi, scalar2=None, op0=ALU.is_gt),
                   reads=[b], writes=[b])
            sch.op("dve", lambda e: e.scalar_tensor_tensor(out=r[:], in0=m[:], scalar=-TWO_PI, in1=r[:],
                                                           op0=ALU.mult, op1=ALU.add), reads=[b], writes=[b])
            sch.op("dve", lambda e: e.tensor_scalar(out=m[:], in0=r[:], scalar1=-math.pi, scalar2=None, op0=ALU.is_lt),
                   reads=[b], writes=[b])
            sch.op("dve", lambda e: e.scalar_tensor_tensor(out=r[:], in0=m[:], scalar=TWO_PI, in1=r[:],
                                                           op0=ALU.mult, op1=ALU.add), reads=[b], writes=[b])
            sch.op("act", lambda e, dst=dst: e.activation(out=dst[:], in_=r[:], func=AF.Sin), reads=[b], writes=[c.b_rope])
        sch.barrier()
        sch.emit()


def rope_ops(c, sch, env, x_ap, out_ap, nh, tb, b_x, b_out, own=False):
    if not hasattr(env, "rp_a"):
        env.rp_a = env.T("rp_a", [128, 16, 32], F32)
        env.rp_b = env.T("rp_b", [128, 16, 32], F32)
        env.b_rp = Buf("rp")
    a, b2 = env.rp_a[:, 0:nh, :], env.rp_b[:, 0:nh, :]
    cos_t, sin_t = (c.cos_o, c.sin_o) if (own and not c.full) else (c.cos, c.sin)
    cs = cos_t[:, tb, :].unsqueeze(1).to_broadcast([128, nh, 32])
    sn = sin_t[:, tb, :].unsqueeze(1).to_broadcast([128, nh, 32])
    x1, x2 = x_ap[:, :, 0:32], x_ap[:, :, 32:64]
    rd = [b_x, c.b_rope, env.b_rp]
    sch.op("dve", lambda e: e.tensor_tensor(out=a, in0=x1, in1=cs, op=ALU.mult), reads=rd, writes=[env.b_rp])
    sch.op("dve", lambda e: e.tensor_tensor(out=b2, in0=x2, in1=sn, op=ALU.mult), reads=rd, writes=[env.b_rp])
    sch.op("dve", lambda e: e.tensor_tensor(out=out_ap[:, :, 0:32], in0=a, in1=b2, op=ALU.subtract),
           reads=[env.b_rp], writes=[b_out])
    sch.op("dve", lambda e: e.tensor_tensor(out=a, in0=x2, in1=cs, op=ALU.mult), reads=rd + [b_out], writes=[env.b_rp])
    sch.op("dve", lambda e: e.tensor_tensor(out=b2, in0=x1, in1=sn, op=ALU.mult), reads=rd, writes=[env.b_rp])
    sch.op("dve", lambda e: e.tensor_tensor(out=out_ap[:, :, 32:64], in0=a, in1=b2, op=ALU.add),
           reads=[env.b_rp], writes=[b_out])


def load_bc(sch, env, name, ap, n):
    t = env.T(name, [128, n], F32)
    b = Buf(name)
    sch.dma("sp", lambda e: e.dma_start(out=t[:], in_=ap.partition_broadcast(128)), writes=[b])
    return t, b


def stage(env, kind):
    if not hasattr(env, "stg"):
        env.stg = {"b": [env.T("stb%d" % i, [128, 512], BF16) for i in range(4)],
                   "f": [env.T("stf%d" % i, [128, 512], F32) for i in range(3)]}
        env.bstg = {"b": [Buf("stb%d" % i) for i in range(4)], "f": [Buf("stf%d" % i) for i in range(3)]}
        env.nstg = {"b": 0, "f": 0}
    i = env.nstg[kind] % len(env.stg[kind])
    env.nstg[kind] += 1
    return env.stg[kind][i], env.bstg[kind][i]


def phase_inproj(c, sch, L):
    W = c.w
    S, So = c.S, c.So
    sc = c.sc
    w_in = W["w_in"][L]

    def chunks_of(names):
        out = []
        for nm in names:
            lo, hi = SEG[nm]
            for q, s0 in enumerate(range(lo, hi, 512)):
                out.append((s0, min(512, hi - s0), (nm, q)))
        return out

    def setup_q(env):
        env.g_cq = load_bc(sch, env, "g_cq", W["mla_cq_norm"][L], 512)
        env.g_sbq = load_bc(sch, env, "g_sbq", W["sb_q_norm"][L], 64)
        env.g_fxq = load_bc(sch, env, "g_fxq", W["fox_q_norm"][L], 64)

    def post_q(env, tag, ci, tb, nl, ps, b_ps):
        nm, q = tag
        tok = slice(nl * 128, (nl + 1) * 128)
        if nm == "mla_cq":
            ob, b_ob = stage(env, "b")
            headnorm(c, sch, env, ps, b_ps, 1, 512, env.g_cq[0], env.g_cq[1], ob[:, 0:512], b_ob)
            for k in range(4):
                transpose_out(c, sch, env, ob[:, k * 128:(k + 1) * 128], 128,
                              lambda k=k: (sc["cqT"][:, k, tok], c.b_sc["cqT"]), b_ob)
        elif nm in ("sb_q", "fox_q"):
            g = env.g_sbq if nm == "sb_q" else env.g_fxq
            dst = "sbQT" if nm == "sb_q" else "foxQT"
            ob, b_ob = stage(env, "b")
            headnorm(c, sch, env, ps, b_ps, 8, 64, g[0], g[1], ob[:, 0:512], b_ob)
            for k in range(4):
                transpose_out(c, sch, env, ob[:, k * 128:(k + 1) * 128], 128,
                              lambda k=k: (sc[dst][q * 4 + k, :, tok], c.b_sc[dst]), b_ob)
        elif nm == "gates":
            ob, b_ob = stage(env, "b")
            sch.op("act", lambda e: e.activation(out=ob[:], in_=ps[:], func=AF.Sigmoid), reads=[b_ps], writes=[b_ob])
            sch.dma("sp", lambda e: e.dma_start(out=sc["gates"][tok, q * 512:(q + 1) * 512], in_=ob[:]),
                    reads=[b_ob], writes=[c.b_sc["gates"]])

    phase_linear(c, sch, sc["xnT"] if c.full else sc["xnTo"], KD, w_in, chunks_of(["mla_cq", "sb_q", "fox_q", "gates"]),
                 list(range(c.NBo)), post_q, setup_q)

    def setup_k(env):
        env.g_ckv = load_bc(sch, env, "g_ckv", W["mla_ckv_norm"][L], 512)
        env.g_sbk = load_bc(sch, env, "g_sbk", W["sb_k_norm"][L], 64)
        env.g_fxk = load_bc(sch, env, "g_fxk", W["fox_k_norm"][L], 64)
        env.fb = load_bc(sch, env, "fbias", W["fox_f_bias"][L], 16)

    def post_k(env, tag, ci, tb, nl, ps, b_ps):
        nm, q = tag
        tok = slice(tb * 128, (tb + 1) * 128)
        if nm == "mla_ckv":
            ob, b_ob = stage(env, "b")
            headnorm(c, sch, env, ps, b_ps, 1, 512, env.g_ckv[0], env.g_ckv[1], ob[:, 0:512], b_ob)
            for k in range(4):
                transpose_out(c, sch, env, ob[:, k * 128:(k + 1) * 128], 128,
                              lambda k=k: (sc["ckvT"][:, k, tok], c.b_sc["ckvT"]), b_ob)
        elif nm in ("sb_k", "fox_k"):
            g = env.g_sbk if nm == "sb_k" else env.g_fxk
            dst = "sbKT" if nm == "sb_k" else "foxKT"
            ob, b_ob = stage(env, "b")
            headnorm(c, sch, env, ps, b_ps, 8, 64, g[0], g[1], ob[:, 0:512], b_ob)
            for k in range(4):
                transpose_out(c, sch, env, ob[:, k * 128:(k + 1) * 128], 128,
                              lambda k=k: (sc[dst][q * 4 + k, :, tok], c.b_sc[dst]), b_ob)
        elif nm in ("sb_v", "fox_v"):
            dst = "sbV" if nm == "sb_v" else "foxV"
            ob, b_ob = stage(env, "b")
            sch.op("act", lambda e: e.copy(out=ob[:], in_=ps[:]), reads=[b_ps], writes=[b_ob])
            sch.dma("sp", lambda e: e.dma_start(out=sc[dst][tok, q * 512:(q + 1) * 512], in_=ob[:]),
                    reads=[b_ob], writes=[c.b_sc[dst]])
        elif nm == "mla_krope":
            of, b_of = stage(env, "f")
            sch.op("act", lambda e: e.copy(out=of[:, 0:64], in_=ps[:, 0:64]), reads=[b_ps], writes=[b_of])
            sch.dma("sp", lambda e: e.dma_start(out=sc["kr"][tok, :], in_=of[:, 0:64]), reads=[b_of], writes=[c.b_sc["kr"]])
        elif nm == "fox_f":
            of, b_of = stage(env, "f")
            sch.op("dve", lambda e: e.tensor_tensor(out=of[:, 0:16], in0=ps[:, 0:16], in1=env.fb[0][:, 0:16], op=ALU.add),
                   reads=[b_ps, env.fb[1]], writes=[b_of])
            sch.op("act", lambda e: e.activation(out=of[:, 16:32], in_=of[:, 0:16], func=AF.Exp, scale=-1.0),
                   reads=[b_of], writes=[b_of])
            sch.op("act", lambda e: e.activation(out=of[:, 32:48], in_=of[:, 16:32], func=AF.Ln, bias=1.0),
                   reads=[b_of], writes=[b_of])
            sch.op("dve", lambda e: e.tensor_scalar(out=of[:, 48:64], in0=of[:, 32:48], scalar1=-1.0, scalar2=None,
                                                    op0=ALU.mult), reads=[b_of], writes=[b_of])
            sch.dma("sp", lambda e: e.dma_start(out=sc["lf"][tok, :], in_=of[:, 48:64]), reads=[b_of], writes=[c.b_sc["lf"]])

    phase_linear(c, sch, sc["xnT"], KD, w_in,
                 chunks_of(["mla_ckv", "mla_krope", "sb_k", "sb_v", "fox_k", "fox_v", "fox_f"]),
                 list(range(c.NB)), post_k, setup_k)


WSHAPES = {
    "norm_mix": (D,), "w_in": (D, D_IN), "mla_cq_norm": (512,), "mla_w_uq": (512, 3072), "mla_ckv_norm": (512,),
    "mla_w_ukv": (512, 4096), "mla_q_norm": (192,), "mla_k_norm": (192,), "sb_q_norm": (64,), "sb_k_norm": (64,),
    "fox_q_norm": (64,), "fox_k_norm": (64,), "fox_f_bias": (16,), "w_branch_mla": (2048, 2048),
    "w_branch_sb": (1024, 2048), "w_branch_fox": (1024, 2048), "w_out": (2048, 2048), "norm_ffn": (D,),
    "w_group": (D, 8), "w_router": (D, 64), "w_e_gate": (64, D, 512), "w_e_up": (64, D, 512), "w_e_down": (64, 512, D),
}


def scratch_shapes(S):
    So = S
    return {
        "xnT": ([128, KD, S], BF16), "xnTo": ([128, KD, So], BF16), "sbQT": ([8, 128, So], BF16), "sbKT": ([8, 128, S], BF16), "sbV": ([S, 1024], BF16),
        "foxQT": ([8, 128, So], BF16), "foxKT": ([8, 128, S], BF16), "foxV": ([S, 1024], BF16),
        "foxQa": ([64, So], BF16), "foxKa": ([64, S], BF16), "lf": ([S, 16], F32),
        "cqT": ([128, 4, So], BF16), "ckvT": ([128, 4, S], BF16), "kr": ([S, 64], F32),
        "mlaQTn": ([16, 128, So], BF16), "mlaQTr": ([16, 64, So], BF16), "mlaKTn": ([16, 128, S], BF16),
        "mlaKTr": ([16, 64, S], BF16), "mlaV": ([S, 2048], BF16), "gates": ([So, 6144], BF16),
        "OT_mla": ([2048, So], BF16), "OT_sb": ([1024, So], BF16), "OT_fox": ([1024, So], BF16),
        "xmid": ([So, D], F32),
    }


def make_scratch(c, dump=()):
    c.sc, c.b_sc = {}, {}
    for nm, (shape, dt) in scratch_shapes(c.S).items():
        kind = "ExternalOutput" if nm in dump else "Internal"
        c.sc[nm] = c.nc.dram_tensor("sc_" + nm, shape, dt, kind=kind).ap()
        c.b_sc[nm] = Buf("sc_" + nm, multi=True)


def setup_masks(c, sch, es):
    nc = c.nc
    mk = lambda n, dt: es.enter_context(nc.sbuf_tensor(uname(n), [128, 128], dt))
    c.mask_incl, c.mask_strict = mk("mincl", BF16), mk("mstrict", BF16)
    c.ntril8, c.nones8, c.ones_bf = mk("ntril8", BF16), mk("nones8", BF16), mk("onesbf", BF16)
    c.triu_f, c.ones_f = mk("triuf", F32), mk("onesf", F32)
    tmp = mk("mtmp", F32)
    b = c.b_const

    def sel(dst, val, pattern, cm, op):
        sch.op("pool", lambda e: e.memset(tmp[:], val), reads=[b], writes=[b])
        sch.op("pool", lambda e: e.affine_select(out=tmp[:], in_=tmp[:], pattern=pattern, compare_op=op, fill=0.0,
                                                 base=0, channel_multiplier=cm), reads=[b], writes=[b])
        sch.op("pool", lambda e: e.tensor_copy(out=dst[:], in_=tmp[:]), reads=[b], writes=[b])
    sel(c.mask_incl, 1.0, [[1, 128]], -1, ALU.is_ge)
    sel(c.mask_strict, 1.0, [[1, 128]], -1, ALU.is_gt)
    sel(c.ntril8, -8.0, [[-1, 128]], 1, ALU.is_ge)
    sel(c.triu_f, 1.0, [[1, 128]], -1, ALU.is_ge)
    sch.op("pool", lambda e: e.memset(c.nones8[:], -8.0), reads=[b], writes=[b])
    sch.op("pool", lambda e: e.memset(c.ones_bf[:], 1.0), reads=[b], writes=[b])
    sch.op("pool", lambda e: e.memset(c.ones_f[:], 1.0), reads=[b], writes=[b])


def phase_attn(c, sch, kind):
    nc = c.nc
    sc, S, So, NB = c.sc, c.S, c.So, c.NB
    dv = 128 if kind == "mla" else 64
    scale = (192 if kind == "mla" else 64) ** -0.5
    with contextlib.ExitStack() as es:
        T = lambda name, shape, dt: es.enter_context(nc.sbuf_tensor(uname(name), shape, dt))
        P = lambda name, shape, dt: es.enter_context(nc.psum_tensor(uname(name), shape, dt))
        nparts = 2 if kind == "mla" else 1
        KT = [[T("kt", [128, S], BF16) for _ in range(nparts)] for _ in range(2)]
        QT = [[T("qt", [128, So], BF16) for _ in range(nparts)] for _ in range(2)]
        Vt = [T("vt", [128, NB, dv], BF16) for _ in range(2)]
        b_kt = [[Buf("kt%d%d" % (i, j)) for j in range(nparts)] for i in range(2)]
        b_qt = [[Buf("qt%d%d" % (i, j)) for j in range(nparts)] for i in range(2)]
        b_kta = [Buf("kta%d" % i) for i in range(2)]
        b_qta = [Buf("qta%d" % i) for i in range(2)]
        b_v = [Buf("vt%d" % i) for i in range(2)]
        ps_s = [P("pss", [128, 4, 128], F32) for _ in range(2)]
        b_ps_s = [Buf("pss%d" % i) for i in range(2)]
        ps_o = [P("pso", [128, 128], F32) for _ in range(2)]
        b_ps_o = [Buf("pso%d" % i) for i in range(2)]
        pt = [T("pt", [128, 4, 128], BF16) for _ in range(3)]
        b_pt = [Buf("pt%d" % i) for i in range(3)]
        osb = [T("osb", [128, 128], BF16) for _ in range(2)]
        b_osb = [Buf("osb%d" % i) for i in range(2)]
        if kind == "sb":
            ps_b = [P("psb", [128, 4, 128], F32) for _ in range(2)]
            b_ps_b = [Buf("psb%d" % i) for i in range(2)]
            et = [T("et", [128, 4, 128], F32) for _ in range(2)]
            b_et = [Buf("et%d" % i) for i in range(2)]
            lb = [T("lb", [128, 4, 128], BF16) for _ in range(2)]
            b_lb = [Buf("lb%d" % i) for i in range(2)]
            rs = [T("rs", [128, 128], BF16) for _ in range(2)]
            b_rs = [Buf("rs%d" % i) for i in range(2)]
        else:
            ps_d = [P("psd", [128, 128], F32) for _ in range(2)]
            b_ps_d = [Buf("psd%d" % i) for i in range(2)]
            rd = [T("rd", [128, 128], F32) for _ in range(2)]
            b_rd = [Buf("rd%d" % i) for i in range(2)]
        if kind == "mla":
            rows = [128, 64]
        elif kind == "fox":
            rows = [68]
        else:
            rows = [64]
        cnt = {"g": 0, "o": 0, "pt": 0, "rs": 0}
        def do_head(h):
            hb = h % 2
            if kind == "mla":
                ksrc = [sc["mlaKTn"][h], sc["mlaKTr"][h]]
                qsrc = [sc["mlaQTn"][h][:, 0:So], sc["mlaQTr"][h][:, 0:So]]
                knames, qnames = ["mlaKTn", "mlaKTr"], ["mlaQTn", "mlaQTr"]
                vsrc, vname = sc["mlaV"][:, h * 128:(h + 1) * 128], "mlaV"
            else:
                pre = "fox" if kind == "fox" else "sb"
                ksrc = [sc[pre + "KT"][h // 2, (h % 2) * 64:(h % 2) * 64 + 64, :]]
                qsrc = [sc[pre + "QT"][h // 2, (h % 2) * 64:(h % 2) * 64 + 64, 0:So]]
                knames, qnames = [pre + "KT"], [pre + "QT"]
                vsrc, vname = sc[pre + "V"][:, h * 64:(h + 1) * 64], pre + "V"
            for p in range(nparts):
                r = min(rows[p], 128 if kind == "mla" else 64)
                sch.dma("sp", lambda e, p=p, r=r: e.dma_start(out=KT[hb][p][0:r, :], in_=ksrc[p]),
                        reads=[c.b_sc[knames[p]]], writes=[b_kt[hb][p]])
                sch.dma("sp", lambda e, p=p, r=r: e.dma_start(out=QT[hb][p][0:r, :], in_=qsrc[p]),
                        reads=[c.b_sc[qnames[p]]], writes=[b_qt[hb][p]])
            kr_, qr_ = [b_kt[hb][p] for p in range(nparts)], [b_qt[hb][p] for p in range(nparts)]
            if kind == "fox":
                sch.dma("sp", lambda e: e.dma_start(out=KT[hb][0][64:68, :], in_=sc["foxKa"][h * 4:(h + 1) * 4, :]),
                        reads=[c.b_sc["foxKa"]], writes=[b_kta[hb]])
                sch.dma("sp", lambda e: e.dma_start(out=QT[hb][0][64:68, :], in_=sc["foxQa"][h * 4:(h + 1) * 4, 0:So]),
                        reads=[c.b_sc["foxQa"]], writes=[b_qta[hb]])
                kr_, qr_ = kr_ + [b_kta[hb]], qr_ + [b_qta[hb]]
            sch.dma("pool", lambda e: e.dma_start(out=Vt[hb][:], in_=vsrc.rearrange("(b p) d -> p b d", p=128)),
                    reads=[c.b_sc[vname]], writes=[b_v[hb]])

            def qk(psum_ap, kb, j, first=True, last=True):
                for p in range(nparts):
                    r = rows[p]
                    sch.op("pe", lambda e, p=p, r=r: e.matmul(
                        psum_ap, KT[hb][p][0:r, kb * 128:(kb + 1) * 128], QT[hb][p][0:r, j * 128:(j + 1) * 128],
                        start=(first and p == 0), stop=(last and p == nparts - 1)), reads=kr_ + qr_, writes=[])

            def do_q(j):
                jp = j % 2
                if c.full:
                    i = j
                    mlist = [(i, c.mask_incl[:], c.mask_strict[:])]
                else:
                    i = 2 * j + 1
                    mlist = [(i - 1 + wch, c.maskt[:, 0 * 4 + jp * 2 + wch, :], c.maskt[:, 1 * 4 + jp * 2 + wch, :])
                             for wch in range(2)]
                oi = cnt["o"] % 2
                cnt["o"] += 1
                groups = [list(range(g0, min(g0 + 4, i + 1))) for g0 in range(0, i + 1, 4)]
                if kind == "sb":
                    groups = [list(reversed(g)) for g in reversed(groups)]
                st = {"nproc": 0}

                def do_grp(grp):
                    gi = cnt["g"] % 2
                    cnt["g"] += 1
                    pi = cnt["pt"] % 3
                    cnt["pt"] += 1
                    n = len(grp)
                    for sl, kb in enumerate(grp):
                        for p in range(nparts):
                            r = rows[p]
                            sch.op("pe", lambda e, p=p, r=r, sl=sl, kb=kb: e.matmul(
                                ps_s[gi][0:128, sl, :], KT[hb][p][0:r, kb * 128:(kb + 1) * 128],
                                QT[hb][p][0:r, j * 128:(j + 1) * 128], start=(p == 0), stop=(p == nparts - 1)),
                                reads=kr_ + qr_, writes=[b_ps_s[gi]])
                    if kind != "sb":
                        sch.op("act", lambda e, n=n, gi=gi, pi=pi: e.activation(out=pt[pi][:, 0:n, :], in_=ps_s[gi][:, 0:n, :],
                                                                                func=AF.Exp, scale=scale),
                               reads=[b_ps_s[gi]], writes=[b_pt[pi]])
                        if i in grp:
                            for (mkb, m_incl, m_strict) in mlist:
                                sl = grp.index(mkb)
                                sch.op("dve", lambda e, sl=sl, pi=pi, m_incl=m_incl: e.tensor_tensor(
                                    out=pt[pi][:, sl, :], in0=pt[pi][:, sl, :], in1=m_incl, op=ALU.mult),
                                    reads=[b_pt[pi], c.b_const], writes=[b_pt[pi]])
                        for sl, kb in enumerate(grp):
                            sch.op("pe", lambda e, sl=sl, kb=kb, pi=pi, oi=oi: e.matmul(
                                ps_o[oi][0:dv, :], Vt[hb][:, kb, :], pt[pi][:, sl, :], start=(kb == 0), stop=(kb == i)),
                                reads=[b_v[hb], b_pt[pi]], writes=[b_ps_o[oi]])
                            sch.op("pe", lambda e, sl=sl, kb=kb, pi=pi, oi=oi: e.matmul(
                                ps_d[oi][0:dv, :], c.ones_bf[:, 0:dv], pt[pi][:, sl, :], start=(kb == 0), stop=(kb == i)),
                                reads=[c.b_const, b_pt[pi]], writes=[b_ps_d[oi]])
                    else:
                        ei = gi
                        sch.op("act", lambda e, n=n, gi=gi: e.activation(out=et[gi][:, 0:n, :], in_=ps_s[gi][:, 0:n, :],
                                                                         func=AF.Exp, scale=scale),
                               reads=[b_ps_s[gi]], writes=[b_et[gi]])
                        sch.op("act", lambda e, n=n, gi=gi: e.activation(out=lb[gi][:, 0:n, :], in_=et[gi][:, 0:n, :],
                                                                         func=AF.Ln, bias=1.0),
                               reads=[b_et[gi]], writes=[b_lb[gi]])
                        if i in grp:
                            for (mkb, m_incl, m_strict) in mlist:
                                sl = grp.index(mkb)
                                sch.op("dve", lambda e, sl=sl, gi=gi, m_strict=m_strict: e.tensor_tensor(
                                    out=lb[gi][:, sl, :], in0=lb[gi][:, sl, :], in1=m_strict, op=ALU.mult),
                                    reads=[b_lb[gi], c.b_const], writes=[b_lb[gi]])
                        for sl, kb in enumerate(grp):
                            first = (st["nproc"] == 0)
                            ri = cnt["rs"] % 2
                            for p in range(nparts):
                                r = rows[p]
                                sch.op("pe", lambda e, p=p, r=r, sl=sl, kb=kb: e.matmul(
                                    ps_b[gi][0:128, sl, :], KT[hb][p][0:r, kb * 128:(kb + 1) * 128],
                                    QT[hb][p][0:r, j * 128:(j + 1) * 128], start=True, stop=False),
                                    reads=kr_ + qr_, writes=[b_ps_b[gi]])
                            sch.op("pe", lambda e, sl=sl, first=first: e.matmul(ps_b[gi][0:128, sl, :], c.ntril8[:], lb[gi][:, sl, :],
                                                                   start=False, stop=first),
                                   reads=[c.b_const, b_lb[gi]], writes=[b_ps_b[gi]])
                            if not first:
                                sch.op("pe", lambda e, sl=sl, ri=ri: e.matmul(ps_b[gi][0:128, sl, :], c.nones8[:], rs[ri][:],
                                                                              start=False, stop=True),
                                       reads=[c.b_const, b_rs[ri]], writes=[b_ps_b[gi]])
                            if first:
                                sch.op("dve", lambda e, sl=sl, gi=gi, ri=ri: e.tensor_copy(out=rs[1 - ri][:], in_=lb[gi][:, sl, :]),
                                       reads=[b_lb[gi]], writes=[b_rs[1 - ri]])
                            else:
                                sch.op("dve", lambda e, sl=sl, gi=gi, ri=ri: e.tensor_tensor(
                                    out=rs[1 - ri][:], in0=rs[ri][:], in1=lb[gi][:, sl, :], op=ALU.add),
                                    reads=[b_lb[gi], b_rs[ri]], writes=[b_rs[1 - ri]])
                            cnt["rs"] += 1
                            st["nproc"] += 1
                        sch.op("act", lambda e, n=n, gi=gi, pi=pi: e.activation(out=pt[pi][:, 0:n, :], in_=ps_b[gi][:, 0:n, :],
                                                                                func=AF.Exp, scale=scale),
                               reads=[b_ps_b[gi]], writes=[b_pt[pi]])
                        if i in grp:
                            for (mkb, m_incl, m_strict) in mlist:
                                sl = grp.index(mkb)
                                sch.op("dve", lambda e, sl=sl, pi=pi, m_strict=m_strict: e.tensor_tensor(
                                    out=pt[pi][:, sl, :], in0=pt[pi][:, sl, :], in1=m_strict, op=ALU.mult),
                                    reads=[b_pt[pi], c.b_const], writes=[b_pt[pi]])
                        for sl, kb in enumerate(grp):
                            sch.op("pe", lambda e, sl=sl, kb=kb, pi=pi, oi=oi: e.matmul(
                                ps_o[oi][0:dv, :], Vt[hb][:, kb, :], pt[pi][:, sl, :], start=(kb == i), stop=(kb == 0)),
                                reads=[b_v[hb], b_pt[pi]], writes=[b_ps_o[oi]])
                for grp in groups:
                    do_grp(grp)
                if kind != "sb":
                    sch.op("dve", lambda e, oi=oi: e.reciprocal(out=rd[oi][0:dv, :], in_=ps_d[oi][0:dv, :]),
                           reads=[b_ps_d[oi]], writes=[b_rd[oi]])
                    sch.op("dve", lambda e, oi=oi: e.tensor_tensor(out=osb[oi][0:dv, :], in0=ps_o[oi][0:dv, :],
                                                                   in1=rd[oi][0:dv, :], op=ALU.mult),
                           reads=[b_ps_o[oi], b_rd[oi]], writes=[b_osb[oi]])
                else:
                    sch.op("act", lambda e, oi=oi: e.copy(out=osb[oi][0:dv, :], in_=ps_o[oi][0:dv, :]),
                           reads=[b_ps_o[oi]], writes=[b_osb[oi]])
                dst = "OT_" + kind
                sch.dma("sp", lambda e, oi=oi, j=j: e.dma_start(out=sc[dst][h * dv:(h + 1) * dv, j * 128:(j + 1) * 128],
                                                                in_=osb[oi][0:dv, :]),
                        reads=[b_osb[oi]], writes=[c.b_sc[dst]])
            for j in range(c.NBo):
                do_q(j)

        for h in range(H):
            do_head(h)
        sch.barrier()
        sch.emit()


def phase_mla_up(c, sch, L):
    W, sc = c.w, c.sc

    def setup_q(env):
        env.g_q = load_bc(sch, env, "g_mq", W["mla_q_norm"][L], 192)
        env.qf = [env.T("mqf%d" % i, [128, 384], F32) for i in range(2)]
        env.b_qf = [Buf("mqf%d" % i) for i in range(2)]
        env.qn = 0

    def post_q(env, tag, ci, tb, nl, ps, b_ps):
        tok = slice(nl * 128, (nl + 1) * 128)
        qi = env.qn % 2
        env.qn += 1
        qf, b_qf = env.qf[qi], env.b_qf[qi]
        headnorm(c, sch, env, ps, b_ps, 2, 192, env.g_q[0], env.g_q[1], qf[:, 0:384], b_qf)
        ob, b_ob = stage(env, "b")
        v3 = qf[:, 0:384].rearrange("p (h d) -> p h d", d=192)
        o3 = ob[:, 0:384].rearrange("p (h d) -> p h d", d=192)
        sch.op("act", lambda e: e.copy(out=o3[:, :, 0:128], in_=v3[:, :, 0:128]), reads=[b_qf], writes=[b_ob])
        rope_ops(c, sch, env, v3[:, :, 128:192], o3[:, :, 128:192], 2, nl, b_qf, b_ob, own=True)
        for hh in range(2):
            h = ci * 2 + hh
            transpose_out(c, sch, env, ob[:, hh * 192:hh * 192 + 128], 128,
                          lambda h=h: (sc["mlaQTn"][h, :, tok], c.b_sc["mlaQTn"]), b_ob)
            transpose_out(c, sch, env, ob[:, hh * 192 + 128:hh * 192 + 192], 64,
                          lambda h=h: (sc["mlaQTr"][h, :, tok], c.b_sc["mlaQTr"]), b_ob)

    chunks = [(i * 384, 384, "mq") for i in range(8)]
    phase_linear(c, sch, sc["cqT"], 4, W["mla_w_uq"][L], chunks, list(range(c.NBo)), post_q, setup_q)

    def setup_k(env):
        env.g_k = load_bc(sch, env, "g_mk", W["mla_k_norm"][L], 192)
        env.krt = [env.T("krt%d" % i, [128, 64], F32) for i in range(2)]
        env.b_krt = [Buf("krt%d" % i) for i in range(2)]
        env.krr = [env.T("krr%d" % i, [128, 64], F32) for i in range(2)]
        env.sm = env.T("mks", [128, 16], F32)
        env.b_sm = Buf("mks")
        env.kf = env.T("mkf", [128, 256], F32)
        env.b_kf = Buf("mkf")
        env.kn = 0
        env.last_tb = None

    def post_k(env, tag, ci, tb, nl, ps, b_ps):
        tok = slice(tb * 128, (tb + 1) * 128)
        ki = env.kn % 2
        env.kn += 1
        krt, krr, b_krt = env.krt[ki], env.krr[ki], env.b_krt[ki]
        sm, b_sm = env.sm, env.b_sm
        sch.dma("sp", lambda e: e.dma_start(out=krt[:], in_=sc["kr"][tok, :]), reads=[c.b_sc["kr"]], writes=[b_krt])
        sch.op("dve", lambda e: e.tensor_tensor(out=krr[:], in0=krt[:], in1=krt[:], op=ALU.mult), reads=[b_krt], writes=[b_krt])
        sch.op("dve", lambda e: e.tensor_reduce(out=sm[:, 0:1], in_=krr[:], axis=AX.X, op=ALU.add), reads=[b_krt], writes=[b_sm])
        sch.op("dve", lambda e: e.tensor_tensor(out=krt[:], in0=krt[:], in1=env.g_k[0][:, 128:192], op=ALU.mult),
               reads=[b_krt, env.g_k[1]], writes=[b_krt])
        rope_ops(c, sch, env, krt[:].rearrange("p (h d) -> p h d", h=1), krr[:].rearrange("p (h d) -> p h d", h=1), 1, tb,
                 b_krt, b_krt)
        kf, b_kf = env.kf, env.b_kf
        p3 = ps[:, 0:512].rearrange("p (h d) -> p h d", d=256)
        k3 = kf[:, 0:256].rearrange("p (h d) -> p h d", d=128)
        sch.op("act", lambda e: e.activation(out=k3, in_=p3[:, :, 0:128], func=AF.Square), reads=[b_ps], writes=[b_kf])
        sch.op("dve", lambda e: e.tensor_reduce(out=sm[:, 1:3], in_=k3, axis=AX.X, op=ALU.add), reads=[b_kf], writes=[b_sm])
        sch.op("dve", lambda e: e.tensor_tensor(out=sm[:, 1:3], in0=sm[:, 1:3], in1=sm[:, 0:1].to_broadcast([128, 2]), op=ALU.add),
               reads=[b_sm], writes=[b_sm])
        rstd_ops(sch, sm[:, 1:3], sm[:, 4:6], sm[:, 8:10], 192, b_sm)
        sch.op("dve", lambda e: e.tensor_tensor(out=k3, in0=p3[:, :, 0:128], in1=sm[:, 8:10].unsqueeze(2).to_broadcast([128, 2, 128]),
                                                op=ALU.mult), reads=[b_ps, b_sm, b_kf], writes=[b_kf])
        ob, b_ob = stage(env, "b")
        o3 = ob[:, 0:512].rearrange("p (h d) -> p h d", d=256)
        sch.op("dve", lambda e: e.tensor_tensor(out=o3[:, :, 0:128], in0=k3,
                                                in1=env.g_k[0][:, 0:128].unsqueeze(1).to_broadcast([128, 2, 128]), op=ALU.mult),
               reads=[b_kf, env.g_k[1]], writes=[b_ob])
        sch.op("act", lambda e: e.copy(out=o3[:, :, 128:256], in_=p3[:, :, 128:256]), reads=[b_ps], writes=[b_ob])
        ob2, b_ob2 = stage(env, "b")
        sch.op("dve", lambda e: e.tensor_tensor(out=ob2[:, 0:128].rearrange("p (h d) -> p h d", d=64),
                                                in0=krr[:].unsqueeze(1).to_broadcast([128, 2, 64]),
                                                in1=sm[:, 8:10].unsqueeze(2).to_broadcast([128, 2, 64]), op=ALU.mult),
               reads=[b_krt, b_sm], writes=[b_ob2])
        for hh in range(2):
            h = ci * 2 + hh
            transpose_out(c, sch, env, ob[:, hh * 256:hh * 256 + 128], 128,
                          lambda h=h: (sc["mlaKTn"][h, :, tok], c.b_sc["mlaKTn"]), b_ob)
            transpose_out(c, sch, env, ob2[:, hh * 64:(hh + 1) * 64], 64,
                          lambda h=h: (sc["mlaKTr"][h, :, tok], c.b_sc["mlaKTr"]), b_ob2)
            sch.dma("sp", lambda e, h=h, hh=hh: e.dma_start(out=sc["mlaV"][tok, h * 128:(h + 1) * 128],
                                                            in_=ob[:, hh * 256 + 128:hh * 256 + 256]),
                    reads=[b_ob], writes=[c.b_sc["mlaV"]])

    chunks = [(i * 512, 512, "mkv") for i in range(8)]
    phase_linear(c, sch, sc["ckvT"], 4, W["mla_w_ukv"][L], chunks, list(range(c.NB)), post_k, setup_k)


def phase_foxcum(c, sch):
    nc, sc, NB = c.nc, c.sc, c.NB
    with contextlib.ExitStack() as es:
        T = lambda name, shape, dt: es.enter_context(nc.sbuf_tensor(uname(name), shape, dt))
        lf = T("lf", [128, NB, 16], F32)
        cum = T("cum", [128, NB, 16], F32)
        pre = T("pre", [128, NB, 16], F32)
        hi = T("hi", [128, NB, 16], BF16)
        hif = T("hif", [128, NB, 16], F32)
        lo = T("lo", [128, NB, 16], BF16)
        aug = [T("aug%d" % i, [128, 16, 4], BF16) for i in range(2)]
        b_aug = [Buf("aug%d" % i) for i in range(2)]
        ps1 = es.enter_context(nc.psum_tensor(uname("cps1"), [128, NB * 16], F32))
        ps2 = es.enter_context(nc.psum_tensor(uname("cps2"), [128, NB * 16], F32))
        b = Buf("cumall")
        seltmp = T("seltmp", [128, 16], BF16)
        b_st = Buf("seltmp")
        b_p1, b_p2 = Buf("cps1"), Buf("cps2")
        env = Ctx()
        env.T, env.es, env.nc = T, es, nc
        sch.dma("sp", lambda e: e.dma_start(out=lf[:], in_=sc["lf"].rearrange("(b p) h -> p b h", p=128)),
                reads=[c.b_sc["lf"]], writes=[b])
        lf2 = lf[:].rearrange("p b h -> p (b h)")
        sch.op("pe", lambda e: e.matmul(ps1[:], c.triu_f[:], lf2, start=True, stop=True), reads=[b, c.b_const], writes=[b_p1])
        sch.op("pe", lambda e: e.matmul(ps2[:], c.ones_f[:], lf2, start=True, stop=True), reads=[b, c.b_const], writes=[b_p2])
        sch.op("dve", lambda e: e.memset(pre[:, 0, :], 0.0), writes=[b])
        p2 = ps2[:].rearrange("p (b h) -> p b h", h=16)
        for bb in range(1, NB):
            sch.op("dve", lambda e, bb=bb: e.tensor_tensor(out=pre[:, bb, :], in0=pre[:, bb - 1, :], in1=p2[:, bb - 1, :], op=ALU.add),
                   reads=[b, b_p2], writes=[b])
        sch.op("dve", lambda e: e.tensor_tensor(out=cum[:], in0=pre[:], in1=ps1[:].rearrange("p (b h) -> p b h", h=16), op=ALU.add),
               reads=[b, b_p1], writes=[b])
        sch.op("dve", lambda e: e.tensor_scalar(out=cum[:], in0=cum[:], scalar1=8.0, scalar2=None, op0=ALU.mult), reads=[b], writes=[b])
        sch.op("dve", lambda e: e.tensor_copy(out=hi[:], in_=cum[:]), reads=[b], writes=[b])
        sch.op("dve", lambda e: e.tensor_copy(out=hif[:], in_=hi[:]), reads=[b], writes=[b])
        sch.op("dve", lambda e: e.tensor_tensor(out=hif[:], in0=cum[:], in1=hif[:], op=ALU.subtract), reads=[b], writes=[b])
        sch.op("dve", lambda e: e.tensor_copy(out=lo[:], in_=hif[:]), reads=[b], writes=[b])
        n = 0
        for side in ("k", "q"):
            blks = list(range(NB)) if side == "k" else list(range(c.NBo))
            for nl, tb in enumerate(blks):
                ai = n % 2
                n += 1
                a, b_a = aug[ai], b_aug[ai]
                if side == "q" and c.full:
                    sch.op("dve", lambda e, a=a, tb=tb: e.tensor_copy(out=a[:, :, 0], in_=hi[:, tb, :]), reads=[b], writes=[b_a])
                    sch.op("dve", lambda e, a=a, tb=tb: e.tensor_copy(out=a[:, :, 1], in_=lo[:, tb, :]), reads=[b], writes=[b_a])
                    sch.op("dve", lambda e, a=a: e.memset(a[:, :, 2:4], 1.0), writes=[b_a])
                    dst, tok = "foxQa", slice(nl * 128, (nl + 1) * 128)
                elif side == "q":
                    sa, sb_ = c.selt[:, 2 * (nl % 2):2 * (nl % 2) + 1], c.selt[:, 2 * (nl % 2) + 1:2 * (nl % 2) + 2]
                    for col, src in ((0, hi), (1, lo)):
                        sch.op("dve", lambda e, src=src, nl=nl, sa=sa: e.tensor_scalar(out=seltmp[:], in0=src[:, 2 * nl, :], scalar1=sa,
                                                                                      scalar2=None, op0=ALU.mult),
                               reads=[b, c.b_const], writes=[b_st])
                        sch.op("dve", lambda e, src=src, nl=nl, sb_=sb_, a=a, col=col: e.scalar_tensor_tensor(
                            out=a[:, :, col], in0=src[:, 2 * nl + 1, :], scalar=sb_, in1=seltmp[:], op0=ALU.mult, op1=ALU.add),
                            reads=[b, b_st, c.b_const], writes=[b_a])
                    sch.op("dve", lambda e, a=a: e.memset(a[:, :, 2:4], 1.0), writes=[b_a])
                    dst, tok = "foxQa", slice(nl * 128, (nl + 1) * 128)
                else:
                    sch.op("dve", lambda e, a=a: e.memset(a[:, :, 0:2], 1.0), writes=[b_a])
                    sch.op("dve", lambda e, a=a, tb=tb: e.tensor_scalar(out=a[:, :, 2], in0=hi[:, tb, :], scalar1=-1.0, scalar2=None,
                                                                        op0=ALU.mult), reads=[b], writes=[b_a])
                    sch.op("dve", lambda e, a=a, tb=tb: e.tensor_scalar(out=a[:, :, 3], in0=lo[:, tb, :], scalar1=-1.0, scalar2=None,
                                                                        op0=ALU.mult), reads=[b], writes=[b_a])
                    dst, tok = "foxKa", slice(tb * 128, (tb + 1) * 128)
                transpose_out(c, sch, env, a[:].rearrange("p h j -> p (h j)"), 64,
                              lambda dst=dst, tok=tok: (sc[dst][:, tok], c.b_sc[dst]), b_a)
        sch.barrier()
        sch.emit()


def phase_merge_out(c, sch, L, x_ap):
    nc, sc, W = c.nc, c.sc, c.w
    NBo = c.NBo
    TT = 4
    with contextlib.ExitStack() as es:
        T = lambda name, shape, dt: es.enter_context(nc.sbuf_tensor(uname(name), shape, dt))
        P = lambda name, shape, dt: es.enter_context(nc.psum_tensor(uname(name), shape, dt))
        brs = [("mla", 16), ("sb", 8), ("fox", 8)]
        ot = [T("ot" + n, [128, kc, TT * 128], BF16) for n, kc in brs]
        b_ot = [Buf("ot" + n) for n, kc in brs]
        wb = [[T("wb" + n, [128, kc, 512], BF16) for n, kc in brs] for _ in range(2)]
        b_wb = [[Buf("wb%d%s" % (i, n)) for n, kc in brs] for i in range(2)]
        mT = T("mT", [128, 16, TT * 128], BF16)
        b_mT = Buf("mT")
        wo = [T("wo", [128, 16, 512], BF16) for _ in range(2)]
        b_wo = [Buf("wo%d" % i) for i in range(2)]
        gt = [T("gt", [128, 3, 512], BF16) for _ in range(2)]
        b_gt = [Buf("gt%d" % i) for i in range(2)]
        t0 = [T("mt0", [128, 512], F32) for _ in range(2)]
        t1 = [T("mt1", [128, 512], F32) for _ in range(2)]
        b_t0 = [Buf("mt0%d" % i) for i in range(2)]
        b_t1 = [Buf("mt1%d" % i) for i in range(2)]
        mb = [T("mb", [128, 512], BF16) for _ in range(2)]
        b_mb = [Buf("mb%d" % i) for i in range(2)]
        xo = [T("xo", [128, 512], F32) for _ in range(2)]
        b_xo = [Buf("xo%d" % i) for i in range(2)]
        psb = [P("psbr", [128, 512], F32) for _ in range(3)]
        b_psb = [Buf("psbr%d" % i) for i in range(3)]
        pst = [P("pstr", [128, 4, 128], BF16) for _ in range(2)]
        b_pst = [Buf("pstr%d" % i) for i in range(2)]
        pso = [P("psout", [128, 512], F32) for _ in range(2)]
        b_pso = [Buf("psout%d" % i) for i in range(2)]
        wsrc = [W["w_branch_mla"][L], W["w_branch_sb"][L], W["w_branch_fox"][L]]
        wv = [w.rearrange("(k p) n -> p k n", p=128) for w in wsrc]
        wov = W["w_out"][L].rearrange("(k p) n -> p k n", p=128)
        ctr = {"w": 0, "g": 0, "t": 0, "o": 0, "x": 0}

        def do_tile(tl0):
            nt = min(TT, NBo - tl0)
            tsl = slice(tl0 * 128, (tl0 + nt) * 128)
            for bi, (n, kc) in enumerate(brs):
                sch.dma("sp", lambda e, bi=bi, n=n: e.dma_start(
                    out=ot[bi][:, :, 0:nt * 128], in_=sc["OT_" + n][:, tsl].rearrange("(k p) t -> p k t", p=128)),
                    reads=[c.b_sc["OT_" + n]], writes=[b_ot[bi]])
            for cc in range(4):
                wi = ctr["w"] % 2
                ctr["w"] += 1
                for bi in range(3):
                    sch.dma("pool", lambda e, bi=bi, wi=wi, cc=cc: e.dma_start(out=wb[wi][bi][:], in_=wv[bi][:, :, cc * 512:(cc + 1) * 512]),
                            writes=[b_wb[wi][bi]])
                for j in range(nt):
                    gi = ctr["g"] % 2
                    ctr["g"] += 1
                    tok = slice((tl0 + j) * 128, (tl0 + j + 1) * 128)
                    sch.dma("sp", lambda e, gi=gi, tok=tok, cc=cc: e.dma_start(
                        out=gt[gi][:], in_=sc["gates"][tok, :].rearrange("t (b n) -> t b n", b=3)[:, :, cc * 512:(cc + 1) * 512]),
                        reads=[c.b_sc["gates"]], writes=[b_gt[gi]])
                    for bi, (n, kc) in enumerate(brs):
                        for k in range(kc):
                            sch.op("pe", lambda e, bi=bi, k=k, kc=kc, j=j, wi=wi: e.matmul(
                                psb[bi][:], ot[bi][:, k, j * 128:(j + 1) * 128], wb[wi][bi][:, k, :],
                                start=(k == 0), stop=(k == kc - 1)), reads=[b_ot[bi], b_wb[wi][bi]], writes=[b_psb[bi]])
                    sch.op("dve", lambda e, gi=gi: e.tensor_tensor(out=t0[gi][:], in0=psb[0][:], in1=gt[gi][:, 0, :], op=ALU.mult),
                           reads=[b_psb[0], b_gt[gi]], writes=[b_t0[gi]])
                    sch.op("dve", lambda e, gi=gi: e.tensor_tensor(out=t1[gi][:], in0=psb[1][:], in1=gt[gi][:, 1, :], op=ALU.mult),
                           reads=[b_psb[1], b_gt[gi]], writes=[b_t1[gi]])
                    sch.op("pool", lambda e, gi=gi: e.tensor_tensor(out=t0[gi][:], in0=t0[gi][:], in1=t1[gi][:], op=ALU.add),
                           reads=[b_t0[gi], b_t1[gi]], writes=[b_t0[gi]])
                    sch.op("dve", lambda e, gi=gi: e.tensor_tensor(out=t1[gi][:], in0=psb[2][:], in1=gt[gi][:, 2, :], op=ALU.mult),
                           reads=[b_psb[2], b_gt[gi]], writes=[b_t1[gi]])
                    sch.op("pool", lambda e, gi=gi: e.tensor_tensor(out=mb[gi][:], in0=t0[gi][:], in1=t1[gi][:], op=ALU.add),
                           reads=[b_t0[gi], b_t1[gi]], writes=[b_mb[gi]])
                    ti = ctr["t"] % 2
                    ctr["t"] += 1
                    for q in range(4):
                        sch.op("pe", lambda e, gi=gi, q=q, ti=ti: e.transpose(out=pst[ti][:, q, :], in_=mb[gi][:, q * 128:(q + 1) * 128],
                                                                              identity=c.ident[:]),
                               reads=[b_mb[gi], c.b_const], writes=[b_pst[ti]])
                    sch.op("act", lambda e, ti=ti, cc=cc, j=j: e.copy(out=mT[:, cc * 4:(cc + 1) * 4, j * 128:(j + 1) * 128], in_=pst[ti][:]),
                           reads=[b_pst[ti]], writes=[b_mT])
            for oc in range(4):
                wi = ctr["o"] % 2
                ctr["o"] += 1
                sch.dma("pool", lambda e, wi=wi, oc=oc: e.dma_start(out=wo[wi][:], in_=wov[:, :, oc * 512:(oc + 1) * 512]),
                        writes=[b_wo[wi]])
                for j in range(nt):
                    xi = ctr["x"] % 2
                    ctr["x"] += 1
                    gb = tl0 + j
                    tok = slice((tl0 + j) * 128, (tl0 + j + 1) * 128)
                    sch.dma("sp", lambda e, xi=xi, gb=gb, oc=oc: e.dma_start(out=xo[xi][:], in_=x_ap[gb * 128:(gb + 1) * 128, oc * 512:(oc + 1) * 512]),
                            writes=[b_xo[xi]])
                    for k in range(16):
                        sch.op("pe", lambda e, xi=xi, k=k, j=j, wi=wi: e.matmul(
                            pso[xi][:], mT[:, k, j * 128:(j + 1) * 128], wo[wi][:, k, :], start=(k == 0), stop=(k == 15)),
                            reads=[b_mT, b_wo[wi]], writes=[b_pso[xi]])
                    sch.op("dve", lambda e, xi=xi: e.tensor_tensor(out=xo[xi][:], in0=pso[xi][:], in1=xo[xi][:], op=ALU.add),
                           reads=[b_pso[xi], b_xo[xi]], writes=[b_xo[xi]])
                    sch.dma("sp", lambda e, xi=xi, tok=tok, oc=oc: e.dma_start(out=sc["xmid"][tok, oc * 512:(oc + 1) * 512], in_=xo[xi][:]),
                            reads=[b_xo[xi]], writes=[c.b_sc["xmid"]])
        for tl0 in range(0, NBo, TT):
            do_tile(tl0)
        sch.barrier()
        sch.emit()


def phase_moe(c, sch, L, out_ap, b_out):
    nc, sc, W = c.nc, c.sc, c.w
    NBo = c.NBo
    TT = 4
    with contextlib.ExitStack() as es:
        T = lambda name, shape, dt: es.enter_context(nc.sbuf_tensor(uname(name), shape, dt))
        P = lambda name, shape, dt: es.enter_context(nc.psum_tensor(uname(name), shape, dt))
        yacc = T("yacc", [128, TT, D], F32)
        b_y = [Buf("yacc%d" % i) for i in range(TT)]
        sq = T("msq", [128, D], F32)
        b_sq = Buf("msq")
        hf = T("mhf", [128, D], F32)
        b_hf = Buf("mhf")
        gbc = T("mgbc", [128, D], F32)
        b_g = Buf("mgbc")
        hT = T("mhT", [128, KD, TT * 128], BF16)
        b_hT = Buf("mhT")
        hhi = T("mhhi", [128, D], BF16)
        hlo = T("mhlo", [128, D], BF16)
        hloT = T("mhloT", [128, KD, 128], BF16)
        b_hhi, b_hlo, b_hloT = Buf("mhhi"), Buf("mhlo"), Buf("mhloT")
        w72 = T("mw72", [128, KD, 72], F32)
        whi = T("mwhi", [128, KD, 72], BF16)
        wlo = T("mwlo", [128, KD, 72], BF16)
        wr = T("mwr", [128, KD, 64], F32)
        wgp = T("mwgp", [128, KD, 8], F32)
        b_wr = Buf("mwr")
        b_wgp = Buf("mwgp")
        sm = T("msm", [128, 512], F32)
        b_sm = Buf("msm")
        coef = T("mcoef", [128, TT, 64], F32)
        b_coef = Buf("mcoef")
        wg = [T("mwg", [128, KD, 512], BF16) for _ in range(1)]
        wu = [T("mwu", [128, KD, 512], BF16) for _ in range(1)]
        wd = [T("mwd", [128, 4, D], BF16) for _ in range(1)]
        b_wg = [Buf("mwg%d" % i) for i in range(2)]
        b_wu = [Buf("mwu%d" % i) for i in range(2)]
        b_wd = [Buf("mwd%d" % i) for i in range(2)]
        aT = [T("maT", [128, 4, TT * 128], BF16) for _ in range(2)]
        b_aT = [Buf("maT%d" % i) for i in range(2)]
        sg = [T("msg", [128, TT * 128], F32) for _ in range(2)]
        b_sg = [Buf("msg%d" % i) for i in range(2)]
        pt = [P("mpt", [128, 4, 128], BF16) for _ in range(1)]
        b_pt = [Buf("mpt%d" % i) for i in range(1)]
        pr = P("mpr", [128, 72], F32)
        b_pr = Buf("mpr")
        pg = [P("mpg", [128, TT * 128], F32) for _ in range(2)]
        pu = [P("mpu", [128, TT * 128], F32) for _ in range(2)]
        b_pg = [Buf("mpg%d" % i) for i in range(2)]
        b_pu = [Buf("mpu%d" % i) for i in range(2)]
        pd = [P("mpd", [128, 512], F32) for _ in range(2)]
        b_pd = [Buf("mpd%d" % i) for i in range(2)]
        ctr = {"t": 0, "w": 0, "m": 0, "d": 0}
        sch.dma("sp", lambda e: e.dma_start(out=gbc[:], in_=W["norm_ffn"][L].partition_broadcast(128)), writes=[b_g])
        import os
        if int(os.environ.get("ROUTE_STAGE", 9)) >= 1:
            sch.dma("pool", lambda e: e.dma_start(out=wgp[:], in_=W["w_group"][L].rearrange("(k p) n -> p k n", p=128)), writes=[b_wgp])
        if int(os.environ.get("ROUTE_STAGE", 9)) >= 1:
            sch.dma("pool", lambda e: e.dma_start(out=wr[:], in_=W["w_router"][L].rearrange("(k p) n -> p k n", p=128)), writes=[b_wr])
        sch.op("dve", lambda e: e.tensor_copy(out=w72[:, :, 0:8], in_=wgp[:]), reads=[b_wgp], writes=[b_wr])
        sch.op("dve", lambda e: e.tensor_copy(out=w72[:, :, 8:72], in_=wr[:]), reads=[b_wr], writes=[b_wr])
        sch.op("dve", lambda e: e.tensor_copy(out=whi[:], in_=w72[:]), reads=[b_wr], writes=[b_wr])
        sch.op("dve", lambda e: e.tensor_tensor(out=wlo[:], in0=w72[:], in1=whi[:], op=ALU.subtract), reads=[b_wr], writes=[b_wr])
        S_ = lambda a, b: sm[:, a:b]

        def route(j, tl):
            tok = slice(tl * 128, (tl + 1) * 128)
            sch.dma("sp", lambda e: e.dma_start(out=yacc[:, j, :], in_=sc["xmid"][tok, :]), reads=[c.b_sc["xmid"]], writes=[b_y[j]])
            sch.op("act", lambda e: e.activation(out=sq[:], in_=yacc[:, j, :], func=AF.Square), reads=[b_y[j]], writes=[b_sq])
            sch.op("dve", lambda e: e.tensor_reduce(out=S_(0, 1), in_=sq[:], axis=AX.X, op=ALU.add), reads=[b_sq], writes=[b_sm])
            rstd_ops(sch, S_(0, 1), S_(1, 2), S_(2, 3), D, b_sm)
            sch.op("dve", lambda e: e.scalar_tensor_tensor(out=hf[:], in0=yacc[:, j, :], scalar=S_(2, 3), in1=gbc[:],
                                                           op0=ALU.mult, op1=ALU.mult), reads=[b_y[j], b_sm, b_g], writes=[b_hf])
            import os
            stage = int(os.environ.get("ROUTE_STAGE", 9))
            if stage < 2:
                return
            sch.op("dve", lambda e: e.tensor_copy(out=hhi[:], in_=hf[:]), reads=[b_hf], writes=[b_hhi])
            sch.op("dve", lambda e: e.tensor_tensor(out=hlo[:], in0=hf[:], in1=hhi[:], op=ALU.subtract),
                   reads=[b_hf, b_hhi], writes=[b_hlo])
            for src, b_src, which in ((hhi, b_hhi, 0), (hlo, b_hlo, 1)):
                for q in range(4):
                    ti = 0
                    for k4 in range(4):
                        k = q * 4 + k4
                        sch.op("pe", lambda e, k=k, k4=k4, ti=ti, src=src: e.transpose(
                            out=pt[ti][:, k4, :], in_=src[:, k * 128:(k + 1) * 128], identity=c.ident[:]),
                            reads=[b_src, c.b_const], writes=[b_pt[ti]])
                    if which == 0:
                        sch.op("act", lambda e, q=q, ti=ti: e.copy(out=hT[:, q * 4:(q + 1) * 4, j * 128:(j + 1) * 128], in_=pt[ti][:]),
                               reads=[b_pt[ti]], writes=[b_hT])
                    else:
                        sch.op("dve", lambda e, q=q, ti=ti: e.tensor_copy(out=hloT[:, q * 4:(q + 1) * 4, :], in_=pt[ti][:]),
                               reads=[b_pt[ti]], writes=[b_hloT])
            if stage < 3:
                return
            combos = [(0, whi), (0, wlo), (1, whi)]
            for ci_, (which, wt) in enumerate(combos):
                for k in range(KD):
                    lhs = hT[:, k, j * 128:(j + 1) * 128] if which == 0 else hloT[:, k, :]
                    sch.op("pe", lambda e, k=k, lhs=lhs, wt=wt, ci_=ci_: e.matmul(
                        pr[:], lhs, wt[:, k, :], start=(ci_ == 0 and k == 0), stop=(ci_ == 2 and k == KD - 1)),
                        reads=[b_hT, b_hloT, b_wr], writes=[b_pr])
            nmax = int(os.environ.get("ROUTE_NOPS", 999))
            cnt_ = {"n": 0}

            def V(fn, extra=()):
                cnt_["n"] += 1
                if cnt_["n"] <= nmax:
                    sch.op("dve", fn, reads=[b_sm] + list(extra), writes=[b_sm])

            def A(fn):
                cnt_["n"] += 1
                if cnt_["n"] <= nmax:
                    sch.op("act", fn, reads=[b_sm], writes=[b_sm])
            lg, lgE = S_(8, 80), S_(16, 80)
            if stage < 4:
                return
            V(lambda e: e.tensor_copy(out=lg, in_=pr[:]), [b_pr])
            V(lambda e: e.tensor_reduce(out=S_(3, 4), in_=S_(8, 16), axis=AX.X, op=ALU.max))
            V(lambda e: e.tensor_scalar(out=S_(80, 88), in0=S_(8, 16), scalar1=S_(3, 4), scalar2=None, op0=ALU.is_equal))
            V(lambda e: e.tensor_scalar(out=S_(4, 5), in0=S_(3, 4), scalar1=-1.0, scalar2=None, op0=ALU.mult))
            A(lambda e: e.activation(out=S_(88, 96), in_=S_(8, 16), func=AF.Exp, bias=S_(4, 5), scale=1.0))
            V(lambda e: e.tensor_reduce(out=S_(5, 6), in_=S_(88, 96), axis=AX.X, op=ALU.add))
            V(lambda e: e.reciprocal(out=S_(5, 6), in_=S_(5, 6)))
            V(lambda e: e.tensor_tensor(out=S_(96, 160).rearrange("p (g j) -> p g j", j=8),
                                        in0=lgE.rearrange("p (g j) -> p g j", j=8),
                                        in1=S_(80, 88).unsqueeze(2).to_broadcast([128, 8, 8]), op=ALU.mult))
            V(lambda e: e.tensor_reduce(out=S_(160, 168), in_=S_(96, 160).rearrange("p (g j) -> p j g", j=8), axis=AX.X, op=ALU.add))
            V(lambda e: e.tensor_reduce(out=S_(6, 7), in_=S_(160, 168), axis=AX.X, op=ALU.max))
            V(lambda e: e.tensor_scalar(out=S_(168, 176), in0=S_(160, 168), scalar1=S_(6, 7), scalar2=None, op0=ALU.is_equal))
            V(lambda e: e.scalar_tensor_tensor(out=S_(176, 184), in0=S_(168, 176), scalar=-1e30, in1=S_(160, 168),
                                               op0=ALU.mult, op1=ALU.add))
            V(lambda e: e.tensor_reduce(out=S_(7, 8), in_=S_(176, 184), axis=AX.X, op=ALU.max))
            V(lambda e: e.tensor_scalar(out=S_(184, 192), in0=S_(176, 184), scalar1=S_(7, 8), scalar2=None, op0=ALU.is_equal))
            V(lambda e: e.tensor_tensor(out=S_(192, 193), in0=S_(7, 8), in1=S_(6, 7), op=ALU.subtract))
            A(lambda e: e.activation(out=S_(193, 194), in_=S_(192, 193), func=AF.Exp))
            V(lambda e: e.tensor_scalar(out=S_(194, 195), in0=S_(193, 194), scalar1=1.0, scalar2=None, op0=ALU.add))
            V(lambda e: e.reciprocal(out=S_(194, 195), in_=S_(194, 195)))
            V(lambda e: e.tensor_tensor(out=S_(195, 196), in0=S_(193, 194), in1=S_(194, 195), op=ALU.mult))
            V(lambda e: e.tensor_tensor(out=S_(194, 195), in0=S_(194, 195), in1=S_(5, 6), op=ALU.mult))
            V(lambda e: e.tensor_tensor(out=S_(195, 196), in0=S_(195, 196), in1=S_(5, 6), op=ALU.mult))
            V(lambda e: e.tensor_scalar(out=S_(200, 208), in0=S_(168, 176), scalar1=S_(194, 195), scalar2=None, op0=ALU.mult))
            V(lambda e: e.scalar_tensor_tensor(out=S_(200, 208), in0=S_(184, 192), scalar=S_(195, 196), in1=S_(200, 208),
                                               op0=ALU.mult, op1=ALU.add))
            V(lambda e: e.tensor_copy(out=S_(208, 272).rearrange("p (g j) -> p g j", j=8),
                                      in_=S_(200, 208).unsqueeze(1).to_broadcast([128, 8, 8])))
            sch.op("dve", lambda e: e.tensor_tensor(out=coef[:, j, :].rearrange("p (g j) -> p g j", j=8),
                                                    in0=S_(208, 272).rearrange("p (g j) -> p g j", j=8),
                                                    in1=S_(80, 88).unsqueeze(2).to_broadcast([128, 8, 8]), op=ALU.mult),
                   reads=[b_sm], writes=[b_coef])

        def expert(e_, nt):
            wi = 0
            N = nt * 128
            sch.dma("pool", lambda e: e.dma_start(out=wg[wi][:], in_=W["w_e_gate"][L][e_].rearrange("(k p) n -> p k n", p=128)),
                    writes=[b_wg[wi]])
            sch.dma("pool", lambda e: e.dma_start(out=wu[wi][:], in_=W["w_e_up"][L][e_].rearrange("(k p) n -> p k n", p=128)),
                    writes=[b_wu[wi]])
            sch.dma("pool", lambda e: e.dma_start(out=wd[wi][:], in_=W["w_e_down"][L][e_].rearrange("(k p) n -> p k n", p=128)),
                    writes=[b_wd[wi]])
            ai = ctr["w"] % 2
            ctr["w"] += 1
            for m in range(4):
                mi = ctr["m"] % 2
                ctr["m"] += 1
                for k in range(KD):
                    sch.op("pe", lambda e, k=k, m=m, mi=mi: e.matmul(pg[mi][:, 0:N], wg[wi][:, k, m * 128:(m + 1) * 128], hT[:, k, 0:N],
                                                                     start=(k == 0), stop=(k == KD - 1)),
                           reads=[b_wg[wi], b_hT], writes=[b_pg[mi]])
                for k in range(KD):
                    sch.op("pe", lambda e, k=k, m=m, mi=mi: e.matmul(pu[mi][:, 0:N], wu[wi][:, k, m * 128:(m + 1) * 128], hT[:, k, 0:N],
                                                                     start=(k == 0), stop=(k == KD - 1)),
                           reads=[b_wu[wi], b_hT], writes=[b_pu[mi]])
                sch.op("act", lambda e, mi=mi: e.activation(out=sg[mi][:, 0:N], in_=pg[mi][:, 0:N], func=AF.Silu),
                       reads=[b_pg[mi]], writes=[b_sg[mi]])
                sch.op("dve", lambda e, mi=mi, m=m: e.tensor_tensor(out=aT[ai][:, m, 0:N], in0=pu[mi][:, 0:N], in1=sg[mi][:, 0:N], op=ALU.mult),
                       reads=[b_pu[mi], b_sg[mi]], writes=[b_aT[ai]])
            for j in range(nt):
                for n in range(4):
                    di = ctr["d"] % 2
                    ctr["d"] += 1
                    for cch in range(4):
                        sch.op("pe", lambda e, cch=cch, j=j, n=n, di=di: e.matmul(pd[di][:], aT[ai][:, cch, j * 128:(j + 1) * 128],
                                                                           wd[wi][:, cch, n * 512:(n + 1) * 512],
                                                                           start=(cch == 0), stop=(cch == 3)),
                               reads=[b_aT[ai], b_wd[wi]], writes=[b_pd[di]])
                    sch.op("dve", lambda e, j=j, n=n, di=di: e.scalar_tensor_tensor(
                        out=yacc[:, j, n * 512:(n + 1) * 512], in0=pd[di][:], scalar=coef[:, j, e_:e_ + 1],
                        in1=yacc[:, j, n * 512:(n + 1) * 512], op0=ALU.mult, op1=ALU.add),
                        reads=[b_pd[di], b_coef, b_y[j]], writes=[b_y[j]])

        def do_tile(tl0):
            nt = min(TT, NBo - tl0)
            for j in range(nt):
                route(j, tl0 + j)
            import os
            for e_ in range(int(os.environ.get("MOE_NE", 64))):
                expert(e_, nt)
            for j in range(nt):
                tl = tl0 + j
                sch.dma("sp", lambda e, j=j, tl=tl: e.dma_start(out=out_ap[tl * 128:(tl + 1) * 128, :], in_=yacc[:, j, :]),
                        reads=[b_y[j]], writes=[b_out])

        for tl0 in range(0, NBo, TT):
            do_tile(tl0)
        sch.barrier()
        sch.emit()


def setup_percore(c, sch, es, masks_ap, selv_ap):
    nc = c.nc
    c.maskt = es.enter_context(nc.sbuf_tensor(uname("maskt"), [128, 8, 128], BF16))
    c.selt = es.enter_context(nc.sbuf_tensor(uname("selt"), [128, 4], F32))
    b = Buf("percore", multi=True, persist=True)
    sch.dma("pool", lambda e: e.dma_start(out=c.maskt[:], in_=masks_ap.rearrange("m p q -> p m q")), writes=[b])
    sch.dma("sp", lambda e: e.dma_start(out=c.selt[:], in_=selv_ap.partition_broadcast(128)), writes=[b])
    sch.op("dve", lambda e: e.tensor_copy(out=c.selt[:], in_=c.selt[:]), reads=[b, c.b_const], writes=[c.b_const])


def percore_consts(par):
    q = np.arange(128)[None, :]
    p = np.arange(128)[:, None]
    tri = {0: (q >= p).astype(np.float32), 1: (q > p).astype(np.float32)}
    ones, zeros = np.ones((128, 128), np.float32), np.zeros((128, 128), np.float32)
    masks = np.zeros((2, 2, 2, 128, 128), np.float32)
    selv = np.zeros((4,), np.float32)
    for jp in range(2):
        first = (par == 0) == (jp == 0)
        selv[2 * jp], selv[2 * jp + 1] = (1.0, 0.0) if first else (0.0, 1.0)
        for kind in range(2):
            masks[kind, jp, 0] = tri[kind] if first else ones
            masks[kind, jp, 1] = zeros if first else tri[kind]
    return masks.reshape(8, 128, 128), selv


def layer(c, sch, L, xblk_all, x_own_ap, out_ap, b_out, full=False):
    c.full = full
    c.NBo = c.NB if full else c.NB // 2
    c.So = c.NBo * 128
    NBo = c.NBo
    phase_xnT(c, sch, xblk_all, c.w["norm_mix"][L], c.sc["xnT"], list(range(c.NB)), c.b_sc["xnT"])
    if not full:
        phase_xnT(c, sch, lambda tb: x_own_ap[tb * 128:(tb + 1) * 128, :], c.w["norm_mix"][L], c.sc["xnTo"],
                  list(range(NBo)), c.b_sc["xnTo"])
    phase_inproj(c, sch, L)
    phase_mla_up(c, sch, L)
    phase_foxcum(c, sch)
    for kind in ("mla", "sb", "fox"):
        phase_attn(c, sch, kind)
    phase_merge_out(c, sch, L, x_own_ap)
    import os
    if not os.environ.get("NOMOE"):
        phase_moe(c, sch, L, out_ap, b_out)


def phase_select_own(c, sch, xall_ap, xown_ap, b_in, b_out):
    nc = c.nc
    NBo = c.NB // 2
    with contextlib.ExitStack() as es:
        T = lambda name, shape, dt: es.enter_context(nc.sbuf_tensor(uname(name), shape, dt))
        ta = [T("sela", [128, D], F32) for _ in range(2)]
        tb_ = [T("selb", [128, D], F32) for _ in range(2)]
        b_a = [Buf("sela%d" % i) for i in range(2)]
        b_b = [Buf("selb%d" % i) for i in range(2)]
        for j in range(NBo):
            i = j % 2
            jp = j % 2
            sa, sb_ = c.selt[:, 2 * jp:2 * jp + 1], c.selt[:, 2 * jp + 1:2 * jp + 2]
            sch.dma("sp", lambda e, i=i, j=j: e.dma_start(out=ta[i][:], in_=xall_ap[(2 * j) * 128:(2 * j + 1) * 128, :]),
                    reads=[b_in], writes=[b_a[i]])
            sch.dma("sp", lambda e, i=i, j=j: e.dma_start(out=tb_[i][:], in_=xall_ap[(2 * j + 1) * 128:(2 * j + 2) * 128, :]),
                    reads=[b_in], writes=[b_b[i]])
            sch.op("dve", lambda e, i=i, sa=sa: e.tensor_scalar(out=ta[i][:], in0=ta[i][:], scalar1=sa, scalar2=None, op0=ALU.mult),
                   reads=[b_a[i], c.b_const], writes=[b_a[i]])
            sch.op("dve", lambda e, i=i, sb_=sb_: e.scalar_tensor_tensor(out=tb_[i][:], in0=tb_[i][:], scalar=sb_, in1=ta[i][:],
                                                                         op0=ALU.mult, op1=ALU.add),
                   reads=[b_a[i], b_b[i], c.b_const], writes=[b_b[i]])
            sch.dma("sp", lambda e, i=i, j=j: e.dma_start(out=xown_ap[j * 128:(j + 1) * 128, :], in_=tb_[i][:]),
                    reads=[b_b[i]], writes=[b_out])
        sch.barrier()
        sch.emit()


NCORE = 8


def build_program(S, depth):
    nc = bass.Bass("TRN2", target_bir_lowering=False)
    c = Ctx()
    c.nc, c.S, c.NB = nc, S, S // 128
    So = S // 2
    x = nc.dram_tensor("x", [S, D], F32, kind="ExternalInput").ap()
    pos = nc.dram_tensor("pos", [S], I32, kind="ExternalInput").ap()
    poso = nc.dram_tensor("poso", [So], I32, kind="ExternalInput").ap()
    masks = nc.dram_tensor("masks", [8, 128, 128], F32, kind="ExternalInput").ap()
    selv = nc.dram_tensor("selv", [4], F32, kind="ExternalInput").ap()
    out = nc.dram_tensor("out", [So, D], F32, kind="ExternalOutput").ap()
    xs = [nc.dram_tensor("xl%d" % l, [S, D], F32, kind="Internal").ap() for l in range(depth - 1)]
    xown = nc.dram_tensor("xown", [So, D], F32, kind="Internal").ap()
    c.w = {}
    for n, shp in WSHAPES.items():
        t = nc.dram_tensor(n, [depth] + list(shp), F32, kind="ExternalInput").ap()
        c.w[n] = [t[l] for l in range(depth)]
    make_scratch(c)
    with contextlib.ExitStack() as es:
        sch = Sched(nc, es)
        setup_consts(c, sch, es)
        setup_masks(c, sch, es)
        setup_percore(c, sch, es, masks, selv)
        setup_rope(c, sch, es, pos, c.NB)
        setup_rope(c, sch, es, poso, c.NB // 2, "_o")
        b_xs = [Buf("xl%d" % l, multi=True, persist=True) for l in range(depth - 1)]
        b_xown = Buf("xown", multi=True, persist=True)
        b_out = Buf("out", multi=True, persist=True)
        cur, b_cur = x, None
        for l in range(depth):
            xblk = (lambda cur: (lambda tb: cur[tb * 128:(tb + 1) * 128, :]))(cur)
            if l < depth - 1:
                layer(c, sch, l, xblk, cur, xs[l], b_xs[l], full=True)
                cur, b_cur = xs[l], b_xs[l]
            else:
                phase_select_own(c, sch, cur, xown, b_cur if b_cur is not None else Buf("xin", multi=True, persist=True), b_xown)
                layer(c, sch, l, xblk, xown, out, b_out, full=False)
    return nc


_PROGS = {}


def kernel(**inputs):
    x = np.ascontiguousarray(np.asarray(inputs["x"], dtype=np.float32))
    positions = np.asarray(inputs["positions"]).astype(np.int32)
    B, S, _ = x.shape
    NB = S // 128
    depth = int(np.asarray(inputs["norm_mix"]).shape[0])
    key = (S, depth)
    if key not in _PROGS:
        _PROGS[key] = build_program(S, depth)
    ws = {n: np.ascontiguousarray(np.asarray(inputs[n], dtype=np.float32)) for n in WSHAPES}
    in_maps, owntoks = [], []
    for core in range(NCORE):
        b, par = core // 2, core % 2
        own = own_blocks(NB, par)
        owntok = np.concatenate([np.arange(i * 128, (i + 1) * 128) for i in own])
        owntoks.append(owntok)
        mk_, sv_ = percore_consts(par)
        m = {"x": x[b], "pos": positions[b], "poso": np.ascontiguousarray(positions[b][owntok]), "masks": mk_, "selv": sv_}
        m.update(ws)
        in_maps.append(m)
    res = run_bass_kernel_spmd(_PROGS[key], in_maps, core_ids=list(range(NCORE)))
    outp = np.empty_like(x)
    for core in range(NCORE):
        outp[core // 2][owntoks[core]] = np.asarray(res.results[core]["out"])
    return outp
```

```python
from concourse.bass_utils import run_bass_kernel_spmd
import contextlib
import numpy as np
import concourse.bass as bass
import concourse.mybir as mybir

F32 = mybir.dt.float32
BF16 = mybir.dt.bfloat16
I32 = mybir.dt.int32
AF = mybir.ActivationFunctionType
ALU = mybir.AluOpType
AX = mybir.AxisListType

ENGS = ("pe", "act", "dve", "pool", "sp")


class Buf:
    def __init__(self, name, multi=False, persist=False):
        self.name = name
        self.multi = multi
        self.persist = persist
        self.writers = {}
        self.readers = {}
        self.sem = None
        self.ndma = 0


class Op:
    __slots__ = ("eng", "fn", "deps", "kind", "buf", "token", "needed", "key")

    def __init__(self, eng, fn, kind):
        self.eng = eng
        self.fn = fn
        self.kind = kind
        self.deps = []
        self.buf = None
        self.token = None
        self.needed = False
        self.key = None


class Sched:
    def __init__(self, nc, es):
        self.nc = nc
        self.es = es
        self.ops = []
        self.esem = {e: es.enter_context(nc.semaphore("sem_" + e)) for e in ENGS}
        self.ecount = {e: 0 for e in ENGS}
        self.seen = {e: {} for e in ENGS}
        self.nsem = 0
        self.last_op = {e: None for e in ENGS}
        self.dma_pending = {}
        self.pool_free = []
        self.phase_bufs = []

    def _bufsem(self, buf):
        if buf.sem is None:
            if self.pool_free and not buf.persist:
                buf.sem, buf.ndma = self.pool_free.pop()
            else:
                buf.sem = self.es.enter_context(self.nc.semaphore("bs%d_%s" % (self.nsem, buf.name)))
                self.nsem += 1
            if not buf.persist:
                self.phase_bufs.append(buf)
        return buf.sem

    def _add(self, op, reads, writes):
        deps = {}
        for b in reads:
            for w in b.writers.values():
                deps[id(w)] = w
        for b in writes:
            for r in b.readers.values():
                deps[id(r)] = r
            if not b.multi:
                for w in b.writers.values():
                    deps[id(w)] = w
        deps.pop(id(op), None)
        op.deps = list(deps.values())
        for d in op.deps:
            d.needed = True
        for b in reads:
            b.readers[op.key] = op
        for b in writes:
            if b.readers or not b.multi:
                b.writers = {}
                b.readers = {}
            b.writers[op.key] = op
        self.ops.append(op)
        self.last_op[op.eng] = op

    def op(self, eng, fn, reads=(), writes=()):
        o = Op(eng, fn, "c")
        o.key = eng
        self._add(o, reads, writes)
        return o

    def dma(self, eng, fn, reads=(), writes=()):
        o = Op(eng, fn, "d")
        b = writes[0]
        o.buf = b
        self._bufsem(b)
        o.key = "dma_" + b.name
        self._add(o, reads, writes)
        self.dma_pending[o.key] = o
        return o

    def barrier(self):
        lasts = [o for o in self.last_op.values() if o is not None]
        pend = list(self.dma_pending.values())
        for e in ENGS:
            o = Op(e, None, "c")
            o.key = e
            o.deps = [d for d in lasts + pend]
            for d in o.deps:
                d.needed = True
            self.ops.append(o)
        self.dma_pending = {}

    def emit(self):
        nc = self.nc
        for o in self.ops:
            if o.kind == "d":
                o.buf.ndma += 1
                o.token = (o.buf.sem, 16 * o.buf.ndma)
            elif o.needed and o.fn is not None:
                self.ecount[o.eng] += 1
                o.token = (self.esem[o.eng], self.ecount[o.eng])
        per = {e: [o for o in self.ops if o.eng == e] for e in ENGS}

        def run(engname, eng):
            seen = self.seen[engname]
            for o in per[engname]:
                need = {}
                stack = list(o.deps)
                while stack:
                    d = stack.pop()
                    if d.token is None:
                        stack.extend(d.deps)
                        continue
                    if d.kind == "c" and d.eng == engname and engname == "pe":
                        continue
                    sem, val = d.token
                    k = id(sem)
                    if k not in need or need[k][1] < val:
                        need[k] = (sem, val)
                for k, (sem, val) in need.items():
                    if seen.get(k, 0) >= val:
                        continue
                    eng.wait_ge(sem, val)
                    seen[k] = val
                if o.fn is None:
                    continue
                inst = o.fn(eng)
                if o.token is not None:
                    sem, val = o.token
                    inst.then_inc(sem, 16 if o.kind == "d" else 1)

        with nc.Block() as block:
            @block.tensor
            def _(t):
                run("pe", t)

            @block.scalar
            def _(s):
                run("act", s)

            @block.vector
            def _(v):
                run("dve", v)

            @block.gpsimd
            def _(g):
                run("pool", g)

            @block.sync
            def _(sy):
                run("sp", sy)
        self.ops = []
        self.last_op = {e: None for e in ENGS}
        for b in self.phase_bufs:
            self.pool_free.append((b.sem, b.ndma))
            b.sem = None
        self.phase_bufs = []


import contextlib
import numpy as np
import concourse.bass as bass
import concourse.mybir as mybir

D = 2048
KD = D // 128
H = 16
EPS = 1e-6
SEG = {}
_o = 0
for _n, _w in (("mla_cq", 512), ("mla_ckv", 512), ("mla_krope", 64), ("sb_q", 1024), ("sb_k", 1024),
               ("sb_v", 1024), ("fox_q", 1024), ("fox_k", 1024), ("fox_v", 1024), ("fox_f", 16),
               ("gates", 6144)):
    SEG[_n] = (_o, _o + _w)
    _o += _w
D_IN = _o


class Ctx:
    pass


_UID = [0]


def uname(n):
    _UID[0] += 1
    return "%s_u%d" % (n, _UID[0])


def own_blocks(nb, par):
    return [i for i in range(nb) if (i % 4 in (0, 3)) == (par == 0)]


def phase_xnT(c, sch, x_ap, gain_ap, xnT_ap, blocks, wr_buf):
    nc = c.nc
    with contextlib.ExitStack() as es:
        T = lambda name, shape, dt: es.enter_context(nc.sbuf_tensor(uname(name), shape, dt))
        xin = [T("xin%d" % i, [128, D], F32) for i in range(2)]
        sq = T("sq", [128, D], F32)
        gbc = T("gbc", [128, D], F32)
        xn = [T("xn%d" % i, [128, D], BF16) for i in range(2)]
        ss = T("ss", [128, 4], F32)
        xt = [T("xt%d" % i, [128, KD, 128], BF16) for i in range(2)]
        pst = [es.enter_context(nc.psum_tensor(uname("pst%d" % i), [128, 4, 128], BF16)) for i in range(4)]
        b_xin = [Buf("xin%d" % i) for i in range(2)]
        b_sq, b_g, b_ss = Buf("sq"), Buf("gbc"), Buf("ss")
        b_xn = [Buf("xn%d" % i) for i in range(2)]
        b_xt = [Buf("xt%d" % i) for i in range(2)]
        b_ps = [Buf("pst%d" % i) for i in range(4)]
        sch.dma("sp", lambda e: e.dma_start(out=gbc[:], in_=gain_ap.partition_broadcast(128)), writes=[b_g])
        for n, tb in enumerate(blocks):
            i = n % 2
            sch.dma("sp", lambda e, i=i, tb=tb: e.dma_start(out=xin[i][:], in_=x_ap(tb)),
                    writes=[b_xin[i]])
            sch.op("act", lambda e, i=i: e.activation(out=sq[:], in_=xin[i][:], func=AF.Square),
                   reads=[b_xin[i]], writes=[b_sq])
            sch.op("dve", lambda e: e.tensor_reduce(out=ss[:, 0:1], in_=sq[:], axis=AX.X, op=ALU.add),
                   reads=[b_sq], writes=[b_ss])
            sch.op("dve", lambda e: e.tensor_scalar(out=ss[:, 1:2], in0=ss[:, 0:1], scalar1=1.0 / D, scalar2=EPS,
                                                    op0=ALU.mult, op1=ALU.add), reads=[b_ss], writes=[b_ss])
            sch.op("act", lambda e: e.activation(out=ss[:, 3:4], in_=ss[:, 1:2], func=AF.Ln), reads=[b_ss], writes=[b_ss])
            sch.op("act", lambda e: e.activation(out=ss[:, 2:3], in_=ss[:, 3:4], func=AF.Exp, scale=-0.5),
                   reads=[b_ss], writes=[b_ss])
            sch.op("dve", lambda e, i=i: e.scalar_tensor_tensor(out=xn[i][:], in0=xin[i][:], scalar=ss[:, 2:3],
                                                                in1=gbc[:], op0=ALU.mult, op1=ALU.mult),
                   reads=[b_xin[i], b_ss, b_g], writes=[b_xn[i]])
            for q in range(4):
                pi = (n * 4 + q) % 4
                for j in range(4):
                    k = q * 4 + j
                    sch.op("pe", lambda e, i=i, k=k, pi=pi, j=j: e.transpose(
                        out=pst[pi][:, j, :], in_=xn[i][:, k * 128:(k + 1) * 128], identity=c.ident[:]),
                        reads=[b_xn[i], c.b_const], writes=[b_ps[pi]])
                eng = "act" if q % 2 else "dve"
                if eng == "act":
                    sch.op("act", lambda e, i=i, q=q, pi=pi: e.copy(out=xt[i][:, q * 4:(q + 1) * 4, :], in_=pst[pi][:]),
                           reads=[b_ps[pi]], writes=[b_xt[i]])
                else:
                    sch.op("dve", lambda e, i=i, q=q, pi=pi: e.tensor_copy(out=xt[i][:, q * 4:(q + 1) * 4, :], in_=pst[pi][:]),
                           reads=[b_ps[pi]], writes=[b_xt[i]])
            sch.dma("sp", lambda e, i=i, tb=tb: e.dma_start(out=xnT_ap[:, :, tb * 128:(tb + 1) * 128], in_=xt[i][:]),
                    reads=[b_xt[i]], writes=[wr_buf])
        sch.barrier()
        sch.emit()


def setup_consts(c, sch, es):
    nc = c.nc
    c.ident = es.enter_context(nc.sbuf_tensor("ident", [128, 128], BF16))
    c.identf = es.enter_context(nc.sbuf_tensor("identf", [128, 128], F32))
    c.b_const = Buf("const")
    sch.op("pool", lambda e: e.memset(c.identf[:], 1.0), writes=[c.b_const])
    sch.op("pool", lambda e: e.affine_select(out=c.identf[:], in_=c.identf[:], pattern=[[-1, 128]],
                                             compare_op=ALU.is_equal, fill=0.0, base=0, channel_multiplier=1),
           reads=[c.b_const], writes=[c.b_const])
    sch.op("pool", lambda e: e.tensor_copy(out=c.ident[:], in_=c.identf[:]), reads=[c.b_const], writes=[c.b_const])


def rstd_ops(sch, ss_ap, tmp_ap, out_ap, d, b_small):
    sch.op("dve", lambda e: e.tensor_scalar(out=tmp_ap, in0=ss_ap, scalar1=1.0 / d, scalar2=EPS,
                                            op0=ALU.mult, op1=ALU.add), reads=[b_small], writes=[b_small])
    sch.op("act", lambda e: e.activation(out=tmp_ap, in_=tmp_ap, func=AF.Ln), reads=[b_small], writes=[b_small])
    sch.op("act", lambda e: e.activation(out=out_ap, in_=tmp_ap, func=AF.Exp, scale=-0.5),
           reads=[b_small], writes=[b_small])


def phase_linear(c, sch, inT_ap, KC, W_ap, chunks, blocks, post, extra_setup=None, TT=8):
    nc = c.nc
    with contextlib.ExitStack() as es:
        T = lambda name, shape, dt: es.enter_context(nc.sbuf_tensor(uname(name), shape, dt))
        inT = T("inT", [128, KC, TT * 128], BF16)
        b_in = Buf("inT")
        wch = [T("wch%d" % i, [128, KC, 512], BF16) for i in range(2)]
        b_w = [Buf("wch%d" % i) for i in range(2)]
        ps = [es.enter_context(nc.psum_tensor(uname("lps%d" % i), [128, 512], F32)) for i in range(3)]
        b_ps = [Buf("lps%d" % i) for i in range(3)]
        env = Ctx()
        env.T, env.es, env.nc = T, es, nc
        if extra_setup is not None:
            extra_setup(env)
        Wv = W_ap.rearrange("(k p) n -> p k n", p=128)
        cnt = 0
        wcnt = 0
        for t0 in range(0, len(blocks), TT):
            tbs = blocks[t0:t0 + TT]
            for j, tb in enumerate(tbs):
                sch.dma("sp", lambda e, j=j, tb=tb: e.dma_start(out=inT[:, :, j * 128:(j + 1) * 128],
                                                                 in_=inT_ap[:, :, tb * 128:(tb + 1) * 128]),
                        writes=[b_in])
            for ci, (lo, n, tag) in enumerate(chunks):
                wi = wcnt % 2
                wcnt += 1
                sch.dma("pool", lambda e, wi=wi, lo=lo, n=n: e.dma_start(out=wch[wi][:, :, 0:n], in_=Wv[:, :, lo:lo + n]),
                        writes=[b_w[wi]])
                for j, tb in enumerate(tbs):
                    pi = cnt % 3
                    cnt += 1
                    for k in range(KC):
                        sch.op("pe", lambda e, pi=pi, k=k, j=j, wi=wi, n=n: e.matmul(
                            ps[pi][:, 0:n], inT[:, k, j * 128:(j + 1) * 128], wch[wi][:, k, 0:n],
                            start=(k == 0), stop=(k == KC - 1)),
                            reads=[b_in, b_w[wi]], writes=[b_ps[pi]])
                    post(env, tag, ci, tb, t0 + j, ps[pi], b_ps[pi])
        sch.barrier()
        sch.emit()


def transpose_out(c, sch, env, src_ap, ncols, dst_fn, b_src, key="tr"):
    nc = env.nc
    if not hasattr(env, "trs"):
        env.trs = [env.es.enter_context(nc.psum_tensor(uname("trp%d" % i), [128, 128], BF16)) for i in range(2)]
        env.b_trs = [Buf("trp%d" % i) for i in range(2)]
        env.tro = [env.T("tro%d" % i, [128, 128], BF16) for i in range(4)]
        env.b_tro = [Buf("tro%d" % i) for i in range(4)]
        env.trn = 0
    n = env.trn
    env.trn += 1
    p, o = n % 2, n % 4
    sch.op("pe", lambda e: e.transpose(out=env.trs[p][0:ncols, :], in_=src_ap, identity=c.ident[:]),
           reads=[b_src, c.b_const], writes=[env.b_trs[p]])
    if n % 2:
        sch.op("act", lambda e: e.copy(out=env.tro[o][0:ncols, :], in_=env.trs[p][0:ncols, :]),
               reads=[env.b_trs[p]], writes=[env.b_tro[o]])
    else:
        sch.op("dve", lambda e: e.tensor_copy(out=env.tro[o][0:ncols, :], in_=env.trs[p][0:ncols, :]),
               reads=[env.b_trs[p]], writes=[env.b_tro[o]])
    dst_ap, b_dst = dst_fn()
    sch.dma("sp", lambda e: e.dma_start(out=dst_ap, in_=env.tro[o][0:ncols, :]), reads=[env.b_tro[o]], writes=[b_dst])


def headnorm(c, sch, env, ps, b_ps, nh, d, gain_t, b_gain, out_ap, b_out):
    if not hasattr(env, "hn_sq"):
        env.hn_sq = env.T("hn_sq", [128, 512], F32)
        env.hn_t = env.T("hn_t", [128, 512], F32)
        env.hn_s = env.T("hn_s", [128, 64], F32)
        env.b_hn = Buf("hn")
        env.b_hns = Buf("hns")
    n = nh * d
    sq, t1, s = env.hn_sq, env.hn_t, env.hn_s
    sch.op("act", lambda e: e.activation(out=sq[:, 0:n], in_=ps[:, 0:n], func=AF.Square), reads=[b_ps], writes=[env.b_hn])
    sch.op("dve", lambda e: e.tensor_reduce(out=s[:, 0:nh], in_=sq[:, 0:n].rearrange("p (h d) -> p h d", d=d),
                                            axis=AX.X, op=ALU.add), reads=[env.b_hn], writes=[env.b_hns])
    rstd_ops(sch, s[:, 0:nh], s[:, 16:16 + nh], s[:, 32:32 + nh], d, env.b_hns)
    sch.op("dve", lambda e: e.tensor_tensor(out=t1[:, 0:n].rearrange("p (h d) -> p h d", d=d),
                                            in0=ps[:, 0:n].rearrange("p (h d) -> p h d", d=d),
                                            in1=s[:, 32:32 + nh].unsqueeze(2).to_broadcast([128, nh, d]), op=ALU.mult),
           reads=[b_ps, env.b_hns, env.b_hn], writes=[env.b_hn])
    sch.op("dve", lambda e: e.tensor_tensor(out=out_ap.rearrange("p (h d) -> p h d", d=d),
                                            in0=t1[:, 0:n].rearrange("p (h d) -> p h d", d=d),
                                            in1=gain_t[:, 0:d].unsqueeze(1).to_broadcast([128, nh, d]), op=ALU.mult),
           reads=[env.b_hn, b_gain], writes=[b_out])


def setup_rope(c, sch, es, pos_ap, NB, suffix=""):
    nc = c.nc
    import math
    cos_t = es.enter_context(nc.sbuf_tensor(uname("cos"), [128, NB, 32], F32))
    sin_t = es.enter_context(nc.sbuf_tensor(uname("sin"), [128, NB, 32], F32))
    setattr(c, "cos" + suffix, cos_t)
    setattr(c, "sin" + suffix, sin_t)
    if not hasattr(c, "b_rope"):
        c.b_rope = Buf("rope")
    with contextlib.ExitStack() as es2:
        T = lambda name, shape, dt: es2.enter_context(nc.sbuf_tensor(uname(name), shape, dt))
        posi = T("posi", [128, NB], I32)
        posf = T("posf", [128, NB], F32)
        invf = T("invf", [128, 32], F32)
        ang = T("ang", [128, NB, 32], F32)
        r = T("rr", [128, NB, 32], F32)
        b = Buf("ropetmp")
        sch.dma("sp", lambda e: e.dma_start(out=posi[:], in_=pos_ap.rearrange("(b p) -> p b", p=128), allow_slow_non_contiguous=True), writes=[b])
        sch.op("dve", lambda e: e.tensor_copy(out=posf[:], in_=posi[:]), reads=[b], writes=[b])
        for i in range(32):
            v = float(np.float32(10000.0) ** np.float32(-(2.0 * i) / 64.0))
            sch.op("pool", lambda e, i=i, v=v: e.memset(invf[:, i:i + 1], v), writes=[b])
        sch.op("dve", lambda e: e.tensor_tensor(out=ang[:], in0=posf[:].unsqueeze(2).to_broadcast([128, NB, 32]),
                                                in1=invf[:].unsqueeze(1).to_broadcast([128, NB, 32]), op=ALU.mult),
               reads=[b], writes=[b])
        qi = T("qi", [128, NB, 32], I32)
        m = T("rm", [128, NB, 32], F32)
        TWO_PI = 2 * math.pi
        for dst, sh in ((sin_t, 0.0), (cos_t, math.pi / 2)):
            sch.op("dve", lambda e, sh=sh: e.tensor_scalar(out=m[:], in0=ang[:], scalar1=sh, scalar2=None, op0=ALU.add),
                   reads=[b], writes=[b])
            sch.op("dve", lambda e: e.tensor_scalar(out=r[:], in0=m[:], scalar1=1.0 / TWO_PI, scalar2=None, op0=ALU.mult),
                   reads=[b], writes=[b])
            sch.op("dve", lambda e: e.tensor_copy(out=qi[:], in_=r[:]), reads=[b], writes=[b])
            sch.op("dve", lambda e: e.tensor_copy(out=r[:], in_=qi[:]), reads=[b], writes=[b])
            sch.op("dve", lambda e: e.scalar_tensor_tensor(out=r[:], in0=r[:], scalar=-TWO_PI, in1=m[:],
                                                           op0=ALU.mult, op1=ALU.add), reads=[b], writes=[b])
            sch.op("dve", lambda e: e.tensor_scalar(out=m[:], in0=r[:], scalar1=math.pi, scalar2=None, op0=ALU.is_gt),
                   reads=[b], writes=[b])
            sch.op("dve", lambda e: e.scalar_tensor_tensor(out=r[:], in0=m[:], scalar=-TWO_PI, in1=r[:],
                                                           op0=ALU.mult, op1=ALU.add), reads=[b], writes=[b])
            sch.op("dve", lambda e: e.tensor_scalar(out=m[:], in0=r[:], scalar1=-math.pi, scalar2=None, op0=ALU.is_lt),
                   reads=[b], writes=[b])
            sch.op("dve", lambda e: e.scalar_tensor_tensor(out=r[:], in0=m[:], scalar=TWO_PI, in1=r[:],
                                                           op0=ALU.mult, op1=ALU.add), reads=[b], writes=[b])
            sch.op("act", lambda e, dst=dst: e.activation(out=dst[:], in_=r[:], func=AF.Sin), reads=[b], writes=[c.b_rope])
        sch.barrier()
        sch.emit()


def rope_ops(c, sch, env, x_ap, out_ap, nh, tb, b_x, b_out, own=False):
    if not hasattr(env, "rp_a"):
        env.rp_a = env.T("rp_a", [128, 16, 32], F32)
        env.rp_b = env.T("rp_b", [128, 16, 32], F32)
        env.b_rp = Buf("rp")
    a, b2 = env.rp_a[:, 0:nh, :], env.rp_b[:, 0:nh, :]
    cos_t, sin_t = (c.cos_o, c.sin_o) if (own and not c.full) else (c.cos, c.sin)
    cs = cos_t[:, tb, :].unsqueeze(1).to_broadcast([128, nh, 32])
    sn = sin_t[:, tb, :].unsqueeze(1).to_broadcast([128, nh, 32])
    x1, x2 = x_ap[:, :, 0:32], x_ap[:, :, 32:64]
    rd = [b_x, c.b_rope, env.b_rp]
    sch.op("dve", lambda e: e.tensor_tensor(out=a, in0=x1, in1=cs, op=ALU.mult), reads=rd, writes=[env.b_rp])
    sch.op("dve", lambda e: e.tensor_tensor(out=b2, in0=x2, in1=sn, op=ALU.mult), reads=rd, writes=[env.b_rp])
    sch.op("dve", lambda e: e.tensor_tensor(out=out_ap[:, :, 0:32], in0=a, in1=b2, op=ALU.subtract),
           reads=[env.b_rp], writes=[b_out])
    sch.op("dve", lambda e: e.tensor_tensor(out=a, in0=x2, in1=cs, op=ALU.mult), reads=rd + [b_out], writes=[env.b_rp])
    sch.op("dve", lambda e: e.tensor_tensor(out=b2, in0=x1, in1=sn, op=ALU.mult), reads=rd, writes=[env.b_rp])
    sch.op("dve", lambda e: e.tensor_tensor(out=out_ap[:, :, 32:64], in0=a, in1=b2, op=ALU.add),
           reads=[env.b_rp], writes=[b_out])


def load_bc(sch, env, name, ap, n):
    t = env.T(name, [128, n], F32)
    b = Buf(name)
    sch.dma("sp", lambda e: e.dma_start(out=t[:], in_=ap.partition_broadcast(128)), writes=[b])
    return t, b


def stage(env, kind):
    if not hasattr(env, "stg"):
        env.stg = {"b": [env.T("stb%d" % i, [128, 512], BF16) for i in range(4)],
                   "f": [env.T("stf%d" % i, [128, 512], F32) for i in range(3)]}
        env.bstg = {"b": [Buf("stb%d" % i) for i in range(4)], "f": [Buf("stf%d" % i) for i in range(3)]}
        env.nstg = {"b": 0, "f": 0}
    i = env.nstg[kind] % len(env.stg[kind])
    env.nstg[kind] += 1
    return env.stg[kind][i], env.bstg[kind][i]


def phase_inproj(c, sch, L):
    W = c.w
    S, So = c.S, c.So
    sc = c.sc
    w_in = W["w_in"][L]

    def chunks_of(names):
        out = []
        for nm in names:
            lo, hi = SEG[nm]
            for q, s0 in enumerate(range(lo, hi, 512)):
                out.append((s0, min(512, hi - s0), (nm, q)))
        return out

    def setup_q(env):
        env.g_cq = load_bc(sch, env, "g_cq", W["mla_cq_norm"][L], 512)
        env.g_sbq = load_bc(sch, env, "g_sbq", W["sb_q_norm"][L], 64)
        env.g_fxq = load_bc(sch, env, "g_fxq", W["fox_q_norm"][L], 64)

    def post_q(env, tag, ci, tb, nl, ps, b_ps):
        nm, q = tag
        tok = slice(nl * 128, (nl + 1) * 128)
        if nm == "mla_cq":
            ob, b_ob = stage(env, "b")
            headnorm(c, sch, env, ps, b_ps, 1, 512, env.g_cq[0], env.g_cq[1], ob[:, 0:512], b_ob)
            for k in range(4):
                transpose_out(c, sch, env, ob[:, k * 128:(k + 1) * 128], 128,
                              lambda k=k: (sc["cqT"][:, k, tok], c.b_sc["cqT"]), b_ob)
        elif nm in ("sb_q", "fox_q"):
            g = env.g_sbq if nm == "sb_q" else env.g_fxq
            dst = "sbQT" if nm == "sb_q" else "foxQT"
            ob, b_ob = stage(env, "b")
            headnorm(c, sch, env, ps, b_ps, 8, 64, g[0], g[1], ob[:, 0:512], b_ob)
            for k in range(4):
                transpose_out(c, sch, env, ob[:, k * 128:(k + 1) * 128], 128,
                              lambda k=k: (sc[dst][q * 4 + k, :, tok], c.b_sc[dst]), b_ob)
        elif nm == "gates":
            ob, b_ob = stage(env, "b")
            sch.op("act", lambda e: e.activation(out=ob[:], in_=ps[:], func=AF.Sigmoid), reads=[b_ps], writes=[b_ob])
            sch.dma("sp", lambda e: e.dma_start(out=sc["gates"][tok, q * 512:(q + 1) * 512], in_=ob[:]),
                    reads=[b_ob], writes=[c.b_sc["gates"]])

    phase_linear(c, sch, sc["xnT"] if c.full else sc["xnTo"], KD, w_in, chunks_of(["mla_cq", "sb_q", "fox_q", "gates"]),
                 list(range(c.NBo)), post_q, setup_q)

    def setup_k(env):
        env.g_ckv = load_bc(sch, env, "g_ckv", W["mla_ckv_norm"][L], 512)
        env.g_sbk = load_bc(sch, env, "g_sbk", W["sb_k_norm"][L], 64)
        env.g_fxk = load_bc(sch, env, "g_fxk", W["fox_k_norm"][L], 64)
        env.fb = load_bc(sch, env, "fbias", W["fox_f_bias"][L], 16)

    def post_k(env, tag, ci, tb, nl, ps, b_ps):
        nm, q = tag
        tok = slice(tb * 128, (tb + 1) * 128)
        if nm == "mla_ckv":
            ob, b_ob = stage(env, "b")
            headnorm(c, sch, env, ps, b_ps, 1, 512, env.g_ckv[0], env.g_ckv[1], ob[:, 0:512], b_ob)
            for k in range(4):
                transpose_out(c, sch, env, ob[:, k * 128:(k + 1) * 128], 128,
                              lambda k=k: (sc["ckvT"][:, k, tok], c.b_sc["ckvT"]), b_ob)
        elif nm in ("sb_k", "fox_k"):
            g = env.g_sbk if nm == "sb_k" else env.g_fxk
            dst = "sbKT" if nm == "sb_k" else "foxKT"
            ob, b_ob = stage(env, "b")
            headnorm(c, sch, env, ps, b_ps, 8, 64, g[0], g[1], ob[:, 0:512], b_ob)
            for k in range(4):
                transpose_out(c, sch, env, ob[:, k * 128:(k + 1) * 128], 128,
                              lambda k=k: (sc[dst][q * 4 + k, :, tok], c.b_sc[dst]), b_ob)
        elif nm in ("sb_v", "fox_v"):
            dst = "sbV" if nm == "sb_v" else "foxV"
            ob, b_ob = stage(env, "b")
            sch.op("act", lambda e: e.copy(out=ob[:], in_=ps[:]), reads=[b_ps], writes=[b_ob])
            sch.dma("sp", lambda e: e.dma_start(out=sc[dst][tok, q * 512:(q + 1) * 512], in_=ob[:]),
                    reads=[b_ob], writes=[c.b_sc[dst]])
        elif nm == "mla_krope":
            of, b_of = stage(env, "f")
            sch.op("act", lambda e: e.copy(out=of[:, 0:64], in_=ps[:, 0:64]), reads=[b_ps], writes=[b_of])
            sch.dma("sp", lambda e: e.dma_start(out=sc["kr"][tok, :], in_=of[:, 0:64]), reads=[b_of], writes=[c.b_sc["kr"]])
        elif nm == "fox_f":
            of, b_of = stage(env, "f")
            sch.op("dve", lambda e: e.tensor_tensor(out=of[:, 0:16], in0=ps[:, 0:16], in1=env.fb[0][:, 0:16], op=ALU.add),
                   reads=[b_ps, env.fb[1]], writes=[b_of])
            sch.op("act", lambda e: e.activation(out=of[:, 16:32], in_=of[:, 0:16], func=AF.Exp, scale=-1.0),
                   reads=[b_of], writes=[b_of])
            sch.op("act", lambda e: e.activation(out=of[:, 32:48], in_=of[:, 16:32], func=AF.Ln, bias=1.0),
                   reads=[b_of], writes=[b_of])
            sch.op("dve", lambda e: e.tensor_scalar(out=of[:, 48:64], in0=of[:, 32:48], scalar1=-1.0, scalar2=None,
                                                    op0=ALU.mult), reads=[b_of], writes=[b_of])
            sch.dma("sp", lambda e: e.dma_start(out=sc["lf"][tok, :], in_=of[:, 48:64]), reads=[b_of], writes=[c.b_sc["lf"]])

    phase_linear(c, sch, sc["xnT"], KD, w_in,
                 chunks_of(["mla_ckv", "mla_krope", "sb_k", "sb_v", "fox_k", "fox_v", "fox_f"]),
                 list(range(c.NB)), post_k, setup_k)


WSHAPES = {
    "norm_mix": (D,), "w_in": (D, D_IN), "mla_cq_norm": (512,), "mla_w_uq": (512, 3072), "mla_ckv_norm": (512,),
    "mla_w_ukv": (512, 4096), "mla_q_norm": (192,), "mla_k_norm": (192,), "sb_q_norm": (64,), "sb_k_norm": (64,),
    "fox_q_norm": (64,), "fox_k_norm": (64,), "fox_f_bias": (16,), "w_branch_mla": (2048, 2048),
    "w_branch_sb": (1024, 2048), "w_branch_fox": (1024, 2048), "w_out": (2048, 2048), "norm_ffn": (D,),
    "w_group": (D, 8), "w_router": (D, 64), "w_e_gate": (64, D, 512), "w_e_up": (64, D, 512), "w_e_down": (64, 512, D),
}


def scratch_shapes(S):
    So = S
    return {
        "xnT": ([128, KD, S], BF16), "xnTo": ([128, KD, So], BF16), "sbQT": ([8, 128, So], BF16), "sbKT": ([8, 128, S], BF16), "sbV": ([S, 1024], BF16),
        "foxQT": ([8, 128, So], BF16), "foxKT": ([8, 128, S], BF16), "foxV": ([S, 1024], BF16),
        "foxQa": ([64, So], BF16), "foxKa": ([64, S], BF16), "lf": ([S, 16], F32),
        "cqT": ([128, 4, So], BF16), "ckvT": ([128, 4, S], BF16), "kr": ([S, 64], F32),
        "mlaQTn": ([16, 128, So], BF16), "mlaQTr": ([16, 64, So], BF16), "mlaKTn": ([16, 128, S], BF16),
        "mlaKTr": ([16, 64, S], BF16), "mlaV": ([S, 2048], BF16), "gates": ([So, 6144], BF16),
        "OT_mla": ([2048, So], BF16), "OT_sb": ([1024, So], BF16), "OT_fox": ([1024, So], BF16),
        "xmid": ([So, D], F32),
    }


def make_scratch(c, dump=()):
    c.sc, c.b_sc = {}, {}
    for nm, (shape, dt) in scratch_shapes(c.S).items():
        kind = "ExternalOutput" if nm in dump else "Internal"
        c.sc[nm] = c.nc.dram_tensor("sc_" + nm, shape, dt, kind=kind).ap()
        c.b_sc[nm] = Buf("sc_" + nm, multi=True)


def setup_masks(c, sch, es):
    nc = c.nc
    mk = lambda n, dt: es.enter_context(nc.sbuf_tensor(uname(n), [128, 128], dt))
    c.mask_incl, c.mask_strict = mk("mincl", BF16), mk("mstrict", BF16)
    c.ntril8, c.nones8, c.ones_bf = mk("ntril8", BF16), mk("nones8", BF16), mk("onesbf", BF16)
    c.triu_f, c.ones_f = mk("triuf", F32), mk("onesf", F32)
    tmp = mk("mtmp", F32)
    b = c.b_const

    def sel(dst, val, pattern, cm, op):
        sch.op("pool", lambda e: e.memset(tmp[:], val), reads=[b], writes=[b])
        sch.op("pool", lambda e: e.affine_select(out=tmp[:], in_=tmp[:], pattern=pattern, compare_op=op, fill=0.0,
                                                 base=0, channel_multiplier=cm), reads=[b], writes=[b])
        sch.op("pool", lambda e: e.tensor_copy(out=dst[:], in_=tmp[:]), reads=[b], writes=[b])
    sel(c.mask_incl, 1.0, [[1, 128]], -1, ALU.is_ge)
    sel(c.mask_strict, 1.0, [[1, 128]], -1, ALU.is_gt)
    sel(c.ntril8, -8.0, [[-1, 128]], 1, ALU.is_ge)
    sel(c.triu_f, 1.0, [[1, 128]], -1, ALU.is_ge)
    sch.op("pool", lambda e: e.memset(c.nones8[:], -8.0), reads=[b], writes=[b])
    sch.op("pool", lambda e: e.memset(c.ones_bf[:], 1.0), reads=[b], writes=[b])
    sch.op("pool", lambda e: e.memset(c.ones_f[:], 1.0), reads=[b], writes=[b])


def phase_attn(c, sch, kind):
    nc = c.nc
    sc, S, So, NB = c.sc, c.S, c.So, c.NB
    dv = 128 if kind == "mla" else 64
    scale = (192 if kind == "mla" else 64) ** -0.5
    with contextlib.ExitStack() as es:
        T = lambda name, shape, dt: es.enter_context(nc.sbuf_tensor(uname(name), shape, dt))
        P = lambda name, shape, dt: es.enter_context(nc.psum_tensor(uname(name), shape, dt))
        nparts = 2 if kind == "mla" else 1
        KT = [[T("kt", [128, S], BF16) for _ in range(nparts)] for _ in range(2)]
        QT = [[T("qt", [128, So], BF16) for _ in range(nparts)] for _ in range(2)]
        Vt = [T("vt", [128, NB, dv], BF16) for _ in range(2)]
        b_kt = [[Buf("kt%d%d" % (i, j)) for j in range(nparts)] for i in range(2)]
        b_qt = [[Buf("qt%d%d" % (i, j)) for j in range(nparts)] for i in range(2)]
        b_kta = [Buf("kta%d" % i) for i in range(2)]
        b_qta = [Buf("qta%d" % i) for i in range(2)]
        b_v = [Buf("vt%d" % i) for i in range(2)]
        ps_s = [P("pss", [128, 4, 128], F32) for _ in range(2)]
        b_ps_s = [Buf("pss%d" % i) for i in range(2)]
        ps_o = [P("pso", [128, 128], F32) for _ in range(2)]
        b_ps_o = [Buf("pso%d" % i) for i in range(2)]
        pt = [T("pt", [128, 4, 128], BF16) for _ in range(3)]
        b_pt = [Buf("pt%d" % i) for i in range(3)]
        osb = [T("osb", [128, 128], BF16) for _ in range(2)]
        b_osb = [Buf("osb%d" % i) for i in range(2)]
        if kind == "sb":
            ps_b = [P("psb", [128, 4, 128], F32) for _ in range(2)]
            b_ps_b = [Buf("psb%d" % i) for i in range(2)]
            et = [T("et", [128, 4, 128], F32) for _ in range(2)]
            b_et = [Buf("et%d" % i) for i in range(2)]
            lb = [T("lb", [128, 4, 128], BF16) for _ in range(2)]
            b_lb = [Buf("lb%d" % i) for i in range(2)]
            rs = [T("rs", [128, 128], BF16) for _ in range(2)]
            b_rs = [Buf("rs%d" % i) for i in range(2)]
        else:
            ps_d = [P("psd", [128, 128], F32) for _ in range(2)]
            b_ps_d = [Buf("psd%d" % i) for i in range(2)]
            rd = [T("rd", [128, 128], F32) for _ in range(2)]
            b_rd = [Buf("rd%d" % i) for i in range(2)]
        if kind == "mla":
            rows = [128, 64]
        elif kind == "fox":
            rows = [68]
        else:
            rows = [64]
        cnt = {"g": 0, "o": 0, "pt": 0, "rs": 0}
        def do_head(h):
            hb = h % 2
            if kind == "mla":
                ksrc = [sc["mlaKTn"][h], sc["mlaKTr"][h]]
                qsrc = [sc["mlaQTn"][h][:, 0:So], sc["mlaQTr"][h][:, 0:So]]
                knames, qnames = ["mlaKTn", "mlaKTr"], ["mlaQTn", "mlaQTr"]
                vsrc, vname = sc["mlaV"][:, h * 128:(h + 1) * 128], "mlaV"
            else:
                pre = "fox" if kind == "fox" else "sb"
                ksrc = [sc[pre + "KT"][h // 2, (h % 2) * 64:(h % 2) * 64 + 64, :]]
                qsrc = [sc[pre + "QT"][h // 2, (h % 2) * 64:(h % 2) * 64 + 64, 0:So]]
                knames, qnames = [pre + "KT"], [pre + "QT"]
                vsrc, vname = sc[pre + "V"][:, h * 64:(h + 1) * 64], pre + "V"
            for p in range(nparts):
                r = min(rows[p], 128 if kind == "mla" else 64)
                sch.dma("sp", lambda e, p=p, r=r: e.dma_start(out=KT[hb][p][0:r, :], in_=ksrc[p]),
                        reads=[c.b_sc[knames[p]]], writes=[b_kt[hb][p]])
                sch.dma("sp", lambda e, p=p, r=r: e.dma_start(out=QT[hb][p][0:r, :], in_=qsrc[p]),
                        reads=[c.b_sc[qnames[p]]], writes=[b_qt[hb][p]])
            kr_, qr_ = [b_kt[hb][p] for p in range(nparts)], [b_qt[hb][p] for p in range(nparts)]
            if kind == "fox":
                sch.dma("sp", lambda e: e.dma_start(out=KT[hb][0][64:68, :], in_=sc["foxKa"][h * 4:(h + 1) * 4, :]),
                        reads=[c.b_sc["foxKa"]], writes=[b_kta[hb]])
                sch.dma("sp", lambda e: e.dma_start(out=QT[hb][0][64:68, :], in_=sc["foxQa"][h * 4:(h + 1) * 4, 0:So]),
                        reads=[c.b_sc["foxQa"]], writes=[b_qta[hb]])
                kr_, qr_ = kr_ + [b_kta[hb]], qr_ + [b_qta[hb]]
            sch.dma("pool", lambda e: e.dma_start(out=Vt[hb][:], in_=vsrc.rearrange("(b p) d -> p b d", p=128)),
                    reads=[c.b_sc[vname]], writes=[b_v[hb]])

            def qk(psum_ap, kb, j, first=True, last=True):
                for p in range(nparts):
                    r = rows[p]
                    sch.op("pe", lambda e, p=p, r=r: e.matmul(
                        psum_ap, KT[hb][p][0:r, kb * 128:(kb + 1) * 128], QT[hb][p][0:r, j * 128:(j + 1) * 128],
                        start=(first and p == 0), stop=(last and p == nparts - 1)), reads=kr_ + qr_, writes=[])

            def do_q(j):
                jp = j % 2
                if c.full:
                    i = j
                    mlist = [(i, c.mask_incl[:], c.mask_strict[:])]
                else:
                    i = 2 * j + 1
                    mlist = [(i - 1 + wch, c.maskt[:, 0 * 4 + jp * 2 + wch, :], c.maskt[:, 1 * 4 + jp * 2 + wch, :])
                             for wch in range(2)]
                oi = cnt["o"] % 2
                cnt["o"] += 1
                groups = [list(range(g0, min(g0 + 4, i + 1))) for g0 in range(0, i + 1, 4)]
                if kind == "sb":
                    groups = [list(reversed(g)) for g in reversed(groups)]
                st = {"nproc": 0}

                def do_grp(grp):
                    gi = cnt["g"] % 2
                    cnt["g"] += 1
                    pi = cnt["pt"] % 3
                    cnt["pt"] += 1
                    n = len(grp)
                    for sl, kb in enumerate(grp):
                        for p in range(nparts):
                            r = rows[p]
                            sch.op("pe", lambda e, p=p, r=r, sl=sl, kb=kb: e.matmul(
                                ps_s[gi][0:128, sl, :], KT[hb][p][0:r, kb * 128:(kb + 1) * 128],
                                QT[hb][p][0:r, j * 128:(j + 1) * 128], start=(p == 0), stop=(p == nparts - 1)),
                                reads=kr_ + qr_, writes=[b_ps_s[gi]])
                    yield
                    if kind != "sb":
                        sch.op("act", lambda e, n=n, gi=gi, pi=pi: e.activation(out=pt[pi][:, 0:n, :], in_=ps_s[gi][:, 0:n, :],
                                                                                func=AF.Exp, scale=scale),
                               reads=[b_ps_s[gi]], writes=[b_pt[pi]])
                        if i in grp:
                            for (mkb, m_incl, m_strict) in mlist:
                                sl = grp.index(mkb)
                                sch.op("dve", lambda e, sl=sl, pi=pi, m_incl=m_incl: e.tensor_tensor(
                                    out=pt[pi][:, sl, :], in0=pt[pi][:, sl, :], in1=m_incl, op=ALU.mult),
                                    reads=[b_pt[pi], c.b_const], writes=[b_pt[pi]])
                        for sl, kb in enumerate(grp):
                            sch.op("pe", lambda e, sl=sl, kb=kb, pi=pi, oi=oi: e.matmul(
                                ps_o[oi][0:dv, :], Vt[hb][:, kb, :], pt[pi][:, sl, :], start=(kb == 0), stop=(kb == i)),
                                reads=[b_v[hb], b_pt[pi]], writes=[b_ps_o[oi]])
                            sch.op("pe", lambda e, sl=sl, kb=kb, pi=pi, oi=oi: e.matmul(
                                ps_d[oi][0:dv, :], c.ones_bf[:, 0:dv], pt[pi][:, sl, :], start=(kb == 0), stop=(kb == i)),
                                reads=[c.b_const, b_pt[pi]], writes=[b_ps_d[oi]])
                    else:
                        ei = gi
                        sch.op("act", lambda e, n=n, gi=gi: e.activation(out=et[gi][:, 0:n, :], in_=ps_s[gi][:, 0:n, :],
                                                                         func=AF.Exp, scale=scale),
                               reads=[b_ps_s[gi]], writes=[b_et[gi]])
                        sch.op("act", lambda e, n=n, gi=gi: e.activation(out=lb[gi][:, 0:n, :], in_=et[gi][:, 0:n, :],
                                                                         func=AF.Ln, bias=1.0),
                               reads=[b_et[gi]], writes=[b_lb[gi]])
                        if i in grp:
                            for (mkb, m_incl, m_strict) in mlist:
                                sl = grp.index(mkb)
                                sch.op("dve", lambda e, sl=sl, gi=gi, m_strict=m_strict: e.tensor_tensor(
                                    out=lb[gi][:, sl, :], in0=lb[gi][:, sl, :], in1=m_strict, op=ALU.mult),
                                    reads=[b_lb[gi], c.b_const], writes=[b_lb[gi]])
                        for sl, kb in enumerate(grp):
                            first = (st["nproc"] == 0)
                            ri = cnt["rs"] % 2
                            for p in range(nparts):
                                r = rows[p]
                                sch.op("pe", lambda e, p=p, r=r, sl=sl, kb=kb: e.matmul(
                                    ps_b[gi][0:128, sl, :], KT[hb][p][0:r, kb * 128:(kb + 1) * 128],
                                    QT[hb][p][0:r, j * 128:(j + 1) * 128], start=True, stop=False),
                                    reads=kr_ + qr_, writes=[b_ps_b[gi]])
                            sch.op("pe", lambda e, sl=sl, first=first: e.matmul(ps_b[gi][0:128, sl, :], c.ntril8[:], lb[gi][:, sl, :],
                                                                   start=False, stop=first),
                                   reads=[c.b_const, b_lb[gi]], writes=[b_ps_b[gi]])
                            if not first:
                                sch.op("pe", lambda e, sl=sl, ri=ri: e.matmul(ps_b[gi][0:128, sl, :], c.nones8[:], rs[ri][:],
                                                                              start=False, stop=True),
                                       reads=[c.b_const, b_rs[ri]], writes=[b_ps_b[gi]])
                            if first:
                                sch.op("dve", lambda e, sl=sl, gi=gi, ri=ri: e.tensor_copy(out=rs[1 - ri][:], in_=lb[gi][:, sl, :]),
                                       reads=[b_lb[gi]], writes=[b_rs[1 - ri]])
                            else:
                                sch.op("dve", lambda e, sl=sl, gi=gi, ri=ri: e.tensor_tensor(
                                    out=rs[1 - ri][:], in0=rs[ri][:], in1=lb[gi][:, sl, :], op=ALU.add),
                                    reads=[b_lb[gi], b_rs[ri]], writes=[b_rs[1 - ri]])
                            cnt["rs"] += 1
                            st["nproc"] += 1
                        sch.op("act", lambda e, n=n, gi=gi, pi=pi: e.activation(out=pt[pi][:, 0:n, :], in_=ps_b[gi][:, 0:n, :],
                                                                                func=AF.Exp, scale=scale),
                               reads=[b_ps_b[gi]], writes=[b_pt[pi]])
                        if i in grp:
                            for (mkb, m_incl, m_strict) in mlist:
                                sl = grp.index(mkb)
                                sch.op("dve", lambda e, sl=sl, pi=pi, m_strict=m_strict: e.tensor_tensor(
                                    out=pt[pi][:, sl, :], in0=pt[pi][:, sl, :], in1=m_strict, op=ALU.mult),
                                    reads=[b_pt[pi], c.b_const], writes=[b_pt[pi]])
                        for sl, kb in enumerate(grp):
                            sch.op("pe", lambda e, sl=sl, kb=kb, pi=pi, oi=oi: e.matmul(
                                ps_o[oi][0:dv, :], Vt[hb][:, kb, :], pt[pi][:, sl, :], start=(kb == i), stop=(kb == 0)),
                                reads=[b_v[hb], b_pt[pi]], writes=[b_ps_o[oi]])
                if kind != "sb":
                    pend = None
                    for grp in groups:
                        g_ = do_grp(grp)
                        next(g_)
                        if pend is not None:
                            for _ in pend:
                                pass
                        pend = g_
                    for _ in pend:
                        pass
                else:
                    for grp in groups:
                        for _ in do_grp(grp):
                            pass
                if kind != "sb":
                    sch.op("dve", lambda e, oi=oi: e.reciprocal(out=rd[oi][0:dv, :], in_=ps_d[oi][0:dv, :]),
                           reads=[b_ps_d[oi]], writes=[b_rd[oi]])
                    sch.op("dve", lambda e, oi=oi: e.tensor_tensor(out=osb[oi][0:dv, :], in0=ps_o[oi][0:dv, :],
                                                                   in1=rd[oi][0:dv, :], op=ALU.mult),
                           reads=[b_ps_o[oi], b_rd[oi]], writes=[b_osb[oi]])
                else:
                    sch.op("act", lambda e, oi=oi: e.copy(out=osb[oi][0:dv, :], in_=ps_o[oi][0:dv, :]),
                           reads=[b_ps_o[oi]], writes=[b_osb[oi]])
                dst = "OT_" + kind
                sch.dma("sp", lambda e, oi=oi, j=j: e.dma_start(out=sc[dst][h * dv:(h + 1) * dv, j * 128:(j + 1) * 128],
                                                                in_=osb[oi][0:dv, :]),
                        reads=[b_osb[oi]], writes=[c.b_sc[dst]])
            for j in range(c.NBo):
                do_q(j)

        for h in range(H):
            do_head(h)
        sch.barrier()
        sch.emit()


def phase_mla_up(c, sch, L):
    W, sc = c.w, c.sc

    def setup_q(env):
        env.g_q = load_bc(sch, env, "g_mq", W["mla_q_norm"][L], 192)
        env.qf = [env.T("mqf%d" % i, [128, 384], F32) for i in range(2)]
        env.b_qf = [Buf("mqf%d" % i) for i in range(2)]
        env.qn = 0

    def post_q(env, tag, ci, tb, nl, ps, b_ps):
        tok = slice(nl * 128, (nl + 1) * 128)
        qi = env.qn % 2
        env.qn += 1
        qf, b_qf = env.qf[qi], env.b_qf[qi]
        headnorm(c, sch, env, ps, b_ps, 2, 192, env.g_q[0], env.g_q[1], qf[:, 0:384], b_qf)
        ob, b_ob = stage(env, "b")
        v3 = qf[:, 0:384].rearrange("p (h d) -> p h d", d=192)
        o3 = ob[:, 0:384].rearrange("p (h d) -> p h d", d=192)
        sch.op("act", lambda e: e.copy(out=o3[:, :, 0:128], in_=v3[:, :, 0:128]), reads=[b_qf], writes=[b_ob])
        rope_ops(c, sch, env, v3[:, :, 128:192], o3[:, :, 128:192], 2, nl, b_qf, b_ob, own=True)
        for hh in range(2):
            h = ci * 2 + hh
            transpose_out(c, sch, env, ob[:, hh * 192:hh * 192 + 128], 128,
                          lambda h=h: (sc["mlaQTn"][h, :, tok], c.b_sc["mlaQTn"]), b_ob)
            transpose_out(c, sch, env, ob[:, hh * 192 + 128:hh * 192 + 192], 64,
                          lambda h=h: (sc["mlaQTr"][h, :, tok], c.b_sc["mlaQTr"]), b_ob)

    chunks = [(i * 384, 384, "mq") for i in range(8)]
    phase_linear(c, sch, sc["cqT"], 4, W["mla_w_uq"][L], chunks, list(range(c.NBo)), post_q, setup_q)

    def setup_k(env):
        env.g_k = load_bc(sch, env, "g_mk", W["mla_k_norm"][L], 192)
        env.krt = [env.T("krt%d" % i, [128, 64], F32) for i in range(2)]
        env.b_krt = [Buf("krt%d" % i) for i in range(2)]
        env.krr = [env.T("krr%d" % i, [128, 64], F32) for i in range(2)]
        env.sm = env.T("mks", [128, 16], F32)
        env.b_sm = Buf("mks")
        env.kf = env.T("mkf", [128, 256], F32)
        env.b_kf = Buf("mkf")
        env.kn = 0
        env.last_tb = None

    def post_k(env, tag, ci, tb, nl, ps, b_ps):
        tok = slice(tb * 128, (tb + 1) * 128)
        ki = env.kn % 2
        env.kn += 1
        krt, krr, b_krt = env.krt[ki], env.krr[ki], env.b_krt[ki]
        sm, b_sm = env.sm, env.b_sm
        sch.dma("sp", lambda e: e.dma_start(out=krt[:], in_=sc["kr"][tok, :]), reads=[c.b_sc["kr"]], writes=[b_krt])
        sch.op("dve", lambda e: e.tensor_tensor(out=krr[:], in0=krt[:], in1=krt[:], op=ALU.mult), reads=[b_krt], writes=[b_krt])
        sch.op("dve", lambda e: e.tensor_reduce(out=sm[:, 0:1], in_=krr[:], axis=AX.X, op=ALU.add), reads=[b_krt], writes=[b_sm])
        sch.op("dve", lambda e: e.tensor_tensor(out=krt[:], in0=krt[:], in1=env.g_k[0][:, 128:192], op=ALU.mult),
               reads=[b_krt, env.g_k[1]], writes=[b_krt])
        rope_ops(c, sch, env, krt[:].rearrange("p (h d) -> p h d", h=1), krr[:].rearrange("p (h d) -> p h d", h=1), 1, tb,
                 b_krt, b_krt)
        kf, b_kf = env.kf, env.b_kf
        p3 = ps[:, 0:512].rearrange("p (h d) -> p h d", d=256)
        k3 = kf[:, 0:256].rearrange("p (h d) -> p h d", d=128)
        sch.op("act", lambda e: e.activation(out=k3, in_=p3[:, :, 0:128], func=AF.Square), reads=[b_ps], writes=[b_kf])
        sch.op("dve", lambda e: e.tensor_reduce(out=sm[:, 1:3], in_=k3, axis=AX.X, op=ALU.add), reads=[b_kf], writes=[b_sm])
        sch.op("dve", lambda e: e.tensor_tensor(out=sm[:, 1:3], in0=sm[:, 1:3], in1=sm[:, 0:1].to_broadcast([128, 2]), op=ALU.add),
               reads=[b_sm], writes=[b_sm])
        rstd_ops(sch, sm[:, 1:3], sm[:, 4:6], sm[:, 8:10], 192, b_sm)
        sch.op("dve", lambda e: e.tensor_tensor(out=k3, in0=p3[:, :, 0:128], in1=sm[:, 8:10].unsqueeze(2).to_broadcast([128, 2, 128]),
                                                op=ALU.mult), reads=[b_ps, b_sm, b_kf], writes=[b_kf])
        ob, b_ob = stage(env, "b")
        o3 = ob[:, 0:512].rearrange("p (h d) -> p h d", d=256)
        sch.op("dve", lambda e: e.tensor_tensor(out=o3[:, :, 0:128], in0=k3,
                                                in1=env.g_k[0][:, 0:128].unsqueeze(1).to_broadcast([128, 2, 128]), op=ALU.mult),
               reads=[b_kf, env.g_k[1]], writes=[b_ob])
        sch.op("act", lambda e: e.copy(out=o3[:, :, 128:256], in_=p3[:, :, 128:256]), reads=[b_ps], writes=[b_ob])
        ob2, b_ob2 = stage(env, "b")
        sch.op("dve", lambda e: e.tensor_tensor(out=ob2[:, 0:128].rearrange("p (h d) -> p h d", d=64),
                                                in0=krr[:].unsqueeze(1).to_broadcast([128, 2, 64]),
                                                in1=sm[:, 8:10].unsqueeze(2).to_broadcast([128, 2, 64]), op=ALU.mult),
               reads=[b_krt, b_sm], writes=[b_ob2])
        for hh in range(2):
            h = ci * 2 + hh
            transpose_out(c, sch, env, ob[:, hh * 256:hh * 256 + 128], 128,
                          lambda h=h: (sc["mlaKTn"][h, :, tok], c.b_sc["mlaKTn"]), b_ob)
            transpose_out(c, sch, env, ob2[:, hh * 64:(hh + 1) * 64], 64,
                          lambda h=h: (sc["mlaKTr"][h, :, tok], c.b_sc["mlaKTr"]), b_ob2)
            sch.dma("sp", lambda e, h=h, hh=hh: e.dma_start(out=sc["mlaV"][tok, h * 128:(h + 1) * 128],
                                                            in_=ob[:, hh * 256 + 128:hh * 256 + 256]),
                    reads=[b_ob], writes=[c.b_sc["mlaV"]])

    chunks = [(i * 512, 512, "mkv") for i in range(8)]
    phase_linear(c, sch, sc["ckvT"], 4, W["mla_w_ukv"][L], chunks, list(range(c.NB)), post_k, setup_k)


def phase_foxcum(c, sch):
    nc, sc, NB = c.nc, c.sc, c.NB
    with contextlib.ExitStack() as es:
        T = lambda name, shape, dt: es.enter_context(nc.sbuf_tensor(uname(name), shape, dt))
        lf = T("lf", [128, NB, 16], F32)
        cum = T("cum", [128, NB, 16], F32)
        pre = T("pre", [128, NB, 16], F32)
        hi = T("hi", [128, NB, 16], BF16)
        hif = T("hif", [128, NB, 16], F32)
        lo = T("lo", [128, NB, 16], BF16)
        aug = [T("aug%d" % i, [128, 16, 4], BF16) for i in range(2)]
        b_aug = [Buf("aug%d" % i) for i in range(2)]
        ps1 = es.enter_context(nc.psum_tensor(uname("cps1"), [128, NB * 16], F32))
        ps2 = es.enter_context(nc.psum_tensor(uname("cps2"), [128, NB * 16], F32))
        b = Buf("cumall")
        seltmp = T("seltmp", [128, 16], BF16)
        b_st = Buf("seltmp")
        b_p1, b_p2 = Buf("cps1"), Buf("cps2")
        env = Ctx()
        env.T, env.es, env.nc = T, es, nc
        sch.dma("sp", lambda e: e.dma_start(out=lf[:], in_=sc["lf"].rearrange("(b p) h -> p b h", p=128)),
                reads=[c.b_sc["lf"]], writes=[b])
        lf2 = lf[:].rearrange("p b h -> p (b h)")
        sch.op("pe", lambda e: e.matmul(ps1[:], c.triu_f[:], lf2, start=True, stop=True), reads=[b, c.b_const], writes=[b_p1])
        sch.op("pe", lambda e: e.matmul(ps2[:], c.ones_f[:], lf2, start=True, stop=True), reads=[b, c.b_const], writes=[b_p2])
        sch.op("dve", lambda e: e.memset(pre[:, 0, :], 0.0), writes=[b])
        p2 = ps2[:].rearrange("p (b h) -> p b h", h=16)
        for bb in range(1, NB):
            sch.op("dve", lambda e, bb=bb: e.tensor_tensor(out=pre[:, bb, :], in0=pre[:, bb - 1, :], in1=p2[:, bb - 1, :], op=ALU.add),
                   reads=[b, b_p2], writes=[b])
        sch.op("dve", lambda e: e.tensor_tensor(out=cum[:], in0=pre[:], in1=ps1[:].rearrange("p (b h) -> p b h", h=16), op=ALU.add),
               reads=[b, b_p1], writes=[b])
        sch.op("dve", lambda e: e.tensor_scalar(out=cum[:], in0=cum[:], scalar1=8.0, scalar2=None, op0=ALU.mult), reads=[b], writes=[b])
        sch.op("dve", lambda e: e.tensor_copy(out=hi[:], in_=cum[:]), reads=[b], writes=[b])
        sch.op("dve", lambda e: e.tensor_copy(out=hif[:], in_=hi[:]), reads=[b], writes=[b])
        sch.op("dve", lambda e: e.tensor_tensor(out=hif[:], in0=cum[:], in1=hif[:], op=ALU.subtract), reads=[b], writes=[b])
        sch.op("dve", lambda e: e.tensor_copy(out=lo[:], in_=hif[:]), reads=[b], writes=[b])
        n = 0
        for side in ("k", "q"):
            blks = list(range(NB)) if side == "k" else list(range(c.NBo))
            for nl, tb in enumerate(blks):
                ai = n % 2
                n += 1
                a, b_a = aug[ai], b_aug[ai]
                if side == "q" and c.full:
                    sch.op("dve", lambda e, a=a, tb=tb: e.tensor_copy(out=a[:, :, 0], in_=hi[:, tb, :]), reads=[b], writes=[b_a])
                    sch.op("dve", lambda e, a=a, tb=tb: e.tensor_copy(out=a[:, :, 1], in_=lo[:, tb, :]), reads=[b], writes=[b_a])
                    sch.op("dve", lambda e, a=a: e.memset(a[:, :, 2:4], 1.0), writes=[b_a])
                    dst, tok = "foxQa", slice(nl * 128, (nl + 1) * 128)
                elif side == "q":
                    sa, sb_ = c.selt[:, 2 * (nl % 2):2 * (nl % 2) + 1], c.selt[:, 2 * (nl % 2) + 1:2 * (nl % 2) + 2]
                    for col, src in ((0, hi), (1, lo)):
                        sch.op("dve", lambda e, src=src, nl=nl, sa=sa: e.tensor_scalar(out=seltmp[:], in0=src[:, 2 * nl, :], scalar1=sa,
                                                                                      scalar2=None, op0=ALU.mult),
                               reads=[b, c.b_const], writes=[b_st])
                        sch.op("dve", lambda e, src=src, nl=nl, sb_=sb_, a=a, col=col: e.scalar_tensor_tensor(
                            out=a[:, :, col], in0=src[:, 2 * nl + 1, :], scalar=sb_, in1=seltmp[:], op0=ALU.mult, op1=ALU.add),
                            reads=[b, b_st, c.b_const], writes=[b_a])
                    sch.op("dve", lambda e, a=a: e.memset(a[:, :, 2:4], 1.0), writes=[b_a])
                    dst, tok = "foxQa", slice(nl * 128, (nl + 1) * 128)
                else:
                    sch.op("dve", lambda e, a=a: e.memset(a[:, :, 0:2], 1.0), writes=[b_a])
                    sch.op("dve", lambda e, a=a, tb=tb: e.tensor_scalar(out=a[:, :, 2], in0=hi[:, tb, :], scalar1=-1.0, scalar2=None,
                                                                        op0=ALU.mult), reads=[b], writes=[b_a])
                    sch.op("dve", lambda e, a=a, tb=tb: e.tensor_scalar(out=a[:, :, 3], in0=lo[:, tb, :], scalar1=-1.0, scalar2=None,
                                                                        op0=ALU.mult), reads=[b], writes=[b_a])
                    dst, tok = "foxKa", slice(tb * 128, (tb + 1) * 128)
                transpose_out(c, sch, env, a[:].rearrange("p h j -> p (h j)"), 64,
                              lambda dst=dst, tok=tok: (sc[dst][:, tok], c.b_sc[dst]), b_a)
        sch.barrier()
        sch.emit()


def phase_merge_out(c, sch, L, x_ap):
    nc, sc, W = c.nc, c.sc, c.w
    NBo = c.NBo
    TT = 4
    with contextlib.ExitStack() as es:
        T = lambda name, shape, dt: es.enter_context(nc.sbuf_tensor(uname(name), shape, dt))
        P = lambda name, shape, dt: es.enter_context(nc.psum_tensor(uname(name), shape, dt))
        brs = [("mla", 16), ("sb", 8), ("fox", 8)]
        ot = [T("ot" + n, [128, kc, TT * 128], BF16) for n, kc in brs]
        b_ot = [Buf("ot" + n) for n, kc in brs]
        wb = [[T("wb" + n, [128, kc, 512], BF16) for n, kc in brs] for _ in range(2)]
        b_wb = [[Buf("wb%d%s" % (i, n)) for n, kc in brs] for i in range(2)]
        mT = T("mT", [128, 16, TT * 128], BF16)
        b_mT = Buf("mT")
        wo = [T("wo", [128, 16, 512], BF16) for _ in range(2)]
        b_wo = [Buf("wo%d" % i) for i in range(2)]
        gt = [T("gt", [128, 3, 512], BF16) for _ in range(2)]
        b_gt = [Buf("gt%d" % i) for i in range(2)]
        t0 = [T("mt0", [128, 512], F32) for _ in range(2)]
        t1 = [T("mt1", [128, 512], F32) for _ in range(2)]
        b_t0 = [Buf("mt0%d" % i) for i in range(2)]
        b_t1 = [Buf("mt1%d" % i) for i in range(2)]
        mb = [T("mb", [128, 512], BF16) for _ in range(2)]
        b_mb = [Buf("mb%d" % i) for i in range(2)]
        xo = [T("xo", [128, 512], F32) for _ in range(2)]
        b_xo = [Buf("xo%d" % i) for i in range(2)]
        psb = [P("psbr", [128, 512], F32) for _ in range(3)]
        b_psb = [Buf("psbr%d" % i) for i in range(3)]
        pst = [P("pstr", [128, 4, 128], BF16) for _ in range(2)]
        b_pst = [Buf("pstr%d" % i) for i in range(2)]
        pso = [P("psout", [128, 512], F32) for _ in range(2)]
        b_pso = [Buf("psout%d" % i) for i in range(2)]
        wsrc = [W["w_branch_mla"][L], W["w_branch_sb"][L], W["w_branch_fox"][L]]
        wv = [w.rearrange("(k p) n -> p k n", p=128) for w in wsrc]
        wov = W["w_out"][L].rearrange("(k p) n -> p k n", p=128)
        ctr = {"w": 0, "g": 0, "t": 0, "o": 0, "x": 0}

        def do_tile(tl0):
            nt = min(TT, NBo - tl0)
            tsl = slice(tl0 * 128, (tl0 + nt) * 128)
            for bi, (n, kc) in enumerate(brs):
                sch.dma("sp", lambda e, bi=bi, n=n: e.dma_start(
                    out=ot[bi][:, :, 0:nt * 128], in_=sc["OT_" + n][:, tsl].rearrange("(k p) t -> p k t", p=128)),
                    reads=[c.b_sc["OT_" + n]], writes=[b_ot[bi]])
            for cc in range(4):
                wi = ctr["w"] % 2
                ctr["w"] += 1
                for bi in range(3):
                    sch.dma("pool", lambda e, bi=bi, wi=wi, cc=cc: e.dma_start(out=wb[wi][bi][:], in_=wv[bi][:, :, cc * 512:(cc + 1) * 512]),
                            writes=[b_wb[wi][bi]])
                for j in range(nt):
                    gi = ctr["g"] % 2
                    ctr["g"] += 1
                    tok = slice((tl0 + j) * 128, (tl0 + j + 1) * 128)
                    sch.dma("sp", lambda e, gi=gi, tok=tok, cc=cc: e.dma_start(
                        out=gt[gi][:], in_=sc["gates"][tok, :].rearrange("t (b n) -> t b n", b=3)[:, :, cc * 512:(cc + 1) * 512]),
                        reads=[c.b_sc["gates"]], writes=[b_gt[gi]])
                    for bi, (n, kc) in enumerate(brs):
                        for k in range(kc):
                            sch.op("pe", lambda e, bi=bi, k=k, kc=kc, j=j, wi=wi: e.matmul(
                                psb[bi][:], ot[bi][:, k, j * 128:(j + 1) * 128], wb[wi][bi][:, k, :],
                                start=(k == 0), stop=(k == kc - 1)), reads=[b_ot[bi], b_wb[wi][bi]], writes=[b_psb[bi]])
                    sch.op("dve", lambda e, gi=gi: e.tensor_tensor(out=t0[gi][:], in0=psb[0][:], in1=gt[gi][:, 0, :], op=ALU.mult),
                           reads=[b_psb[0], b_gt[gi]], writes=[b_t0[gi]])
                    sch.op("dve", lambda e, gi=gi: e.tensor_tensor(out=t1[gi][:], in0=psb[1][:], in1=gt[gi][:, 1, :], op=ALU.mult),
                           reads=[b_psb[1], b_gt[gi]], writes=[b_t1[gi]])
                    sch.op("pool", lambda e, gi=gi: e.tensor_tensor(out=t0[gi][:], in0=t0[gi][:], in1=t1[gi][:], op=ALU.add),
                           reads=[b_t0[gi], b_t1[gi]], writes=[b_t0[gi]])
                    sch.op("dve", lambda e, gi=gi: e.tensor_tensor(out=t1[gi][:], in0=psb[2][:], in1=gt[gi][:, 2, :], op=ALU.mult),
                           reads=[b_psb[2], b_gt[gi]], writes=[b_t1[gi]])
                    sch.op("pool", lambda e, gi=gi: e.tensor_tensor(out=mb[gi][:], in0=t0[gi][:], in1=t1[gi][:], op=ALU.add),
                           reads=[b_t0[gi], b_t1[gi]], writes=[b_mb[gi]])
                    ti = ctr["t"] % 2
                    ctr["t"] += 1
                    for q in range(4):
                        sch.op("pe", lambda e, gi=gi, q=q, ti=ti: e.transpose(out=pst[ti][:, q, :], in_=mb[gi][:, q * 128:(q + 1) * 128],
                                                                              identity=c.ident[:]),
                               reads=[b_mb[gi], c.b_const], writes=[b_pst[ti]])
                    sch.op("act", lambda e, ti=ti, cc=cc, j=j: e.copy(out=mT[:, cc * 4:(cc + 1) * 4, j * 128:(j + 1) * 128], in_=pst[ti][:]),
                           reads=[b_pst[ti]], writes=[b_mT])
            for oc in range(4):
                wi = ctr["o"] % 2
                ctr["o"] += 1
                sch.dma("pool", lambda e, wi=wi, oc=oc: e.dma_start(out=wo[wi][:], in_=wov[:, :, oc * 512:(oc + 1) * 512]),
                        writes=[b_wo[wi]])
                for j in range(nt):
                    xi = ctr["x"] % 2
                    ctr["x"] += 1
                    gb = tl0 + j
                    tok = slice((tl0 + j) * 128, (tl0 + j + 1) * 128)
                    sch.dma("sp", lambda e, xi=xi, gb=gb, oc=oc: e.dma_start(out=xo[xi][:], in_=x_ap[gb * 128:(gb + 1) * 128, oc * 512:(oc + 1) * 512]),
                            writes=[b_xo[xi]])
                    for k in range(16):
                        sch.op("pe", lambda e, xi=xi, k=k, j=j, wi=wi: e.matmul(
                            pso[xi][:], mT[:, k, j * 128:(j + 1) * 128], wo[wi][:, k, :], start=(k == 0), stop=(k == 15)),
                            reads=[b_mT, b_wo[wi]], writes=[b_pso[xi]])
                    sch.op("dve", lambda e, xi=xi: e.tensor_tensor(out=xo[xi][:], in0=pso[xi][:], in1=xo[xi][:], op=ALU.add),
                           reads=[b_pso[xi], b_xo[xi]], writes=[b_xo[xi]])
                    sch.dma("sp", lambda e, xi=xi, tok=tok, oc=oc: e.dma_start(out=sc["xmid"][tok, oc * 512:(oc + 1) * 512], in_=xo[xi][:]),
                            reads=[b_xo[xi]], writes=[c.b_sc["xmid"]])
        for tl0 in range(0, NBo, TT):
            do_tile(tl0)
        sch.barrier()
        sch.emit()


def phase_moe(c, sch, L, out_ap, b_out):
    nc, sc, W = c.nc, c.sc, c.w
    NBo = c.NBo
    TT = 4
    with contextlib.ExitStack() as es:
        T = lambda name, shape, dt: es.enter_context(nc.sbuf_tensor(uname(name), shape, dt))
        P = lambda name, shape, dt: es.enter_context(nc.psum_tensor(uname(name), shape, dt))
        yacc = T("yacc", [128, TT, D], F32)
        b_y = [Buf("yacc%d" % i) for i in range(TT)]
        sq = T("msq", [128, D], F32)
        b_sq = Buf("msq")
        hf = T("mhf", [128, D], F32)
        b_hf = Buf("mhf")
        gbc = T("mgbc", [128, D], F32)
        b_g = Buf("mgbc")
        hT = T("mhT", [128, KD, TT * 128], BF16)
        b_hT = Buf("mhT")
        hhi = T("mhhi", [128, D], BF16)
        hlo = T("mhlo", [128, D], BF16)
        hloT = T("mhloT", [128, KD, 128], BF16)
        b_hhi, b_hlo, b_hloT = Buf("mhhi"), Buf("mhlo"), Buf("mhloT")
        w72 = T("mw72", [128, KD, 72], F32)
        whi = T("mwhi", [128, KD, 72], BF16)
        wlo = T("mwlo", [128, KD, 72], BF16)
        wr = T("mwr", [128, KD, 64], F32)
        wgp = T("mwgp", [128, KD, 8], F32)
        b_wr = Buf("mwr")
        b_wgp = Buf("mwgp")
        sm = T("msm", [128, 512], F32)
        b_sm = Buf("msm")
        coef = T("mcoef", [128, TT, 64], F32)
        b_coef = Buf("mcoef")
        wg = [T("mwg", [128, KD, 512], BF16) for _ in range(1)]
        wu = [T("mwu", [128, KD, 512], BF16) for _ in range(1)]
        wd = [T("mwd", [128, 4, D], BF16) for _ in range(1)]
        b_wg = [Buf("mwg%d" % i) for i in range(2)]
        b_wu = [Buf("mwu%d" % i) for i in range(2)]
        b_wd = [Buf("mwd%d" % i) for i in range(2)]
        aT = [T("maT", [128, 4, TT * 128], BF16) for _ in range(2)]
        b_aT = [Buf("maT%d" % i) for i in range(2)]
        sg = [T("msg", [128, TT * 128], F32) for _ in range(2)]
        b_sg = [Buf("msg%d" % i) for i in range(2)]
        pt = [P("mpt", [128, 4, 128], BF16) for _ in range(1)]
        b_pt = [Buf("mpt%d" % i) for i in range(1)]
        pr = P("mpr", [128, 72], F32)
        b_pr = Buf("mpr")
        pg = [P("mpg", [128, TT * 128], F32) for _ in range(2)]
        pu = [P("mpu", [128, TT * 128], F32) for _ in range(2)]
        b_pg = [Buf("mpg%d" % i) for i in range(2)]
        b_pu = [Buf("mpu%d" % i) for i in range(2)]
        pd = [P("mpd", [128, 512], F32) for _ in range(2)]
        b_pd = [Buf("mpd%d" % i) for i in range(2)]
        ctr = {"t": 0, "w": 0, "m": 0, "d": 0}
        sch.dma("sp", lambda e: e.dma_start(out=gbc[:], in_=W["norm_ffn"][L].partition_broadcast(128)), writes=[b_g])
        import os
        if int(os.environ.get("ROUTE_STAGE", 9)) >= 1:
            sch.dma("pool", lambda e: e.dma_start(out=wgp[:], in_=W["w_group"][L].rearrange("(k p) n -> p k n", p=128)), writes=[b_wgp])
        if int(os.environ.get("ROUTE_STAGE", 9)) >= 1:
            sch.dma("pool", lambda e: e.dma_start(out=wr[:], in_=W["w_router"][L].rearrange("(k p) n -> p k n", p=128)), writes=[b_wr])
        sch.op("dve", lambda e: e.tensor_copy(out=w72[:, :, 0:8], in_=wgp[:]), reads=[b_wgp], writes=[b_wr])
        sch.op("dve", lambda e: e.tensor_copy(out=w72[:, :, 8:72], in_=wr[:]), reads=[b_wr], writes=[b_wr])
        sch.op("dve", lambda e: e.tensor_copy(out=whi[:], in_=w72[:]), reads=[b_wr], writes=[b_wr])
        sch.op("dve", lambda e: e.tensor_tensor(out=wlo[:], in0=w72[:], in1=whi[:], op=ALU.subtract), reads=[b_wr], writes=[b_wr])
        S_ = lambda a, b: sm[:, a:b]

        def route(j, tl):
            tok = slice(tl * 128, (tl + 1) * 128)
            sch.dma("sp", lambda e: e.dma_start(out=yacc[:, j, :], in_=sc["xmid"][tok, :]), reads=[c.b_sc["xmid"]], writes=[b_y[j]])
            sch.op("act", lambda e: e.activation(out=sq[:], in_=yacc[:, j, :], func=AF.Square), reads=[b_y[j]], writes=[b_sq])
            sch.op("dve", lambda e: e.tensor_reduce(out=S_(0, 1), in_=sq[:], axis=AX.X, op=ALU.add), reads=[b_sq], writes=[b_sm])
            rstd_ops(sch, S_(0, 1), S_(1, 2), S_(2, 3), D, b_sm)
            sch.op("dve", lambda e: e.scalar_tensor_tensor(out=hf[:], in0=yacc[:, j, :], scalar=S_(2, 3), in1=gbc[:],
                                                           op0=ALU.mult, op1=ALU.mult), reads=[b_y[j], b_sm, b_g], writes=[b_hf])
            import os
            stage = int(os.environ.get("ROUTE_STAGE", 9))
            if stage < 2:
                return
            sch.op("dve", lambda e: e.tensor_copy(out=hhi[:], in_=hf[:]), reads=[b_hf], writes=[b_hhi])
            sch.op("dve", lambda e: e.tensor_tensor(out=hlo[:], in0=hf[:], in1=hhi[:], op=ALU.subtract),
                   reads=[b_hf, b_hhi], writes=[b_hlo])
            for src, b_src, which in ((hhi, b_hhi, 0), (hlo, b_hlo, 1)):
                for q in range(4):
                    ti = 0
                    for k4 in range(4):
                        k = q * 4 + k4
                        sch.op("pe", lambda e, k=k, k4=k4, ti=ti, src=src: e.transpose(
                            out=pt[ti][:, k4, :], in_=src[:, k * 128:(k + 1) * 128], identity=c.ident[:]),
                            reads=[b_src, c.b_const], writes=[b_pt[ti]])
                    if which == 0:
                        sch.op("act", lambda e, q=q, ti=ti: e.copy(out=hT[:, q * 4:(q + 1) * 4, j * 128:(j + 1) * 128], in_=pt[ti][:]),
                               reads=[b_pt[ti]], writes=[b_hT])
                    else:
                        sch.op("dve", lambda e, q=q, ti=ti: e.tensor_copy(out=hloT[:, q * 4:(q + 1) * 4, :], in_=pt[ti][:]),
                               reads=[b_pt[ti]], writes=[b_hloT])
            if stage < 3:
                return
            combos = [(0, whi), (0, wlo), (1, whi)]
            for ci_, (which, wt) in enumerate(combos):
                for k in range(KD):
                    lhs = hT[:, k, j * 128:(j + 1) * 128] if which == 0 else hloT[:, k, :]
                    sch.op("pe", lambda e, k=k, lhs=lhs, wt=wt, ci_=ci_: e.matmul(
                        pr[:], lhs, wt[:, k, :], start=(ci_ == 0 and k == 0), stop=(ci_ == 2 and k == KD - 1)),
                        reads=[b_hT, b_hloT, b_wr], writes=[b_pr])
            nmax = int(os.environ.get("ROUTE_NOPS", 999))
            cnt_ = {"n": 0}

            def V(fn, extra=()):
                cnt_["n"] += 1
                if cnt_["n"] <= nmax:
                    sch.op("dve", fn, reads=[b_sm] + list(extra), writes=[b_sm])

            def A(fn):
                cnt_["n"] += 1
                if cnt_["n"] <= nmax:
                    sch.op("act", fn, reads=[b_sm], writes=[b_sm])
            lg, lgE = S_(8, 80), S_(16, 80)
            if stage < 4:
                return
            V(lambda e: e.tensor_copy(out=lg, in_=pr[:]), [b_pr])
            V(lambda e: e.tensor_reduce(out=S_(3, 4), in_=S_(8, 16), axis=AX.X, op=ALU.max))
            V(lambda e: e.tensor_scalar(out=S_(80, 88), in0=S_(8, 16), scalar1=S_(3, 4), scalar2=None, op0=ALU.is_equal))
            V(lambda e: e.tensor_scalar(out=S_(4, 5), in0=S_(3, 4), scalar1=-1.0, scalar2=None, op0=ALU.mult))
            A(lambda e: e.activation(out=S_(88, 96), in_=S_(8, 16), func=AF.Exp, bias=S_(4, 5), scale=1.0))
            V(lambda e: e.tensor_reduce(out=S_(5, 6), in_=S_(88, 96), axis=AX.X, op=ALU.add))
            V(lambda e: e.reciprocal(out=S_(5, 6), in_=S_(5, 6)))
            V(lambda e: e.tensor_tensor(out=S_(96, 160).rearrange("p (g j) -> p g j", j=8),
                                        in0=lgE.rearrange("p (g j) -> p g j", j=8),
                                        in1=S_(80, 88).unsqueeze(2).to_broadcast([128, 8, 8]), op=ALU.mult))
            V(lambda e: e.tensor_reduce(out=S_(160, 168), in_=S_(96, 160).rearrange("p (g j) -> p j g", j=8), axis=AX.X, op=ALU.add))
            V(lambda e: e.tensor_reduce(out=S_(6, 7), in_=S_(160, 168), axis=AX.X, op=ALU.max))
            V(lambda e: e.tensor_scalar(out=S_(168, 176), in0=S_(160, 168), scalar1=S_(6, 7), scalar2=None, op0=ALU.is_equal))
            V(lambda e: e.scalar_tensor_tensor(out=S_(176, 184), in0=S_(168, 176), scalar=-1e30, in1=S_(160, 168),
                                               op0=ALU.mult, op1=ALU.add))
            V(lambda e: e.tensor_reduce(out=S_(7, 8), in_=S_(176, 184), axis=AX.X, op=ALU.max))
            V(lambda e: e.tensor_scalar(out=S_(184, 192), in0=S_(176, 184), scalar1=S_(7, 8), scalar2=None, op0=ALU.is_equal))
            V(lambda e: e.tensor_tensor(out=S_(192, 193), in0=S_(7, 8), in1=S_(6, 7), op=ALU.subtract))
            A(lambda e: e.activation(out=S_(193, 194), in_=S_(192, 193), func=AF.Exp))
            V(lambda e: e.tensor_scalar(out=S_(194, 195), in0=S_(193, 194), scalar1=1.0, scalar2=None, op0=ALU.add))
            V(lambda e: e.reciprocal(out=S_(194, 195), in_=S_(194, 195)))
            V(lambda e: e.tensor_tensor(out=S_(195, 196), in0=S_(193, 194), in1=S_(194, 195), op=ALU.mult))
            V(lambda e: e.tensor_tensor(out=S_(194, 195), in0=S_(194, 195), in1=S_(5, 6), op=ALU.mult))
            V(lambda e: e.tensor_tensor(out=S_(195, 196), in0=S_(195, 196), in1=S_(5, 6), op=ALU.mult))
            V(lambda e: e.tensor_scalar(out=S_(200, 208), in0=S_(168, 176), scalar1=S_(194, 195), scalar2=None, op0=ALU.mult))
            V(lambda e: e.scalar_tensor_tensor(out=S_(200, 208), in0=S_(184, 192), scalar=S_(195, 196), in1=S_(200, 208),
                                               op0=ALU.mult, op1=ALU.add))
            V(lambda e: e.tensor_copy(out=S_(208, 272).rearrange("p (g j) -> p g j", j=8),
                                      in_=S_(200, 208).unsqueeze(1).to_broadcast([128, 8, 8])))
            sch.op("dve", lambda e: e.tensor_tensor(out=coef[:, j, :].rearrange("p (g j) -> p g j", j=8),
                                                    in0=S_(208, 272).rearrange("p (g j) -> p g j", j=8),
                                                    in1=S_(80, 88).unsqueeze(2).to_broadcast([128, 8, 8]), op=ALU.mult),
                   reads=[b_sm], writes=[b_coef])

        def expert(e_, nt):
            wi = 0
            N = nt * 128
            sch.dma("pool", lambda e: e.dma_start(out=wg[wi][:], in_=W["w_e_gate"][L][e_].rearrange("(k p) n -> p k n", p=128)),
                    writes=[b_wg[wi]])
            sch.dma("pool", lambda e: e.dma_start(out=wu[wi][:], in_=W["w_e_up"][L][e_].rearrange("(k p) n -> p k n", p=128)),
                    writes=[b_wu[wi]])
            sch.dma("pool", lambda e: e.dma_start(out=wd[wi][:], in_=W["w_e_down"][L][e_].rearrange("(k p) n -> p k n", p=128)),
                    writes=[b_wd[wi]])
            ai = ctr["w"] % 2
            ctr["w"] += 1
            for m in range(4):
                mi = ctr["m"] % 2
                ctr["m"] += 1
                for k in range(KD):
                    sch.op("pe", lambda e, k=k, m=m, mi=mi: e.matmul(pg[mi][:, 0:N], wg[wi][:, k, m * 128:(m + 1) * 128], hT[:, k, 0:N],
                                                                     start=(k == 0), stop=(k == KD - 1)),
                           reads=[b_wg[wi], b_hT], writes=[b_pg[mi]])
                for k in range(KD):
                    sch.op("pe", lambda e, k=k, m=m, mi=mi: e.matmul(pu[mi][:, 0:N], wu[wi][:, k, m * 128:(m + 1) * 128], hT[:, k, 0:N],
                                                                     start=(k == 0), stop=(k == KD - 1)),
                           reads=[b_wu[wi], b_hT], writes=[b_pu[mi]])
                sch.op("act", lambda e, mi=mi: e.activation(out=sg[mi][:, 0:N], in_=pg[mi][:, 0:N], func=AF.Silu),
                       reads=[b_pg[mi]], writes=[b_sg[mi]])
                sch.op("dve", lambda e, mi=mi, m=m: e.tensor_tensor(out=aT[ai][:, m, 0:N], in0=pu[mi][:, 0:N], in1=sg[mi][:, 0:N], op=ALU.mult),
                       reads=[b_pu[mi], b_sg[mi]], writes=[b_aT[ai]])
            for j in range(nt):
                for n in range(4):
                    di = ctr["d"] % 2
                    ctr["d"] += 1
                    for cch in range(4):
                        sch.op("pe", lambda e, cch=cch, j=j, n=n, di=di: e.matmul(pd[di][:], aT[ai][:, cch, j * 128:(j + 1) * 128],
                                                                           wd[wi][:, cch, n * 512:(n + 1) * 512],
                                                                           start=(cch == 0), stop=(cch == 3)),
                               reads=[b_aT[ai], b_wd[wi]], writes=[b_pd[di]])
                    sch.op("dve", lambda e, j=j, n=n, di=di: e.scalar_tensor_tensor(
                        out=yacc[:, j, n * 512:(n + 1) * 512], in0=pd[di][:], scalar=coef[:, j, e_:e_ + 1],
                        in1=yacc[:, j, n * 512:(n + 1) * 512], op0=ALU.mult, op1=ALU.add),
                        reads=[b_pd[di], b_coef, b_y[j]], writes=[b_y[j]])

        def do_tile(tl0):
            nt = min(TT, NBo - tl0)
            for j in range(nt):
                route(j, tl0 + j)
            import os
            for e_ in range(int(os.environ.get("MOE_NE", 64))):
                expert(e_, nt)
            for j in range(nt):
                tl = tl0 + j
                sch.dma("sp", lambda e, j=j, tl=tl: e.dma_start(out=out_ap[tl * 128:(tl + 1) * 128, :], in_=yacc[:, j, :]),
                        reads=[b_y[j]], writes=[b_out])

        for tl0 in range(0, NBo, TT):
            do_tile(tl0)
        sch.barrier()
        sch.emit()


def setup_percore(c, sch, es, masks_ap, selv_ap):
    nc = c.nc
    c.maskt = es.enter_context(nc.sbuf_tensor(uname("maskt"), [128, 8, 128], BF16))
    c.selt = es.enter_context(nc.sbuf_tensor(uname("selt"), [128, 4], F32))
    b = Buf("percore", multi=True, persist=True)
    sch.dma("pool", lambda e: e.dma_start(out=c.maskt[:], in_=masks_ap.rearrange("m p q -> p m q")), writes=[b])
    sch.dma("sp", lambda e: e.dma_start(out=c.selt[:], in_=selv_ap.partition_broadcast(128)), writes=[b])
    sch.op("dve", lambda e: e.tensor_copy(out=c.selt[:], in_=c.selt[:]), reads=[b, c.b_const], writes=[c.b_const])


def percore_consts(par):
    q = np.arange(128)[None, :]
    p = np.arange(128)[:, None]
    tri = {0: (q >= p).astype(np.float32), 1: (q > p).astype(np.float32)}
    ones, zeros = np.ones((128, 128), np.float32), np.zeros((128, 128), np.float32)
    masks = np.zeros((2, 2, 2, 128, 128), np.float32)
    selv = np.zeros((4,), np.float32)
    for jp in range(2):
        first = (par == 0) == (jp == 0)
        selv[2 * jp], selv[2 * jp + 1] = (1.0, 0.0) if first else (0.0, 1.0)
        for kind in range(2):
            masks[kind, jp, 0] = tri[kind] if first else ones
            masks[kind, jp, 1] = zeros if first else tri[kind]
    return masks.reshape(8, 128, 128), selv


def layer(c, sch, L, xblk_all, x_own_ap, out_ap, b_out, full=False):
    c.full = full
    c.NBo = c.NB if full else c.NB // 2
    c.So = c.NBo * 128
    NBo = c.NBo
    phase_xnT(c, sch, xblk_all, c.w["norm_mix"][L], c.sc["xnT"], list(range(c.NB)), c.b_sc["xnT"])
    if not full:
        phase_xnT(c, sch, lambda tb: x_own_ap[tb * 128:(tb + 1) * 128, :], c.w["norm_mix"][L], c.sc["xnTo"],
                  list(range(NBo)), c.b_sc["xnTo"])
    phase_inproj(c, sch, L)
    phase_mla_up(c, sch, L)
    phase_foxcum(c, sch)
    for kind in ("mla", "sb", "fox"):
        phase_attn(c, sch, kind)
    phase_merge_out(c, sch, L, x_own_ap)
    import os
    if not os.environ.get("NOMOE"):
        phase_moe(c, sch, L, out_ap, b_out)


def phase_select_own(c, sch, xall_ap, xown_ap, b_in, b_out):
    nc = c.nc
    NBo = c.NB // 2
    with contextlib.ExitStack() as es:
        T = lambda name, shape, dt: es.enter_context(nc.sbuf_tensor(uname(name), shape, dt))
        ta = [T("sela", [128, D], F32) for _ in range(2)]
        tb_ = [T("selb", [128, D], F32) for _ in range(2)]
        b_a = [Buf("sela%d" % i) for i in range(2)]
        b_b = [Buf("selb%d" % i) for i in range(2)]
        for j in range(NBo):
            i = j % 2
            jp = j % 2
            sa, sb_ = c.selt[:, 2 * jp:2 * jp + 1], c.selt[:, 2 * jp + 1:2 * jp + 2]
            sch.dma("sp", lambda e, i=i, j=j: e.dma_start(out=ta[i][:], in_=xall_ap[(2 * j) * 128:(2 * j + 1) * 128, :]),
                    reads=[b_in], writes=[b_a[i]])
            sch.dma("sp", lambda e, i=i, j=j: e.dma_start(out=tb_[i][:], in_=xall_ap[(2 * j + 1) * 128:(2 * j + 2) * 128, :]),
                    reads=[b_in], writes=[b_b[i]])
            sch.op("dve", lambda e, i=i, sa=sa: e.tensor_scalar(out=ta[i][:], in0=ta[i][:], scalar1=sa, scalar2=None, op0=ALU.mult),
                   reads=[b_a[i], c.b_const], writes=[b_a[i]])
            sch.op("dve", lambda e, i=i, sb_=sb_: e.scalar_tensor_tensor(out=tb_[i][:], in0=tb_[i][:], scalar=sb_, in1=ta[i][:],
                                                                         op0=ALU.mult, op1=ALU.add),
                   reads=[b_a[i], b_b[i], c.b_const], writes=[b_b[i]])
            sch.dma("sp", lambda e, i=i, j=j: e.dma_start(out=xown_ap[j * 128:(j + 1) * 128, :], in_=tb_[i][:]),
                    reads=[b_b[i]], writes=[b_out])
        sch.barrier()
        sch.emit()


NCORE = 8


def build_program(S, depth):
    nc = bass.Bass("TRN2", target_bir_lowering=False)
    c = Ctx()
    c.nc, c.S, c.NB = nc, S, S // 128
    So = S // 2
    x = nc.dram_tensor("x", [S, D], F32, kind="ExternalInput").ap()
    pos = nc.dram_tensor("pos", [S], I32, kind="ExternalInput").ap()
    poso = nc.dram_tensor("poso", [So], I32, kind="ExternalInput").ap()
    masks = nc.dram_tensor("masks", [8, 128, 128], F32, kind="ExternalInput").ap()
    selv = nc.dram_tensor("selv", [4], F32, kind="ExternalInput").ap()
    out = nc.dram_tensor("out", [So, D], F32, kind="ExternalOutput").ap()
    xs = [nc.dram_tensor("xl%d" % l, [S, D], F32, kind="Internal").ap() for l in range(depth - 1)]
    xown = nc.dram_tensor("xown", [So, D], F32, kind="Internal").ap()
    c.w = {}
    for n, shp in WSHAPES.items():
        t = nc.dram_tensor(n, [depth] + list(shp), F32, kind="ExternalInput").ap()
        c.w[n] = [t[l] for l in range(depth)]
    make_scratch(c)
    with contextlib.ExitStack() as es:
        sch = Sched(nc, es)
        setup_consts(c, sch, es)
        setup_masks(c, sch, es)
        setup_percore(c, sch, es, masks, selv)
        setup_rope(c, sch, es, pos, c.NB)
        setup_rope(c, sch, es, poso, c.NB // 2, "_o")
        b_xs = [Buf("xl%d" % l, multi=True, persist=True) for l in range(depth - 1)]
        b_xown = Buf("xown", multi=True, persist=True)
        b_out = Buf("out", multi=True, persist=True)
        cur, b_cur = x, None
        for l in range(depth):
            xblk = (lambda cur: (lambda tb: cur[tb * 128:(tb + 1) * 128, :]))(cur)
            if l < depth - 1:
                layer(c, sch, l, xblk, cur, xs[l], b_xs[l], full=True)
                cur, b_cur = xs[l], b_xs[l]
            else:
                phase_select_own(c, sch, cur, xown, b_cur if b_cur is not None else Buf("xin", multi=True, persist=True), b_xown)
                layer(c, sch, l, xblk, xown, out, b_out, full=False)
    return nc


_PROGS = {}


def kernel(**inputs):
    x = np.ascontiguousarray(np.asarray(inputs["x"], dtype=np.float32))
    positions = np.asarray(inputs["positions"]).astype(np.int32)
    B, S, _ = x.shape
    NB = S // 128
    depth = int(np.asarray(inputs["norm_mix"]).shape[0])
    key = (S, depth)
    if key not in _PROGS:
        _PROGS[key] = build_program(S, depth)
    ws = {n: np.ascontiguousarray(np.asarray(inputs[n], dtype=np.float32)) for n in WSHAPES}
    in_maps, owntoks = [], []
    for core in range(NCORE):
        b, par = core // 2, core % 2
        own = own_blocks(NB, par)
        owntok = np.concatenate([np.arange(i * 128, (i + 1) * 128) for i in own])
        owntoks.append(owntok)
        mk_, sv_ = percore_consts(par)
        m = {"x": x[b], "pos": positions[b], "poso": np.ascontiguousarray(positions[b][owntok]), "masks": mk_, "selv": sv_}
        m.update(ws)
        in_maps.append(m)
    res = run_bass_kernel_spmd(_PROGS[key], in_maps, core_ids=list(range(NCORE)))
    outp = np.empty_like(x)
    for core in range(NCORE):
        outp[core // 2][owntoks[core]] = np.asarray(res.results[core]["out"])
    return outp
```
